# Optimizing a Trainium2 kernel written in Bass

```python
import math
import jax, jax.numpy as jnp
from jax import lax
import numpy as np

D_MODEL = 1024
BATCH = 16
SEQ = 2048
DEPTH = 2

DN_ALPHA = (2.0 * DEPTH) ** 0.25
DN_BETA = (8.0 * DEPTH) ** -0.25
LN_EPS = 1e-5

A_HEAD_DIM = 64
A_HEADS = (D_MODEL // 2) // A_HEAD_DIM
A_KV_HEADS = 2
IDX_HEADS = 4
IDX_DIM = 64
TOPK_MAX = 256
Q_BLOCK = 128

REL_BUCKETS = 32
REL_MAX_DIST = 128

B_HEADS = 4
B_VAL_DIM = (D_MODEL // 2) // B_HEADS
B_KEY_DIM = B_VAL_DIM // 2
GATE_RANK = 16
GATE_TAU = 16.0
GLA_CHUNK = 64

C_GROUP = 16
C_GROUPS = D_MODEL // C_GROUP
C_STATE = 64

MOE_GROUPS = 4
MOE_EXPERTS = 4
MOE_HIDDEN = 512
MOE_TOPK = 2

N_EVEN = (DEPTH + 1) // 2
N_ODD = DEPTH // 2

AB_SPLITS = (A_HEADS * A_HEAD_DIM, A_KV_HEADS * A_HEAD_DIM, A_KV_HEADS * A_HEAD_DIM,
             IDX_HEADS * IDX_DIM, IDX_DIM, IDX_HEADS,
             B_HEADS * B_KEY_DIM, B_HEADS * B_KEY_DIM, B_HEADS * B_VAL_DIM, GATE_RANK,
             B_HEADS * B_VAL_DIM)
AB_PROJ = 2644
AB_OUT = A_HEADS * A_HEAD_DIM + B_HEADS * B_VAL_DIM

kernel_name = "hybrid_dsa_gla_s5_hiermoe_deepnorm"


def layer_norm(x, g, b):
    xf = x.astype(jnp.float32)
    mu = jnp.mean(xf, -1, keepdims=True)
    var = jnp.mean(jnp.square(xf - mu), -1, keepdims=True)
    return ((xf - mu) * lax.rsqrt(var + LN_EPS) * g + b).astype(x.dtype)


def t5_bucket(rel):
    n = jnp.maximum(rel, 0)
    max_exact = REL_BUCKETS // 2
    nf = jnp.maximum(n, max_exact).astype(jnp.float32)
    large = max_exact + (jnp.log(nf / max_exact) / math.log(REL_MAX_DIST / max_exact)
                         * (REL_BUCKETS - max_exact)).astype(jnp.int32)
    large = jnp.minimum(large, REL_BUCKETS - 1)
    return jnp.where(n < max_exact, n, large)


def dsa_attention(q, k, v, iq, ik, iw, rel_bias):
    bsz, L = q.shape[0], q.shape[1]
    k_sel = min(TOPK_MAX, L // 4)
    nb = L // Q_BLOCK
    rep = A_HEADS // A_KV_HEADS
    s_pos = jnp.arange(L, dtype=jnp.int32)
    ikf = ik.astype(jnp.float32)

    def to_blocks(t):
        return jnp.swapaxes(t.reshape((bsz, nb, Q_BLOCK) + t.shape[2:]), 0, 1)

    def block(args):
        bi, qb, iqb, iwb = args
        t_pos = bi * Q_BLOCK + jnp.arange(Q_BLOCK, dtype=jnp.int32)
        dots = jax.nn.relu(jnp.einsum('bthd,bsd->bths', iqb.astype(jnp.float32), ikf) * IDX_DIM ** -0.5)
        score = jnp.einsum('bths,bth->bts', dots, iwb.astype(jnp.float32) * IDX_HEADS ** -0.5)
        causal = s_pos[None, None, :] <= t_pos[None, :, None]
        score = jnp.where(causal, score, -jnp.inf)
        _, idx = lax.top_k(score, k_sel)
        valid = idx <= t_pos[None, :, None]
        kg = jax.vmap(lambda kk, ii: kk[ii])(k, idx)
        vg = jax.vmap(lambda vv, ii: vv[ii])(v, idx)
        bias = rel_bias[t5_bucket(t_pos[None, :, None] - idx)]
        bias = jnp.moveaxis(bias.reshape(bsz, Q_BLOCK, k_sel, A_KV_HEADS, rep), 2, 4)
        qg = qb.reshape(bsz, Q_BLOCK, A_KV_HEADS, rep, A_HEAD_DIM)
        logits = jnp.einsum('btgrd,btkgd->btgrk', qg, kg).astype(jnp.float32) * A_HEAD_DIM ** -0.5
        logits = logits + bias.astype(jnp.float32)
        logits = jnp.where(valid[:, :, None, None, :], logits, -jnp.inf)
        p = jax.nn.softmax(logits, axis=-1).astype(vg.dtype)
        o = jnp.einsum('btgrk,btkgd->btgrd', p, vg)
        return o.reshape(bsz, Q_BLOCK, A_HEADS * A_HEAD_DIM).astype(jnp.float32)

    out = lax.map(block, (jnp.arange(nb, dtype=jnp.int32), to_blocks(q), to_blocks(iq), to_blocks(iw)))
    return jnp.swapaxes(out, 0, 1).reshape(bsz, L, A_HEADS * A_HEAD_DIM)


def gla_attention(q, k, v, log_a):
    bsz, L, H, dk = q.shape
    dv = v.shape[-1]
    nc = L // GLA_CHUNK

    def chunk(t):
        return t.reshape(bsz, nc, GLA_CHUNK, H, t.shape[-1])

    q, k, v, log_a = chunk(q), chunk(k), chunk(v), chunk(log_a)
    cum = lax.cumsum(log_a, axis=2)
    last = cum[:, :, -1:]
    q_dec = q * jnp.exp(cum)
    k_inv = k * jnp.exp(-cum)
    k_end = k * jnp.exp(last - cum)
    mask = jnp.tril(jnp.ones((GLA_CHUNK, GLA_CHUNK), dtype=bool))
    scores = jnp.where(mask, jnp.einsum('bnihd,bnjhd->bnhij', q_dec, k_inv), 0.0)
    o_intra = jnp.einsum('bnhij,bnjhe->bnihe', scores, v)
    upd = jnp.einsum('bnjhd,bnjhe->bnhde', k_end, v)
    decay = jnp.exp(last[:, :, 0])

    def step(S, inp):
        dec, u = inp
        return dec[..., None] * S + u, S

    S0 = jnp.zeros((bsz, H, dk, dv), dtype=q.dtype)
    _, S_prev = lax.scan(step, S0, (jnp.moveaxis(decay, 1, 0), jnp.moveaxis(upd, 1, 0)))
    S_prev = jnp.moveaxis(S_prev, 0, 1)
    o_inter = jnp.einsum('bnihd,bnhde->bnihe', q_dec, S_prev)
    return (o_intra + o_inter).reshape(bsz, L, H, dv)


def even_mixer(x, w_in, rel_bias, gate_w2, gate_b, norm_g, w_out):
    f32 = jnp.float32
    bsz, L, _ = x.shape
    h = x @ w_in
    offsets = np.cumsum(AB_SPLITS)[:-1].tolist()
    aq, ak, av, iq, ik, iw, bq, bk, bv, bg, br = jnp.split(h, offsets, axis=-1)
    o_a = dsa_attention(aq.reshape(bsz, L, A_HEADS, A_HEAD_DIM),
                        ak.reshape(bsz, L, A_KV_HEADS, A_HEAD_DIM),
                        av.reshape(bsz, L, A_KV_HEADS, A_HEAD_DIM),
                        iq.reshape(bsz, L, IDX_HEADS, IDX_DIM), ik, iw, rel_bias)
    log_a = jax.nn.log_sigmoid((bg @ gate_w2 + gate_b).astype(f32)) / GATE_TAU
    o_b = gla_attention(bq.astype(f32).reshape(bsz, L, B_HEADS, B_KEY_DIM) * B_KEY_DIM ** -0.5,
                        bk.astype(f32).reshape(bsz, L, B_HEADS, B_KEY_DIM),
                        bv.astype(f32).reshape(bsz, L, B_HEADS, B_VAL_DIM),
                        log_a.reshape(bsz, L, B_HEADS, B_KEY_DIM))
    mu = jnp.mean(o_b, -1, keepdims=True)
    var = jnp.mean(jnp.square(o_b - mu), -1, keepdims=True)
    o_b = ((o_b - mu) * lax.rsqrt(var + LN_EPS)).reshape(bsz, L, B_HEADS * B_VAL_DIM) * norm_g
    o_b = o_b * jax.nn.silu(br.astype(f32))
    o = jnp.concatenate([o_a, o_b.astype(f32)], axis=-1).astype(x.dtype)
    return o @ w_out


def s5_mixer(x, w_in, lam_re, lam_im, log_dt, b_re, b_im, c_re, c_im, d_skip, glu_w1, glu_w2, w_out):
    f32 = jnp.float32
    bsz, L, _ = x.shape
    u = (x @ w_in).astype(f32).reshape(bsz, L, C_GROUPS, C_GROUP)
    lr = jnp.minimum(lam_re.astype(f32), -1e-4)
    li = lam_im.astype(f32)
    dt = jnp.exp(log_dt.astype(f32))[:, None]
    mag = jnp.exp(lr * dt)
    ab_re = mag * jnp.cos(li * dt)
    ab_im = mag * jnp.sin(li * dt)
    den = lr * lr + li * li
    nr = ab_re - 1.0
    coef_re = (nr * lr + ab_im * li) / den
    coef_im = (ab_im * lr - nr * li) / den
    bb_re = coef_re[..., None] * b_re - coef_im[..., None] * b_im
    bb_im = coef_re[..., None] * b_im + coef_im[..., None] * b_re
    bu_re = jnp.einsum('blgc,gpc->blgp', u, bb_re).astype(f32)
    bu_im = jnp.einsum('blgc,gpc->blgp', u, bb_im).astype(f32)
    a_re = jnp.broadcast_to(ab_re, (1, L, C_GROUPS, C_STATE))
    a_im = jnp.broadcast_to(ab_im, (1, L, C_GROUPS, C_STATE))

    def combine(e1, e2):
        a1r, a1i, b1r, b1i = e1
        a2r, a2i, b2r, b2i = e2
        return (a2r * a1r - a2i * a1i, a2r * a1i + a2i * a1r,
                a2r * b1r - a2i * b1i + b2r, a2r * b1i + a2i * b1r + b2i)

    _, _, s_re, s_im = lax.associative_scan(combine, (a_re, a_im, bu_re, bu_im), axis=1)
    y = (jnp.einsum('blgp,gcp->blgc', s_re, c_re) - jnp.einsum('blgp,gcp->blgc', s_im, c_im)
         + d_skip.reshape(C_GROUPS, C_GROUP) * u)
    g = jax.nn.gelu(y.reshape(bsz, L, D_MODEL).astype(f32))
    z = (g @ glu_w1) * jax.nn.sigmoid(g @ glu_w2)
    return z.astype(x.dtype) @ w_out


def hier_moe(x, r_coarse, rb_coarse, r_fine, rb_fine, w_gate, w_up, w_down):
    f32 = jnp.float32
    bsz, L, d = x.shape
    xt = x.reshape(-1, d)
    gl = (xt @ r_coarse + rb_coarse).astype(f32)
    pg = jax.nn.softmax(gl, axis=-1)
    gsel = jnp.argmax(gl, axis=-1)
    g_onehot = jax.nn.one_hot(gsel, MOE_GROUPS, dtype=f32)
    g_w = jnp.sum(pg * g_onehot, axis=-1)
    fl = (jnp.einsum('nd,gde->nge', xt, r_fine) + rb_fine).astype(f32)
    fl_sel = jnp.take_along_axis(fl, gsel[:, None, None], axis=1)[:, 0]
    top_v, top_i = lax.top_k(fl_sel, MOE_TOPK)
    top_w = jax.nn.softmax(top_v, axis=-1) * g_w[:, None]
    e_gate = jnp.sum(jax.nn.one_hot(top_i, MOE_EXPERTS, dtype=f32) * top_w[..., None], axis=1)
    gate = g_onehot[:, :, None] * e_gate[:, None, :]
    y = jnp.zeros(xt.shape, dtype=f32)
    for g in range(MOE_GROUPS):
        hg = jax.nn.silu(jnp.einsum('nd,edf->nef', xt, w_gate[g])) * jnp.einsum('nd,edf->nef', xt, w_up[g])
        y = y + jnp.einsum('nef,efd->nd', hg * gate[:, g, :, None].astype(hg.dtype), w_down[g])
    return y.reshape(bsz, L, d).astype(x.dtype)


def setup_inputs(seed: int = 0) -> dict:
    key = jax.random.key(seed)
    ks = iter(jax.random.split(key, 40))
    nrm = lambda shape, s: jax.random.normal(next(ks), shape, jnp.float32) * s
    D = D_MODEL
    E, F, G = MOE_EXPERTS, MOE_HIDDEN, MOE_GROUPS
    n_idx = jnp.arange(C_STATE, dtype=jnp.float32)
    return {
        "x": nrm((BATCH, SEQ, D), 1.0),
        "rel_bias": nrm((REL_BUCKETS, A_HEADS), 0.3),
        "ab_w_in": nrm((N_EVEN, D, AB_PROJ), D ** -0.5),
        "gla_gate_w2": nrm((N_EVEN, GATE_RANK, B_HEADS * B_KEY_DIM), GATE_RANK ** -0.5),
        "gla_gate_b": nrm((N_EVEN, B_HEADS * B_KEY_DIM), 0.1),
        "gla_norm_g": 1.0 + nrm((N_EVEN, B_HEADS * B_VAL_DIM), 0.01),
        "ab_w_out": nrm((N_EVEN, AB_OUT, D), AB_OUT ** -0.5 * DN_BETA),
        "s5_w_in": nrm((N_ODD, D, D), D ** -0.5),
        "s5_lam_re": -0.5 + nrm((N_ODD, C_GROUPS, C_STATE), 0.01),
        "s5_lam_im": math.pi * n_idx + nrm((N_ODD, C_GROUPS, C_STATE), 0.01),
        "s5_log_dt": jax.random.uniform(next(ks), (N_ODD, C_GROUPS), jnp.float32,
                                        math.log(1e-3), math.log(1e-1)),
        "s5_b_re": nrm((N_ODD, C_GROUPS, C_STATE, C_GROUP), (2 * C_GROUP) ** -0.5),
        "s5_b_im": nrm((N_ODD, C_GROUPS, C_STATE, C_GROUP), (2 * C_GROUP) ** -0.5),
        "s5_c_re": nrm((N_ODD, C_GROUPS, C_GROUP, C_STATE), C_STATE ** -0.5),
        "s5_c_im": nrm((N_ODD, C_GROUPS, C_GROUP, C_STATE), C_STATE ** -0.5),
        "s5_d": nrm((N_ODD, D), 1.0),
        "s5_glu_w1": nrm((N_ODD, D, D), D ** -0.5),
        "s5_glu_w2": nrm((N_ODD, D, D), D ** -0.5),
        "s5_w_out": nrm((N_ODD, D, D), D ** -0.5 * DN_BETA),
        "ln_mix_g": 1.0 + nrm((DEPTH, D), 0.01),
        "ln_mix_b": nrm((DEPTH, D), 0.01),
        "ln_ffn_g": 1.0 + nrm((DEPTH, D), 0.01),
        "ln_ffn_b": nrm((DEPTH, D), 0.01),
        "moe_r_coarse": nrm((DEPTH, D, G), D ** -0.5),
        "moe_rb_coarse": nrm((DEPTH, G), 0.01),
        "moe_r_fine": nrm((DEPTH, G, D, E), D ** -0.5),
        "moe_rb_fine": nrm((DEPTH, G, E), 0.01),
        "moe_w_gate": nrm((DEPTH, G, E, D, F), D ** -0.5),
        "moe_w_up": nrm((DEPTH, G, E, D, F), D ** -0.5),
        "moe_w_down": nrm((DEPTH, G, E, F, D), F ** -0.5 * DN_BETA),
    }


def reference(x, rel_bias, ab_w_in, gla_gate_w2, gla_gate_b, gla_norm_g, ab_w_out,
              s5_w_in, s5_lam_re, s5_lam_im, s5_log_dt, s5_b_re, s5_b_im, s5_c_re, s5_c_im,
              s5_d, s5_glu_w1, s5_glu_w2, s5_w_out,
              ln_mix_g, ln_mix_b, ln_ffn_g, ln_ffn_b,
              moe_r_coarse, moe_rb_coarse, moe_r_fine, moe_rb_fine,
              moe_w_gate, moe_w_up, moe_w_down):
    h = x
    for layer in range(DEPTH):
        i = layer // 2
        if layer % 2 == 0:
            m = even_mixer(h, ab_w_in[i], rel_bias, gla_gate_w2[i], gla_gate_b[i],
                           gla_norm_g[i], ab_w_out[i])
        else:
            m = s5_mixer(h, s5_w_in[i], s5_lam_re[i], s5_lam_im[i], s5_log_dt[i],
                         s5_b_re[i], s5_b_im[i], s5_c_re[i], s5_c_im[i], s5_d[i],
                         s5_glu_w1[i], s5_glu_w2[i], s5_w_out[i])
        h = layer_norm(DN_ALPHA * h + m.astype(h.dtype), ln_mix_g[layer], ln_mix_b[layer])
        f = hier_moe(h, moe_r_coarse[layer], moe_rb_coarse[layer], moe_r_fine[layer],
                     moe_rb_fine[layer], moe_w_gate[layer], moe_w_up[layer], moe_w_down[layer])
        h = layer_norm(DN_ALPHA * h + f, ln_ffn_g[layer], ln_ffn_b[layer])
    return h.astype(x.dtype)
```

```python
import math
import os
import contextlib
import numpy as np
import concourse.bass as bass
import concourse.mybir as mybir
from concourse.bass_utils import run_bass_kernel_spmd

F32 = mybir.dt.float32
BF16 = mybir.dt.bfloat16
I32 = mybir.dt.int32
ALU = mybir.AluOpType
AF = mybir.ActivationFunctionType
AX = mybir.AxisListType

ENGS = ("pe", "act", "dve", "pool", "sp")
N_DMA_SEMS = 56
NSW = int(os.environ.get('NSW', '3'))

D = 1024
L = 2048
NT = 16
SEQ_PER_CORE = 2
ALPHA = (2.0 * 2) ** 0.25
EPS = 1e-5
NEG = -1e30
NTILE = 31
NG_TOK = SEQ_PER_CORE * NT
T5_STARTS = [1, 2, 3, 4, 5, 6, 7, 8, 9, 10, 11, 12, 13, 14, 15, 16, 19, 21, 24, 27, 31, 35, 40,
             46, 52, 59, 67, 77, 87, 99, 113]


class Buf:
    __slots__ = ("name", "lw", "rd")

    def __init__(self, name=""):
        self.name = name
        self.lw = None
        self.rd = []


class Prog:
    def __init__(self, nc):
        self.nc = nc
        self.ops = []
        self.cur = self.ops
        self.nuid = 0
        self.dma_rr = 0
        self.dma_rr_sw = 0

    def op(self, eng, fn, R=(), W=(), dma=False):
        if getattr(self, "mute", False):
            return None
        deps = set()
        raw = set()
        for b in R:
            if b.lw is not None:
                deps.add(b.lw)
                raw.add(b.lw)
        for b in W:
            if b.lw is not None:
                deps.add(b.lw)
            deps.update(b.rd)
        oid = self.nuid
        self.nuid += 1
        self.cur.append(dict(uid=oid, eng=eng, fn=fn, deps=deps, raw=raw, dma=dma))
        for b in R:
            b.rd.append(oid)
        for b in W:
            b.lw = oid
            b.rd = []
        return oid

    def fork(self, n):
        self.streams = [[] for _ in range(n)]

    def stream(self, k):
        self.cur = self.streams[k] if k is not None else self.ops

    def join(self, ratio=None):
        st = self.streams
        ratio = ratio or [1] * len(st)
        idx = [0] * len(st)
        while any(idx[k] < len(st[k]) for k in range(len(st))):
            for k in range(len(st)):
                for _ in range(ratio[k]):
                    if idx[k] < len(st[k]):
                        self.ops.append(st[k][idx[k]])
                        idx[k] += 1
        self.cur = self.ops
        self.streams = None

    def dma(self, out, in_, R=(), W=(), q="sp", **kw):
        return self.op(q, lambda e: e.dma_start(out=out, in_=in_, **kw), R, W, dma=True)

    def emit(self):
        nc = self.nc
        ops = self.ops
        pos = {o["uid"]: i for i, o in enumerate(ops)}
        for i, o in enumerate(ops):
            o["deps"] = set(pos[d] for d in o["deps"])
            o["raw"] = set(pos[d] for d in o["raw"])
            assert all(d < i for d in o["deps"]), "stream merge broke dependency order"
        eng_idx = {e: 0 for e in ENGS}
        dma_cnt = [0] * N_DMA_SEMS
        dma_last = [None] * N_DMA_SEMS
        for i, o in enumerate(ops):
            if o["dma"]:
                if o["eng"] == "pool":
                    k = N_DMA_SEMS - NSW + self.dma_rr_sw % NSW
                    self.dma_rr_sw += 1
                else:
                    k = self.dma_rr % (N_DMA_SEMS - NSW)
                    self.dma_rr += 1
                if dma_last[k] is not None:
                    o["deps"].add(dma_last[k])
                dma_last[k] = i
                dma_cnt[k] += 1
                o["tok"] = ("d%d" % k, dma_cnt[k])
            else:
                eng_idx[o["eng"]] += 1
                o["tok"] = (o["eng"], eng_idx[o["eng"]])
        eclock = {e: {} for e in ENGS}
        for i, o in enumerate(ops):
            e = o["eng"]
            ck = eclock[e]
            wm = {}
            for d in sorted(o["deps"]):
                od = ops[d]
                s, v = od["tok"]
                if (not od["dma"]) and od["eng"] == e and (e == "pe" or d not in o["raw"]):
                    continue
                if ck.get(s, 0) >= v:
                    continue
                if wm.get(s, 0) < v:
                    wm[s] = v
                for s2, v2 in od["clock"].items():
                    if ck.get(s2, 0) < v2:
                        ck[s2] = v2
            o["waits"] = wm
            c2 = dict(ck)
            c2[o["tok"][0]] = o["tok"][1]
            o["clock"] = c2
        need = {e: set() for e in ENGS}
        for o in ops:
            for s, v in o["waits"].items():
                if s in need:
                    need[s].add(v)
        remap = {e: {v: k + 1 for k, v in enumerate(sorted(need[e]))} for e in ENGS}
        self.stats = {e: (eng_idx[e], len(need[e])) for e in ENGS}
        for o in ops:
            o.pop("clock", None)
        with contextlib.ExitStack() as st:
            sems = {}
            for e in ENGS:
                sems[e] = st.enter_context(nc.semaphore("s_" + e))
            for k in range(N_DMA_SEMS):
                if dma_cnt[k]:
                    sems["d%d" % k] = st.enter_context(nc.semaphore("s_d%d" % k))
            block = st.enter_context(nc.Block())
            per = {e: [o for o in ops if o["eng"] == e] for e in ENGS}
            final_dma = [("d%d" % k, dma_cnt[k] * 16) for k in range(N_DMA_SEMS) if dma_cnt[k]]

            def run(engname, eng):
                for o in per[engname]:
                    for s, v in o["waits"].items():
                        if s in remap:
                            eng.wait_ge(sems[s], remap[s][v])
                        else:
                            eng.wait_ge(sems[s], v * 16)
                    ins = o["fn"](eng)
                    s, v = o["tok"]
                    if o["dma"]:
                        ins.then_inc(sems[s], 16)
                    elif v in remap[s]:
                        ins.then_inc(sems[s], 1)
                if engname == "sp":
                    for s, v in final_dma:
                        eng.wait_ge(sems[s], v)

            @block.tensor
            def _(eng):
                run("pe", eng)

            @block.scalar
            def _(eng):
                run("act", eng)

            @block.vector
            def _(eng):
                run("dve", eng)

            @block.gpsimd
            def _(eng):
                run("pool", eng)

            @block.sync
            def _(eng):
                run("sp", eng)


class Arena:
    def __init__(self, ap, words):
        self.ap = ap
        self.words = words
        self.top = 0
        self.live = []
        self.dead = []

    def mark(self):
        return (self.top, len(self.live))

    def release(self, m):
        top, n = m
        self.dead.extend(self.live[n:])
        del self.live[n:]
        self.top = top

    def alloc(self, shape, dt=F32, nbufs=1, name=""):
        per = int(np.prod(shape[1:]))
        words = (per * (2 if dt == BF16 else 4) + 3) // 4
        words = (words + 7) // 8 * 8
        s, e = self.top, self.top + words
        assert e <= self.words, "arena overflow %s %d > %d" % (name, e, self.words)
        self.top = e
        bufs = [Buf(name) for _ in range(nbufs)]
        keep = []
        for (ds, de, db) in self.dead:
            if ds < e and s < de:
                for ob in db:
                    for nb in bufs:
                        nb.rd.extend(ob.rd)
                        if ob.lw is not None:
                            nb.rd.append(ob.lw)
            keep.append((ds, de, db))
        self.dead = keep
        self.live.append((s, e, bufs))
        v = self.ap[0:shape[0], s:e]
        if dt == BF16:
            v = v.bitcast(BF16)[:, 0:per]
        elif dt == I32:
            v = v.bitcast(I32)[:, 0:per]
        else:
            v = v[:, 0:per]
        if len(shape) == 3:
            v = v.rearrange("p (a b) -> p a b", b=shape[2])
        elif len(shape) == 4:
            v = v.rearrange("p (a b c) -> p a b c", b=shape[2], c=shape[3])
        return (v, bufs[0]) if nbufs == 1 else (v, bufs)


def build(debug=None, stop=None, nqt=NT, nseq=SEQ_PER_CORE, gstop=0):
    nc = bass.Bass("TRN2", target_bir_lowering=False)

    def din(name, shape, dt=F32):
        return nc.dram_tensor(name, list(shape), dt, kind="ExternalInput").ap()

    x = din("x", [SEQ_PER_CORE, L, D])
    rel_bias = din("rel_bias", [32, 8])
    ab_w_in = din("ab_w_in", [1, D, 2644])
    gla_gate_w2 = din("gla_gate_w2", [1, 16, 256])
    gla_gate_b = din("gla_gate_b", [1, 256])
    gla_norm_g = din("gla_norm_g", [1, 512])
    ab_w_out = din("ab_w_out", [1, D, D])
    ln_mix_g = din("ln_mix_g", [2, D])
    ln_mix_b = din("ln_mix_b", [2, D])
    s5_w_in = din("s5_w_in", [1, D, D])
    s5_lam_re = din("s5_lam_re", [1, 64, 64])
    s5_lam_im = din("s5_lam_im", [1, 64, 64])
    s5_log_dt = din("s5_log_dt", [1, 64])
    s5_b_re = din("s5_b_re", [1, 64, 64, 16])
    s5_b_im = din("s5_b_im", [1, 64, 64, 16])
    s5_c_re = din("s5_c_re", [1, 64, 16, 64])
    s5_c_im = din("s5_c_im", [1, 64, 16, 64])
    s5_d = din("s5_d", [1, D])
    s5_glu_w1 = din("s5_glu_w1", [1, D, D])
    s5_glu_w2 = din("s5_glu_w2", [1, D, D])
    s5_w_out = din("s5_w_out", [1, D, D])
    gT_dram = nc.dram_tensor("gT_dram", [SEQ_PER_CORE, 128, 8, L], BF16, kind="Internal").ap()
    ln_ffn_g = din("ln_ffn_g", [2, D])
    ln_ffn_b = din("ln_ffn_b", [2, D])
    moe_r_coarse = din("moe_r_coarse", [2, D, 4])
    moe_rb_coarse = din("moe_rb_coarse", [2, 4])
    moe_r_fine = din("moe_r_fine", [2, 4, D, 4])
    moe_rb_fine = din("moe_rb_fine", [2, 4, 4])
    moe_w_gate = din("moe_w_gate", [2, 4, 4, D, 512])
    moe_w_up = din("moe_w_up", [2, 4, 4, D, 512])
    moe_w_down = din("moe_w_down", [2, 4, 4, 512, D])
    wgb = nc.dram_tensor("wgb", [2, 4, 4, D, 512], BF16, kind="Internal").ap()
    wub = nc.dram_tensor("wub", [2, 4, 4, D, 512], BF16, kind="Internal").ap()
    wdb = nc.dram_tensor("wdb", [2, 4, 4, 512, D], BF16, kind="Internal").ap()
    NSLOT = NTILE * 512
    xs_dram = nc.dram_tensor("xs_dram", [NSLOT, D], BF16, kind="Internal").ap()
    ys_dram = nc.dram_tensor("ys_dram", [NSLOT, D], F32, kind="Internal").ap()
    y = nc.dram_tensor("y", [SEQ_PER_CORE, L, D], F32, kind="ExternalOutput").ap()
    hres = nc.dram_tensor("hres", [SEQ_PER_CORE * L, D], F32, kind="Internal").ap()

    st = contextlib.ExitStack()
    with st:
        AW = 53200
        arena_t = st.enter_context(nc.sbuf_tensor("arena", [128, AW], F32))
        A = Arena(arena_t[:, :], AW)
        pbanks = [st.enter_context(nc.psum_tensor("pb%d" % i, [128, 512], F32))[:, :] for i in range(8)]
        pbuf = [Buf("pb%d" % i) for i in range(8)]
        P = Prog(nc)
        hres_b = [Buf("hres%d" % i) for i in range(SEQ_PER_CORE * NT)]
        y_b = Buf("y")

        def pbf(i):
            return pbanks[i].bitcast(BF16)

        rr = {"cp": 0}

        def copy_rr(out, in_, R, W):
            rr["cp"] += 1
            if rr.get("mode") == "act" or rr["cp"] % 2:
                P.op("act", lambda e: e.copy(out=out, in_=in_), R=R, W=W)
            else:
                P.op("dve", lambda e: e.tensor_copy(out=out, in_=in_), R=R, W=W)

        ident, b_ident = A.alloc([128, 128], F32, name="ident")
        identb, b_identb = A.alloc([128, 128], BF16, name="identb")
        P.op("pool", lambda e: e.memset(ident, 1.0), W=[b_ident])
        P.op("pool", lambda e: e.affine_select(out=ident, in_=ident, pattern=[[-1, 128]], compare_op=ALU.is_equal,
                                               fill=0.0, base=0, channel_multiplier=1), R=[b_ident], W=[b_ident])
        P.op("dve", lambda e: e.tensor_copy(out=identb, in_=ident), R=[b_ident], W=[b_identb])

        def wload(shape_cols, src, name):
            t, b = A.alloc([128, 8, shape_cols], BF16, name=name)
            return t, b

        def wdma(dst, b, src2d):
            P.dma(dst, src2d.rearrange("(kc p) n -> p kc n", p=128), W=[b], q="pool")

        def layer_norm_tile(r, b_r, g_bc, b_bc, b_gb, out, b_out, b_bb=None, tmps=None):
            if tmps is not None:
                stt, b_st, mv, b_mv, sd, b_sd = tmps
            else:
                stt, b_st = A.alloc([128, 2, 6], F32, name="lnst")
                mv, b_mv = A.alloc([128, 2], F32, name="lnmv")
            for hh in range(2):
                P.op("dve", (lambda hh: lambda e: e.bn_stats(out=stt[:, hh, :], in_=r[:, hh * 512:(hh + 1) * 512]))(hh),
                     R=[b_r], W=[b_st])
            P.op("dve", lambda e: e.bn_aggr(out=mv, in_=stt.rearrange("p a b -> p (a b)")), R=[b_st], W=[b_mv])
            if tmps is None:
                sd, b_sd = A.alloc([128, 2], F32, name="lnsd")
            P.op("act", lambda e: e.activation(out=sd[:, 0:1], in_=mv[:, 1:2], func=AF.Sqrt, bias=eps_t[:, 0:1], scale=1.0),
                 R=[b_mv, b_eps], W=[b_sd])
            P.op("dve", lambda e: e.reciprocal(out=sd[:, 1:2], in_=sd[:, 0:1]), R=[b_sd], W=[b_sd])
            P.op("dve", lambda e: e.tensor_scalar(out=r, in0=r, scalar1=mv[:, 0:1], scalar2=sd[:, 1:2],
                                                  op0=ALU.subtract, op1=ALU.mult), R=[b_r, b_mv, b_sd], W=[b_r])
            P.op("pool", lambda e: e.tensor_tensor(out=r, in0=r, in1=g_bc, op=ALU.mult), R=[b_r, b_gb], W=[b_r])
            P.op("pool", lambda e: e.tensor_tensor(out=out, in0=r, in1=b_bc, op=ALU.add), R=[b_r, b_gb] + ([b_bb] if b_bb is not None else []), W=[b_out])

        bar_t, b_bar = A.alloc([128, 8], F32, name="bar")

        def barrier():
            bufs = [b for (_, _, bs) in A.live + A.dead for b in bs] + pbuf + hres_b + [b_bar]
            seen = {}
            for b in bufs:
                seen[id(b)] = b
            bufs = list(seen.values())
            P.op("dve", lambda e: e.memset(bar_t, 0.0), R=bufs, W=bufs)

        eps_t, b_eps = A.alloc([128, 1], F32, name="eps")
        P.op("pool", lambda e: e.memset(eps_t, EPS), W=[b_eps])
        halfpi, b_hp = A.alloc([128, 1], F32, name="halfpi")
        P.op("pool", lambda e: e.memset(halfpi, math.pi / 2), W=[b_hp])

        m_l0 = A.mark()
        rr["mode"] = "act"
        W0 = ab_w_in[0]
        Wq, b_Wq = wload(512, None, "Wq")
        Wiq, b_Wiq = wload(256, None, "Wiq")
        Wiw, b_Wiw = wload(4, None, "Wiw")
        Wk, b_Wk = wload(256, None, "Wk")
        Wik, b_Wik = wload(128, None, "Wik")
        Wv, b_Wv = wload(128, None, "Wv")
        Wbq, b_Wbq = wload(256, None, "Wbq")
        Wbk, b_Wbk = wload(256, None, "Wbk")
        Wbv, b_Wbv = wload(512, None, "Wbv")
        Wbg, b_Wbg = wload(16, None, "Wbg")
        Wbr, b_Wbr = wload(512, None, "Wbr")
        Wo, b_Wo = wload(1024, None, "Wo")
        wdma(Wq, b_Wq, W0[:, 0:512])
        for g in range(2):
            for r2 in range(2):
                c0 = (2 * g + r2) * 64
                P.dma(Wk[:, :, c0:c0 + 64], W0[:, 512 + g * 64:512 + (g + 1) * 64].rearrange("(kc p) n -> p kc n", p=128),
                      W=[b_Wk], q="pool")
        wdma(Wv, b_Wv, W0[:, 640:768])
        wdma(Wiq, b_Wiq, W0[:, 768:1024])
        for r2 in range(2):
            P.dma(Wik[:, :, r2 * 64:(r2 + 1) * 64], W0[:, 1024:1088].rearrange("(kc p) n -> p kc n", p=128),
                  W=[b_Wik], q="pool")
        wdma(Wiw, b_Wiw, W0[:, 1088:1092])
        wdma(Wbq, b_Wbq, W0[:, 1092:1348])
        wdma(Wbk, b_Wbk, W0[:, 1348:1604])
        wdma(Wbv, b_Wbv, W0[:, 1604:2116])
        wdma(Wbg, b_Wbg, W0[:, 2116:2132])
        wdma(Wbr, b_Wbr, W0[:, 2132:2644])
        wdma(Wo, b_Wo, ab_w_out[0])

        LG, b_LG = A.alloc([128, 1024], F32, name="LG")
        LB, b_LB = A.alloc([128, 1024], F32, name="LB")
        P.dma(LG, ln_mix_g[0].partition_broadcast(128), W=[b_LG])
        P.dma(LB, ln_mix_b[0].partition_broadcast(128), W=[b_LB])
        NG, b_NG = A.alloc([128, 512], F32, name="NG")
        P.dma(NG, gla_norm_g[0].partition_broadcast(128), W=[b_NG])
        G2, b_G2 = A.alloc([32, 256], F32, name="G2")
        P.op("pool", lambda e: e.memset(G2, 0.0), W=[b_G2])
        P.dma(G2[0:16, :], gla_gate_w2[0], W=[b_G2])
        P.dma(G2[16:17, :], gla_gate_b[0:1, :], W=[b_G2])

        TRI, b_TRI = A.alloc([128, 128], F32, name="TRI")
        TRI2, b_TRI2 = A.alloc([128, 128], F32, name="TRI2")
        MASKT, b_MASKT = A.alloc([128, 128], F32, name="MASKT")
        for (T_, b_, val, strict) in ((TRI, b_TRI, -1.0 / 16, False), (MASKT, b_MASKT, 1.0, False), (TRI2, b_TRI2, -1.0 / 16, True)):
            P.op("pool", (lambda T_, val: lambda e: e.memset(T_, val))(T_, val), W=[b_])
            if not strict:
                P.op("pool", (lambda T_: lambda e: e.affine_select(out=T_, in_=T_, pattern=[[1, 128]], compare_op=ALU.is_ge,
                                                                   fill=0.0, base=0, channel_multiplier=-1))(T_), R=[b_], W=[b_])
                P.op("pool", (lambda T_: lambda e: e.memset(T_[0:64, 64:128], 0.0))(T_), R=[b_], W=[b_])
            else:
                P.op("pool", (lambda T_: lambda e: e.affine_select(out=T_, in_=T_, pattern=[[-1, 128]], compare_op=ALU.is_ge,
                                                                   fill=0.0, base=-1, channel_multiplier=1))(T_), R=[b_], W=[b_])
                P.op("pool", (lambda T_: lambda e: e.memset(T_[64:128, 0:64], 0.0))(T_), R=[b_], W=[b_])

        Bp, b_Bp = A.alloc([128, 8, 256], F32, name="Bp")
        m_tmp = A.mark()
        RB, b_RB = A.alloc([128, 32, 8], F32, name="RB")
        DL, b_DL = A.alloc([128, 32, 8], F32, name="DL")
        P.dma(RB.rearrange("p a b -> p (a b)"), rel_bias.rearrange("a b -> (a b)").partition_broadcast(128), W=[b_RB])
        P.op("dve", lambda e: e.tensor_tensor(out=DL[:, 1:32, :], in0=RB[:, 1:32, :], in1=RB[:, 0:31, :], op=ALU.subtract),
             R=[b_RB], W=[b_DL])
        P.op("dve", lambda e: e.tensor_tensor(out=DL[:, 0:1, :], in0=RB[:, 0:1, :], in1=RB[:, 31:32, :], op=ALU.subtract),
             R=[b_RB], W=[b_DL])
        Dt, b_Dt = A.alloc([128, 256], F32, name="Dt")
        Ib, b_Ib = A.alloc([128, 31, 256], BF16, name="Ib")
        P.op("pool", lambda e: e.iota(out=Dt, pattern=[[-1, 256]], base=128, channel_multiplier=1,
                                      allow_small_or_imprecise_dtypes=True), W=[b_Dt])
        for bi, sv in enumerate(T5_STARTS):
            P.op("dve", (lambda bi, sv: lambda e: e.tensor_scalar(out=Ib[:, bi, :], in0=Dt, scalar1=float(sv) - 0.5, scalar2=None,
                                                                  op0=ALU.is_ge))(bi, sv), R=[b_Dt], W=[b_Ib])
        for h in range(8):
            P.op("dve", (lambda h: lambda e: e.tensor_scalar(out=Bp[:, h, :], in0=Ib[:, 0, :], scalar1=DL[:, 1, h:h + 1],
                                                             scalar2=DL[:, 0, h:h + 1], op0=ALU.mult, op1=ALU.add))(h),
                 R=[b_Ib, b_DL], W=[b_Bp])
            for bi in range(1, 31):
                P.op("dve", (lambda h, bi: lambda e: e.scalar_tensor_tensor(out=Bp[:, h, :], in0=Ib[:, bi, :],
                                                                           scalar=DL[:, bi + 1, h:h + 1], in1=Bp[:, h, :],
                                                                           op0=ALU.mult, op1=ALU.add))(h, bi),
                     R=[b_Ib, b_DL, b_Bp], W=[b_Bp])
        A.release(m_tmp)

        bg1, b_bg1 = A.alloc([32, 128], F32, name="bg1")
        P.op("pool", lambda e: e.memset(bg1, 1.0), W=[b_bg1])

        hT, b_hT = A.alloc([128, 8, L], BF16, nbufs=NT, name="hT")
        kTd, b_kTd = A.alloc([128, 2, L], BF16, name="kTd")
        ikT2, b_ikT2 = A.alloc([128, L], BF16, name="ikT2")
        vtok, b_vtok = A.alloc([128, NT, 128], BF16, name="vtok")
        xs, b_xs = A.alloc([128, 1, 1024], F32, nbufs=1, name="xs")
        b_xs = [b_xs, b_xs]
        sc, b_sc = A.alloc([128, L], F32, name="sc")
        rt, b_rt = A.alloc([128, 2, 512], F32, nbufs=2, name="rt")
        selm2, b_selm2 = A.alloc([128, 2, L], BF16, nbufs=2, name="selm1")
        lg2, b_lg2 = A.alloc([128, 2, L], F32, nbufs=2, name="lg")
        pp2, b_pp2 = A.alloc([128, 2, L], BF16, nbufs=2, name="pp")
        pT2, b_pT2 = A.alloc([128, 1, L], BF16, nbufs=1, name="pT")
        b_pT2 = [b_pT2, b_pT2]
        qTt2, b_qTt2 = A.alloc([128, 2, 4, 128], BF16, nbufs=2, name="qTt")
        iqTt, b_iqTt = A.alloc([128, 2, 128], BF16, name="iqTt")
        iwt, b_iwt = A.alloc([128, 4], F32, name="iwt")
        m8, b_m8 = A.alloc([128, 8], F32, name="m8")
        MB = 22
        TOPK_BISECT = os.environ.get("TOPK", "bisect") == "bisect"
        tkb, b_tkb = A.alloc([128, 16], F32, name="tkb")
        Wbis, b_Wbis = A.alloc([128, 24], F32, name="Wbis")
        POW2, b_POW2 = A.alloc([128, 24], F32, name="POW2")
        for k in range(MB):
            P.op("pool", (lambda k: lambda e: e.memset(POW2[:, k:k + 1], 2.0 ** -(k + 1)))(k), W=[b_POW2])
        sm2, b_sm2 = A.alloc([128, 2, 4], F32, nbufs=2, name="sm")
        oa, b_oa = A.alloc([128, 1024], BF16, name="oa")
        oT, b_oT = A.alloc([128, 8, 128], BF16, name="oT")
        lap, b_lap = A.alloc([128, 256], F32, name="lap")
        EcT, b_EcT = A.alloc([128, 2, 128], F32, name="EcT")
        EnT, b_EnT = A.alloc([128, 2, 128], F32, name="EnT")
        Er, b_Er = A.alloc([128, 256], F32, name="Er")
        qdT, b_qdT = A.alloc([128, 2, 128], BF16, name="qdT")
        kiT, b_kiT = A.alloc([128, 2, 128], BF16, name="kiT")
        kend, b_kend = A.alloc([128, 256], BF16, name="kend")
        vt, b_vt = A.alloc([128, 512], BF16, name="vt")
        sbr, b_sbr = A.alloc([128, 512], F32, name="sbr")
        scT, b_scT = A.alloc([128, 4, 128], BF16, name="scT")
        Sf, b_Sf = A.alloc([128, 2, 128], F32, name="Sf")
        Sb, b_Sb = A.alloc([128, 2, 128], BF16, name="Sb")
        on, b_on = A.alloc([128, 512], F32, name="on")
        gst, b_gst = A.alloc([128, 4, 6], F32, name="gst")
        gmv, b_gmv = A.alloc([128, 4, 2], F32, name="gmv")
        gsd, b_gsd = A.alloc([128, 4, 2], F32, name="gsd")
        rr_, b_rr = A.alloc([128, 1024], F32, name="r")

        wcast_b = [[Buf("wc%d_%d" % (l, k)) for k in range(48)] for l in range(2)]
        precast_state = {"i": 0}

        def precast_next():
            i = precast_state["i"]
            if i >= 32:
                return
            precast_state["i"] = i + 1
            l_, g_, e_ = i // 16, (i % 16) // 4, i % 4
            k_ = (i % 16) * 3
            P.dma(wgb[l_, g_, e_], moe_w_gate[l_, g_, e_], W=[wcast_b[l_][k_]], q="pool")
            P.dma(wub[l_, g_, e_], moe_w_up[l_, g_, e_], W=[wcast_b[l_][k_ + 1]], q="pool")
            P.dma(wdb[l_, g_, e_], moe_w_down[l_, g_, e_], W=[wcast_b[l_][k_ + 2]], q="pool")

        for s in range(nseq):
            if stop == "consts":
                break
            for tt in range(NT):
                par = 0
                P.dma(xs[:, par, :], x[s, tt * 128:(tt + 1) * 128, :], W=[b_xs[par]])
                for g in range(2):
                    bk_ = (2 * tt + g) % 2
                    for c in range(4):
                        kc = 4 * g + c
                        P.op("pe", (lambda par, kc, c, bk_: lambda e: e.transpose(out=pbanks[bk_][:, c * 128:(c + 1) * 128],
                                                                                in_=xs[:, par, kc * 128:(kc + 1) * 128], identity=ident))(par, kc, c, bk_),
                             R=[b_xs[par], b_ident], W=[pbuf[bk_]])
                    copy_rr(hT[:, 4 * g:4 * g + 4, tt * 128:(tt + 1) * 128],
                            pbanks[bk_].rearrange("p (a b) -> p a b", b=128), [pbuf[bk_]], [b_hT[tt]])
            if stop == "s1":
                break
            for n in range(4):
                tsl = slice(n * 512, (n + 1) * 512)
                hb = b_hT[4 * n:4 * n + 4]
                for g in range(3):
                    bk_ = 2 + (n * 3 + g) % 2
                    Wt, bW, c0 = (Wk, b_Wk, g * 128) if g < 2 else (Wik, b_Wik, 0)
                    for kc in range(8):
                        P.op("pe", (lambda Wt, c0, kc, bk_, tsl: lambda e: e.matmul(out=pbanks[bk_], lhsT=Wt[:, kc, c0:c0 + 128], rhs=hT[:, kc, tsl],
                                                                                 start=(kc == 0), stop=(kc == 7)))(Wt, c0, kc, bk_, tsl),
                             R=[bW] + hb, W=[pbuf[bk_]])
                    dst = kTd[:, g, tsl] if g < 2 else ikT2[:, tsl]
                    copy_rr(dst, pbanks[bk_], [pbuf[bk_]], [b_kTd if g < 2 else b_ikT2])
            for g4 in range(4):
                bk_ = 2 + g4 % 2
                for c in range(4):
                    tt = 4 * g4 + c
                    for kc in range(8):
                        P.op("pe", (lambda tt, c, kc, bk_: lambda e: e.matmul(out=pbanks[bk_][:, c * 128:(c + 1) * 128], lhsT=hT[:, kc, tt * 128:(tt + 1) * 128],
                                                                           rhs=Wv[:, kc, :], start=(kc == 0), stop=(kc == 7)))(tt, c, kc, bk_),
                             R=[b_Wv, b_hT[tt]], W=[pbuf[bk_]])
                copy_rr(vtok[:, 4 * g4:4 * g4 + 4, :], pbanks[bk_].rearrange("p (a b) -> p a b", b=128), [pbuf[bk_]], [b_vtok])
            P.op("pool", lambda e: e.memset(Sf, 0.0), W=[b_Sf])
            P.op("pool", lambda e: e.memset(Sb, 0.0), W=[b_Sb])

            if stop == "s2":
                break
            def pre(qt):
                t0 = qt * 128
                S = (qt + 1) * 128
                tq = slice(t0, t0 + 128)
                bh = b_hT[qt]
                nch = (S + 511) // 512
                qTt, b_qTt = qTt2[:, qt % 2], b_qTt2[qt % 2]
                selm1, b_selm1 = selm2[:, qt % 2, :], b_selm2[qt % 2]
                for c in range(4):
                    for kc in range(8):
                        P.op("pe", (lambda c, kc, tq: lambda e: e.matmul(out=pbanks[0][:, c * 128:(c + 1) * 128], lhsT=Wq[:, kc, c * 128:(c + 1) * 128],
                                                                      rhs=hT[:, kc, tq], start=(kc == 0), stop=(kc == 7)))(c, kc, tq),
                             R=[b_Wq, bh], W=[pbuf[0]])
                P.op("act", lambda e: e.activation(out=qTt, in_=pbanks[0].rearrange("p (a b) -> p a b", b=128), func=AF.Copy, scale=0.125),
                     R=[pbuf[0]], W=[b_qTt])
                for c in range(2):
                    for kc in range(8):
                        P.op("pe", (lambda c, kc, tq: lambda e: e.matmul(out=pbanks[1][:, c * 128:(c + 1) * 128], lhsT=Wiq[:, kc, c * 128:(c + 1) * 128],
                                                                      rhs=hT[:, kc, tq], start=(kc == 0), stop=(kc == 7)))(c, kc, tq),
                             R=[b_Wiq, bh], W=[pbuf[1]])
                for kc in range(8):
                    P.op("pe", (lambda kc, tq: lambda e: e.matmul(out=pbanks[1][:, 256:260], lhsT=hT[:, kc, tq], rhs=Wiw[:, kc, :],
                                                               start=(kc == 0), stop=(kc == 7)))(kc, tq), R=[b_Wiw, bh], W=[pbuf[1]])
                P.op("act", lambda e: e.activation(out=iqTt, in_=pbanks[1][:, 0:256].rearrange("p (a b) -> p a b", b=128), func=AF.Copy, scale=0.125),
                     R=[pbuf[1]], W=[b_iqTt])
                P.op("act", lambda e: e.activation(out=iwt, in_=pbanks[1][:, 256:260], func=AF.Copy, scale=0.5), R=[pbuf[1]], W=[b_iwt])
                nch = (S + 511) // 512
                k_ = 0
                for j in range(nch):
                    w_ = min(512, S - j * 512)
                    cs = slice(j * 512, j * 512 + w_)
                    for hi in range(4):
                        c, base = hi // 2, (hi % 2) * 64
                        bk_ = 2 + hi % 2
                        par = k_ % 2
                        k_ += 1
                        P.op("pe", (lambda c, base, bk_, w_, cs: lambda e: e.matmul(out=pbanks[bk_][:, 0:w_], lhsT=iqTt[base:base + 64, c, :],
                                                                                 rhs=ikT2[base:base + 64, cs], start=True, stop=True))(c, base, bk_, w_, cs),
                             R=[b_iqTt, b_ikT2], W=[pbuf[bk_]])
                        P.op("act", (lambda par, bk_, w_: lambda e: e.activation(out=rt[:, par, 0:w_], in_=pbanks[bk_][:, 0:w_], func=AF.Relu))(par, bk_, w_),
                             R=[pbuf[bk_]], W=[b_rt[par]])
                        if hi == 0:
                            P.op("dve", (lambda par, w_, cs: lambda e: e.tensor_scalar(out=sc[:, cs], in0=rt[:, par, 0:w_], scalar1=iwt[:, 0:1], scalar2=None,
                                                                                    op0=ALU.mult))(par, w_, cs), R=[b_rt[par], b_iwt], W=[b_sc])
                        else:
                            P.op("dve", (lambda par, w_, cs, hi: lambda e: e.scalar_tensor_tensor(out=sc[:, cs], in0=rt[:, par, 0:w_], scalar=iwt[:, hi:hi + 1],
                                                                                               in1=sc[:, cs], op0=ALU.mult, op1=ALU.add))(par, w_, cs, hi),
                                 R=[b_rt[par], b_iwt, b_sc], W=[b_sc])
                P.op("pool", (lambda S: lambda e: e.affine_select(out=sc[:, S - 128:S], in_=sc[:, S - 128:S], pattern=[[-1, 128]], compare_op=ALU.is_ge,
                                                                  fill=NEG, base=0, channel_multiplier=1))(S), R=[b_sc], W=[b_sc])
            def topk(qt):
                t0 = qt * 128
                S = (qt + 1) * 128
                tq = slice(t0, t0 + 128)
                bh = b_hT[qt]
                nch = (S + 511) // 512
                qTt, b_qTt = qTt2[:, qt % 2], b_qTt2[qt % 2]
                selm1, b_selm1 = selm2[:, qt % 2, :], b_selm2[qt % 2]
                if qt >= 2 and TOPK_BISECT:
                    rtb = rt.rearrange("p a b -> p (a b)").bitcast(BF16)
                    Wrt = [b_rt[0], b_rt[1]]
                    T = lambda k: tkb[:, k:k + 1]
                    P.op("dve", (lambda S: lambda e: e.tensor_reduce(out=T(0), in_=sc[:, 0:S], axis=AX.X, op=ALU.max))(S), R=[b_sc], W=[b_tkb])
                    P.op("dve", (lambda S: lambda e: e.tensor_reduce(out=T(1), in_=sc[:, 0:S - 128], axis=AX.X, op=ALU.min))(S), R=[b_sc], W=[b_tkb])
                    P.op("dve", lambda e: e.tensor_scalar(out=T(2), in0=T(1), scalar1=-1.0, scalar2=None, op0=ALU.add), R=[b_tkb], W=[b_tkb])
                    P.op("dve", lambda e: e.tensor_tensor(out=T(3), in0=T(0), in1=T(2), op=ALU.subtract), R=[b_tkb], W=[b_tkb])
                    P.op("dve", lambda e: e.tensor_scalar(out=Wbis, in0=POW2, scalar1=T(3), scalar2=None, op0=ALU.mult), R=[b_tkb, b_POW2], W=[b_Wbis])
                    for k in range(MB):
                        P.op("dve", (lambda k: lambda e: e.tensor_tensor(out=T(4), in0=T(2), in1=Wbis[:, k:k + 1], op=ALU.add))(k), R=[b_tkb, b_Wbis], W=[b_tkb])
                        P.op("dve", (lambda S: lambda e: e.tensor_scalar(out=rtb[:, 0:S], in0=sc[:, 0:S], scalar1=T(4), scalar2=None, op0=ALU.is_gt, op1=ALU.add,
                                                                         accum_out=T(5)))(S), R=[b_sc, b_tkb], W=Wrt + [b_tkb])
                        P.op("dve", lambda e: e.tensor_scalar(out=T(6), in0=T(5), scalar1=255.5, scalar2=None, op0=ALU.is_ge), R=[b_tkb], W=[b_tkb])
                        P.op("dve", (lambda k: lambda e: e.scalar_tensor_tensor(out=T(2), in0=T(6), scalar=Wbis[:, k:k + 1], in1=T(2), op0=ALU.mult, op1=ALU.add))(k),
                             R=[b_tkb, b_Wbis], W=[b_tkb])
                    P.op("dve", lambda e: e.tensor_tensor(out=T(7), in0=T(2), in1=Wbis[:, MB - 1:MB], op=ALU.add), R=[b_tkb, b_Wbis], W=[b_tkb])
                    P.op("dve", (lambda S: lambda e: e.tensor_scalar(out=selm1[:, 0:S], in0=sc[:, 0:S], scalar1=T(7), scalar2=None, op0=ALU.is_gt, op1=ALU.add,
                                                                     accum_out=T(8)))(S), R=[b_sc, b_tkb], W=[b_selm1, b_tkb])
                    P.op("dve", (lambda S: lambda e: e.tensor_scalar(out=rtb[:, 0:S], in0=sc[:, 0:S], scalar1=T(2), scalar2=None, op0=ALU.is_gt))(S),
                         R=[b_sc, b_tkb], W=Wrt)
                    P.op("dve", (lambda S: lambda e: e.tensor_tensor(out=rtb[:, 0:S], in0=rtb[:, 0:S], in1=selm1[:, 0:S], op=ALU.subtract))(S), R=Wrt + [b_selm1], W=Wrt)
                    P.op("dve", lambda e: e.tensor_scalar(out=T(9), in0=T(8), scalar1=-1.0, scalar2=256.0, op0=ALU.mult, op1=ALU.add), R=[b_tkb], W=[b_tkb])
                    P.op("dve", (lambda S: lambda e: e.tensor_tensor_scan(out=sc[:, 0:S], data0=rtb[:, 0:S], data1=rtb[:, 0:S], initial=0.0, op0=ALU.add, op1=ALU.bypass))(S),
                         R=Wrt, W=[b_sc])
                    P.op("dve", (lambda S: lambda e: e.scalar_tensor_tensor(out=rtb[:, 0:S], in0=sc[:, 0:S], scalar=T(9), in1=rtb[:, 0:S], op0=ALU.is_le, op1=ALU.mult))(S),
                         R=[b_sc, b_tkb] + Wrt, W=Wrt)
                    P.op("dve", (lambda S: lambda e: e.scalar_tensor_tensor(out=selm1[:, 0:S], in0=rtb[:, 0:S], scalar=-1.0, in1=selm1[:, 0:S], op0=ALU.add, op1=ALU.add))(S),
                         R=Wrt + [b_selm1], W=[b_selm1])
                elif qt >= 2:
                    for it in range(32):
                        P.op("dve", (lambda S: lambda e: e.max(out=m8, in_=sc[:, 0:S]))(S), R=[b_sc], W=[b_m8])
                        P.op("dve", (lambda S: lambda e: e.match_replace(out=sc[:, 0:S], in_to_replace=m8, in_values=sc[:, 0:S], imm_value=-3e38))(S),
                             R=[b_sc, b_m8], W=[b_sc])
                    P.op("dve", (lambda S: lambda e: e.tensor_scalar(out=selm1[:, 0:S], in0=sc[:, 0:S], scalar1=-1e37, scalar2=1.0,
                                                                     op0=ALU.is_le, op1=ALU.subtract))(S), R=[b_sc], W=[b_selm1])
                else:
                    P.op("dve", (lambda S: lambda e: e.tensor_scalar(out=selm1[:, 0:S], in0=sc[:, 0:S], scalar1=-1e29, scalar2=1.0,
                                                                     op0=ALU.is_ge, op1=ALU.subtract))(S), R=[b_sc], W=[b_selm1])
            def rest(qt):
                t0 = qt * 128
                S = (qt + 1) * 128
                tq = slice(t0, t0 + 128)
                bh = b_hT[qt]
                nch = (S + 511) // 512
                qTt, b_qTt = qTt2[:, qt % 2], b_qTt2[qt % 2]
                selm1, b_selm1 = selm2[:, qt % 2, :], b_selm2[qt % 2]
                if os.environ.get("PRECAST", "1") == "1":
                    precast_next()
                P.dma(xs[:, 0, :], x[s, t0:t0 + 128, :], W=[b_xs[0]])
                def hbufs(h):
                    hp = h % 2
                    return (lg2[:, hp, :], b_lg2[hp], pp2[:, hp, :], b_pp2[hp], pT2[:, 0, :], b_pT2[hp], sm2[:, hp, :], b_sm2[hp])

                def stageA(h):
                    c, base, g = h // 2, (h % 2) * 64, h // 4
                    hp = h % 2
                    lg, b_lg, pp, b_pp, pT, b_pT, sm, b_sm = hbufs(h)
                    for j in range(nch):
                        w_ = min(512, S - j * 512)
                        cs = slice(j * 512, j * 512 + w_)
                        bk_ = (2 if j % 2 == 0 else 0) + (h % 2)
                        P.op("pe", (lambda c, base, g, bk_, w_, cs: lambda e: e.matmul(out=pbanks[bk_][:, 0:w_], lhsT=qTt[base:base + 64, c, :],
                                                                                    rhs=kTd[base:base + 64, g, cs], start=True, stop=True))(c, base, g, bk_, w_, cs),
                             R=[b_qTt, b_kTd], W=[pbuf[bk_]])
                        P.op("dve", (lambda bk_, w_, cs, lg=lg: lambda e: e.scalar_tensor_tensor(out=lg[:, cs], in0=selm1[:, cs], scalar=1e30, in1=pbanks[bk_][:, 0:w_],
                                                                                       op0=ALU.mult, op1=ALU.add))(bk_, w_, cs),
                             R=[b_selm1, pbuf[bk_]], W=[b_lg])
                    if qt == 0:
                        P.op("pool", (lambda h, lg=lg: lambda e: e.tensor_tensor(out=lg[:, 0:128], in0=lg[:, 0:128], in1=Bp[:, h, 128:256], op=ALU.add))(h),
                             R=[b_lg, b_Bp], W=[b_lg])
                    else:
                        P.op("pool", (lambda h, S, lg=lg: lambda e: e.tensor_tensor(out=lg[:, S - 256:S], in0=lg[:, S - 256:S], in1=Bp[:, h, :], op=ALU.add))(h, S),
                             R=[b_lg, b_Bp], W=[b_lg])
                    P.op("dve", (lambda S, lg=lg, sm=sm: lambda e: e.tensor_reduce(out=sm[:, 0:1], in_=lg[:, 0:S], axis=AX.X, op=ALU.max, negate=True))(S),
                         R=[b_lg], W=[b_sm])
                    P.op("act", (lambda S, lg=lg, sm=sm, pp=pp: lambda e: e.activation(out=pp[:, 0:S], in_=lg[:, 0:S], func=AF.Exp, bias=sm[:, 0:1], scale=1.0,
                                                                  accum_out=sm[:, 1:2]))(S), R=[b_lg, b_sm], W=[b_pp, b_sm])
                    P.op("dve", (lambda sm=sm: lambda e: e.reciprocal(out=sm[:, 2:3], in_=sm[:, 1:2]))(), R=[b_sm], W=[b_sm])
                def stageB(h):
                    c, base, g = h // 2, (h % 2) * 64, h // 4
                    hp = h % 2
                    lg, b_lg, pp, b_pp, pT, b_pT, sm, b_sm = hbufs(h)
                    nb = qt + 1
                    for g8 in range((nb + 7) // 8):
                        bk_ = 4 + hp
                        n8 = min(8, nb - g8 * 8)
                        for u in range(n8):
                            kb = g8 * 8 + u
                            P.op("pe", (lambda bk_, u, kb, pp=pp: lambda e: e.transpose(out=pbf(bk_)[:, u * 128:(u + 1) * 128], in_=pp[:, kb * 128:(kb + 1) * 128],
                                                                              identity=identb))(bk_, u, kb), R=[b_pp, b_identb], W=[pbuf[bk_]])
                        copy_rr(pT[:, g8 * 1024:g8 * 1024 + n8 * 128], pbf(bk_)[:, 0:n8 * 128], [pbuf[bk_]], [b_pT])
                    for kb in range(nb):
                        P.op("pe", (lambda h, g, kb, nb, pT=pT: lambda e: e.matmul(out=pbanks[6][:, h * 64:(h + 1) * 64], lhsT=pT[:, kb * 128:(kb + 1) * 128],
                                                                         rhs=vtok[:, kb, g * 64:(g + 1) * 64], start=(h == 0 and kb == 0), stop=(kb == nb - 1),
                                                                         skip_group_check=True))(h, g, kb, nb),
                             R=[b_pT, b_vtok], W=[pbuf[6]])
                    P.op("act", (lambda h, sm=sm: lambda e: e.activation(out=oa[:, h * 64:(h + 1) * 64], in_=pbanks[6][:, h * 64:(h + 1) * 64], func=AF.Copy,
                                                                  scale=sm[:, 2:3]))(h), R=[pbuf[6], b_sm], W=[b_oa])

                stageA(0)
                for h in range(8):
                    if h + 1 < 8:
                        stageA(h + 1)
                    stageB(h)

                if stop == "dsa":
                    return
                def ck(k):
                    if gstop == k:
                        P.mute = True
                for c in range(2):
                    for kc in range(8):
                        P.op("pe", (lambda c, kc, tq: lambda e: e.matmul(out=pbanks[0][:, c * 128:(c + 1) * 128], lhsT=Wbq[:, kc, c * 128:(c + 1) * 128],
                                                                      rhs=hT[:, kc, tq], start=(kc == 0), stop=(kc == 7)))(c, kc, tq), R=[b_Wbq, bh], W=[pbuf[0]])
                for c in range(2):
                    for kc in range(8):
                        P.op("pe", (lambda c, kc, tq: lambda e: e.matmul(out=pbanks[0][:, 256 + c * 128:256 + (c + 1) * 128], lhsT=Wbk[:, kc, c * 128:(c + 1) * 128],
                                                                      rhs=hT[:, kc, tq], start=(kc == 0), stop=(kc == 7)))(c, kc, tq), R=[b_Wbk, bh], W=[pbuf[0]])
                for kc in range(8):
                    P.op("pe", (lambda kc, tq: lambda e: e.matmul(out=pbanks[1][:, 0:256], lhsT=hT[:, kc, tq], rhs=Wbk[:, kc, :],
                                                               start=(kc == 0), stop=(kc == 7)))(kc, tq), R=[b_Wbk, bh], W=[pbuf[1]])
                for kc in range(8):
                    P.op("pe", (lambda kc, tq: lambda e: e.matmul(out=pbanks[1][0:16, 256:384], lhsT=Wbg[:, kc, :], rhs=hT[:, kc, tq],
                                                               start=(kc == 0), stop=(kc == 7)))(kc, tq), R=[b_Wbg, bh], W=[pbuf[1]])
                for kc in range(8):
                    P.op("pe", (lambda kc, tq: lambda e: e.matmul(out=pbanks[2], lhsT=hT[:, kc, tq], rhs=Wbv[:, kc, :],
                                                               start=(kc == 0), stop=(kc == 7)))(kc, tq), R=[b_Wbv, bh], W=[pbuf[2]])
                for kc in range(8):
                    P.op("pe", (lambda kc, tq: lambda e: e.matmul(out=pbanks[3], lhsT=hT[:, kc, tq], rhs=Wbr[:, kc, :],
                                                               start=(kc == 0), stop=(kc == 7)))(kc, tq), R=[b_Wbr, bh], W=[pbuf[3]])
                ck(1)
                P.op("act", lambda e: e.copy(out=bg1[0:16, :], in_=pbanks[1][0:16, 256:384]), R=[pbuf[1]], W=[b_bg1])
                P.op("act", lambda e: e.copy(out=vt, in_=pbanks[2]), R=[pbuf[2]], W=[b_vt])
                P.op("act", lambda e: e.activation(out=sbr, in_=pbanks[3], func=AF.Silu), R=[pbuf[3]], W=[b_sbr])
                ck(2)
                P.op("pe", lambda e: e.matmul(out=pbanks[4][:, 0:256], lhsT=bg1[0:17, :], rhs=G2[0:17, :], start=True, stop=True),
                     R=[b_bg1, b_G2], W=[pbuf[4]])
                P.op("act", lambda e: e.activation(out=lap, in_=pbanks[4][:, 0:256], func=AF.Exp, scale=-1.0), R=[pbuf[4]], W=[b_lap])
                P.op("act", lambda e: e.activation(out=lap, in_=lap, func=AF.Ln, bias=1.0, scale=1.0), R=[b_lap], W=[b_lap])
                ck(3)
                for c in range(2):
                    P.op("pe", (lambda c: lambda e: e.matmul(out=pbanks[5][:, c * 128:(c + 1) * 128], lhsT=lap[:, c * 128:(c + 1) * 128], rhs=TRI,
                                                             start=True, stop=True))(c), R=[b_lap, b_TRI], W=[pbuf[5]])
                P.op("pe", lambda e: e.matmul(out=pbanks[5][:, 256:512], lhsT=TRI2, rhs=lap, start=True, stop=True), R=[b_lap, b_TRI2], W=[pbuf[5]])
                ck(4)
                P.op("act", lambda e: e.activation(out=EcT, in_=pbanks[5][:, 0:256].rearrange("p (a b) -> p a b", b=128), func=AF.Exp), R=[pbuf[5]], W=[b_EcT])
                P.op("act", lambda e: e.activation(out=EnT, in_=pbanks[5][:, 0:256].rearrange("p (a b) -> p a b", b=128), func=AF.Exp, scale=-1.0),
                     R=[pbuf[5]], W=[b_EnT])
                P.op("act", lambda e: e.activation(out=Er, in_=pbanks[5][:, 256:512], func=AF.Exp), R=[pbuf[5]], W=[b_Er])
                P.op("dve", lambda e: e.scalar_tensor_tensor(out=qdT, in0=pbanks[0][:, 0:256].rearrange("p (a b) -> p a b", b=128), scalar=0.125, in1=EcT,
                                                             op0=ALU.mult, op1=ALU.mult), R=[pbuf[0], b_EcT], W=[b_qdT])
                P.op("dve", lambda e: e.tensor_tensor(out=kiT, in0=pbanks[0][:, 256:512].rearrange("p (a b) -> p a b", b=128), in1=EnT, op=ALU.mult),
                     R=[pbuf[0], b_EnT], W=[b_kiT])
                P.op("dve", lambda e: e.tensor_tensor(out=kend, in0=pbanks[1][:, 0:256], in1=Er, op=ALU.mult), R=[pbuf[1], b_Er], W=[b_kend])
                ck(5)
                for h in range(4):
                    c, base = h // 2, (h % 2) * 64
                    sb_ = 4 if h % 2 == 0 else 3
                    P.op("pe", (lambda h, c, base, sb_: lambda e: e.matmul(out=pbanks[sb_][:, c * 128:(c + 1) * 128], lhsT=kiT[base:base + 64, c, :],
                                                                          rhs=qdT[base:base + 64, c, :], start=True, stop=True))(h, c, base, sb_),
                         R=[b_kiT, b_qdT], W=[pbuf[sb_]])
                ck(55)
                for h in range(4):
                    sb_ = 4 if h % 2 == 0 else 3
                    P.op("dve", (lambda h, sb_: lambda e: e.tensor_tensor(out=scT[:, h, :], in0=pbanks[sb_][:, (h // 2) * 128:(h // 2 + 1) * 128],
                                                                         in1=MASKT, op=ALU.mult))(h, sb_), R=[pbuf[sb_], b_MASKT], W=[b_scT])
                ck(6)
                for h in range(4):
                    ob_ = 7 if h % 2 == 0 else 6
                    P.op("pe", (lambda h, ob_: lambda e: e.matmul(out=pbanks[ob_][:, (h // 2) * 128:(h // 2 + 1) * 128], lhsT=scT[:, h, :], rhs=vt[:, h * 128:(h + 1) * 128],
                                                                  start=(h < 2), stop=False, skip_group_check=True))(h, ob_), R=[b_scT, b_vt], W=[pbuf[ob_]])
                ck(7)
                for u in range(2):
                    us = slice(u * 64, (u + 1) * 64)
                    for h in range(4):
                        c, base = h // 2, (h % 2) * 64
                        ob_ = 7 if h % 2 == 0 else 6
                        P.op("pe", (lambda h, c, base, us, ob_: lambda e: e.matmul(out=pbanks[ob_][us, c * 128:(c + 1) * 128], lhsT=qdT[base:base + 64, c, us],
                                                                                  rhs=Sb[base:base + 64, c, :], start=False, stop=True,
                                                                                  skip_group_check=True))(h, c, base, us, ob_), R=[b_qdT, b_Sb], W=[pbuf[ob_]])
                    for h in range(4):
                        c, base = h // 2, (h % 2) * 64
                        P.op("pe", (lambda h, c, base, us: lambda e: e.matmul(out=pbanks[5][base:base + 64, c * 128:(c + 1) * 128], lhsT=kend[us, h * 64:(h + 1) * 64],
                                                                             rhs=vt[us, h * 128:(h + 1) * 128], start=True, stop=True))(h, c, base, us),
                             R=[b_kend, b_vt], W=[pbuf[5]])
                    for c in range(2):
                        P.op("dve", (lambda c, u: lambda e: e.scalar_tensor_tensor(out=Sf[:, c, :], in0=Sf[:, c, :], scalar=EcT[:, c, u * 64 + 63:u * 64 + 64],
                                                                                   in1=pbanks[5][:, c * 128:(c + 1) * 128], op0=ALU.mult, op1=ALU.add))(c, u),
                             R=[b_Sf, b_EcT, pbuf[5]], W=[b_Sf])
                    P.op("act", lambda e: e.copy(out=Sb, in_=Sf), R=[b_Sf], W=[b_Sb])
                ck(8)
                for h in range(4):
                    ob_ = 7 if h % 2 == 0 else 6
                    P.op("dve", (lambda h, ob_: lambda e: e.bn_stats(out=gst[:, h, :], in_=pbanks[ob_][:, (h // 2) * 128:(h // 2 + 1) * 128]))(h, ob_), R=[pbuf[ob_]], W=[b_gst])
                for h in range(4):
                    P.op("dve", (lambda h: lambda e: e.bn_aggr(out=gmv[:, h, :], in_=gst[:, h, :]))(h), R=[b_gst], W=[b_gmv])
                P.op("act", lambda e: e.activation(out=gsd[:, :, 0], in_=gmv[:, :, 1], func=AF.Sqrt, bias=eps_t[:, 0:1], scale=1.0),
                     R=[b_gmv, b_eps], W=[b_gsd])
                P.op("dve", lambda e: e.reciprocal(out=gsd[:, :, 1], in_=gsd[:, :, 0]), R=[b_gsd], W=[b_gsd])
                for h in range(4):
                    ob_ = 7 if h % 2 == 0 else 6
                    P.op("dve", (lambda h, ob_: lambda e: e.tensor_scalar(out=on[:, h * 128:(h + 1) * 128], in0=pbanks[ob_][:, (h // 2) * 128:(h // 2 + 1) * 128],
                                                                         scalar1=gmv[:, h, 0:1], scalar2=gsd[:, h, 1:2], op0=ALU.subtract, op1=ALU.mult))(h, ob_),
                         R=[pbuf[ob_], b_gmv, b_gsd], W=[b_on])
                P.op("pool", lambda e: e.tensor_tensor(out=on, in0=on, in1=NG, op=ALU.mult), R=[b_on, b_NG], W=[b_on])
                P.op("pool", lambda e: e.tensor_tensor(out=oa[:, 512:1024], in0=on, in1=sbr, op=ALU.mult), R=[b_on, b_sbr], W=[b_oa])

                P.mute = False
                if stop == "gla":
                    return
                for kc in range(8):
                    P.op("pe", (lambda kc: lambda e: e.transpose(out=pbf(4)[:, kc * 128:(kc + 1) * 128], in_=oa[:, kc * 128:(kc + 1) * 128], identity=identb))(kc),
                         R=[b_oa, b_identb], W=[pbuf[4]])
                copy_rr(oT, pbf(4).rearrange("p (a b) -> p a b", b=128), [pbuf[4]], [b_oT])
                for hh in range(2):
                    for kc in range(8):
                        P.op("pe", (lambda hh, kc: lambda e: e.matmul(out=pbanks[2 + hh], lhsT=oT[:, kc, :], rhs=Wo[:, kc, hh * 512:(hh + 1) * 512],
                                                                    start=(kc == 0), stop=(kc == 7)))(hh, kc), R=[b_oT, b_Wo], W=[pbuf[2 + hh]])
                par = 0
                for hh in range(2):
                    P.op("dve", (lambda hh, par: lambda e: e.scalar_tensor_tensor(out=rr_[:, hh * 512:(hh + 1) * 512], in0=xs[:, par, hh * 512:(hh + 1) * 512],
                                                                               scalar=ALPHA, in1=pbanks[2 + hh], op0=ALU.mult, op1=ALU.add))(hh, par),
                         R=[b_xs[par], pbuf[2 + hh]], W=[b_rr])
                m_ln = A.mark()
                layer_norm_tile(rr_, b_rr, LG, LB, b_LG, rr_, b_rr, b_LB)
                A.release(m_ln)
                gi = s * NT + qt
                P.dma(hres[gi * 128:(gi + 1) * 128, :], rr_, R=[b_rr], W=[hres_b[gi]])

            pre(0)
            topk(0)
            for qt in range(nqt):
                if qt + 1 < nqt:
                    pre(qt + 1)
                    P.fork(2)
                    P.stream(0)
                    topk(qt + 1)
                    P.stream(1)
                    rest(qt)
                    n0, n1 = len(P.streams[0]), len(P.streams[1])
                    P.join([1, max(1, n1 // max(1, n0))])
                else:
                    rest(qt)

        A.release(m_l0)


        xs_b = Buf("xs_dram")
        ys_b = Buf("ys_dram")

        _oob_regs = {}

        def oobkw(e, mx):
            if os.environ.get("OOB", "1") != "1":
                return {}
            if mx not in _oob_regs:
                _oob_regs[mx] = e.to_reg(mx)
            return dict(bounds_check=_oob_regs[mx], oob_is_err=False)

        def moe_stage(layer, final):
            rr["mode"] = "alt"
            m_moe = A.mark()
            NG = NG_TOK
            LGf, b_LGf = A.alloc([128, 1024], F32, name="LGf")
            LBf, b_LBf = A.alloc([128, 1024], F32, name="LBf")
            P.dma(LGf, ln_ffn_g[layer].partition_broadcast(128), W=[b_LGf])
            P.dma(LBf, ln_ffn_b[layer].partition_broadcast(128), W=[b_LBf])
            Wr, b_Wr = A.alloc([128, 8, 20], F32, name="Wr")
            P.dma(Wr[:, :, 0:4], moe_r_coarse[layer].rearrange("(kc p) n -> p kc n", p=128), W=[b_Wr])
            for g in range(4):
                P.dma(Wr[:, :, 4 + 4 * g:8 + 4 * g], moe_r_fine[layer, g].rearrange("(kc p) n -> p kc n", p=128), W=[b_Wr])
            RBb, b_RBb = A.alloc([128, 20], F32, name="RBb")
            P.dma(RBb[:, 0:4], moe_rb_coarse[layer].partition_broadcast(128), W=[b_RBb])
            P.dma(RBb[:, 4:20], moe_rb_fine[layer].rearrange("a b -> (a b)").partition_broadcast(128), W=[b_RBb])
            OH, b_OH = A.alloc([128, NG, 32], BF16, name="OH")
            GW, b_GW = A.alloc([128, NG, 2], F32, name="GW")
            POSi, b_POSi = A.alloc([128, NG * 2], I32, name="POSi")
            IDXG, b_IDXG = A.alloc([128, NTILE * 2], I32, name="IDXG")
            IDXD, b_IDXD = A.alloc([128, NTILE * 4], I32, name="IDXD")
            m_r = A.mark()
            ht, b_ht = A.alloc([128, 2, 1024], F32, nbufs=2, name="ht")
            h1T, b_h1T = A.alloc([128, 8, 128], F32, name="h1T")
            LGT, b_LGT = A.alloc([128, NG, 20], F32, name="LGT")
            rmx, b_rmx = A.alloc([128, NG], F32, name="rmx")
            dg, b_dg = A.alloc([128, NG, 4], F32, name="dg")
            eg, b_eg = A.alloc([128, NG, 4], F32, name="eg")
            gwc, b_gwc = A.alloc([128, NG], F32, name="gwc")
            ohgB, b_ohgB = A.alloc([128, NG, 4], F32, name="ohgB")
            tmpB, b_tmpB = A.alloc([128, NG, 4, 4], F32, name="tmpB")
            flsB, b_flsB = A.alloc([128, NG, 4], F32, name="flsB")
            fl2B, b_fl2B = A.alloc([128, NG, 4], F32, name="fl2B")
            m1B, b_m1B = A.alloc([128, NG], F32, name="m1B")
            m2B, b_m2B = A.alloc([128, NG], F32, name="m2B")
            ohB, b_ohB = A.alloc([128, 2, NG, 4], F32, name="ohB")
            lgt, b_lgt = A.alloc([128, 20], F32, name="lgt")
            sm_, b_sm_ = A.alloc([128, 16], F32, name="rsm")
            ohg, b_ohg = A.alloc([128, 4], F32, name="ohg")
            tmp16, b_tmp16 = A.alloc([128, 4, 4], F32, name="tmp16")
            fls, b_fls = A.alloc([128, 4], F32, name="fls")
            fl2, b_fl2 = A.alloc([128, 4], F32, name="fl2")
            oh12, b_oh12 = A.alloc([128, 2, 4], F32, name="oh12")
            zt, b_zt = A.alloc([128, 4, 1024], BF16, name="zt")
            P.op("pool", lambda e: e.memset(zt, 0.0), W=[b_zt])
            for gi in range(NG):
                par = gi % 2
                if gi < NTILE:
                    P.dma(xs_dram[gi * 512:(gi + 1) * 512, :].rearrange("(p a) d -> p a d", a=4), zt, R=[b_zt], W=[xs_b])
                P.dma(ht[:, par, :], hres[gi * 128:(gi + 1) * 128, :], R=[hres_b[gi]], W=[b_ht[par]])
                for g in range(2):
                    bk_ = g
                    for c in range(4):
                        kc = 4 * g + c
                        P.op("pe", (lambda par, kc, c, bk_: lambda e: e.transpose(out=pbanks[bk_][:, c * 128:(c + 1) * 128],
                                                                                in_=ht[:, par, kc * 128:(kc + 1) * 128], identity=ident))(par, kc, c, bk_),
                             R=[b_ht[par], b_ident], W=[pbuf[bk_]])
                    copy_rr(h1T[:, 4 * g:4 * g + 4, :], pbanks[bk_].rearrange("p (a b) -> p a b", b=128), [pbuf[bk_]], [b_h1T])
                for kc in range(8):
                    P.op("pe", (lambda kc: lambda e: e.matmul(out=pbanks[2][:, 0:20], lhsT=h1T[:, kc, :], rhs=Wr[:, kc, :], start=(kc == 0), stop=(kc == 7)))(kc),
                         R=[b_h1T, b_Wr], W=[pbuf[2]])
                P.op("dve", (lambda gi: lambda e: e.tensor_tensor(out=LGT[:, gi, :], in0=pbanks[2][:, 0:20], in1=RBb, op=ALU.add))(gi), R=[pbuf[2], b_RBb], W=[b_LGT])
            B3 = lambda ap2, n: ap2.unsqueeze(2).to_broadcast([128, NG, n])
            gl = LGT[:, :, 0:4]
            P.op("dve", lambda e: e.tensor_reduce(out=rmx, in_=gl, axis=AX.X, op=ALU.max), R=[b_LGT], W=[b_rmx])
            P.op("dve", lambda e: e.tensor_tensor(out=dg, in0=gl, in1=B3(rmx, 4), op=ALU.subtract), R=[b_LGT, b_rmx], W=[b_dg])
            P.op("act", lambda e: e.activation(out=eg, in_=dg, func=AF.Exp), R=[b_dg], W=[b_eg])
            P.op("dve", lambda e: e.tensor_reduce(out=gwc, in_=eg, axis=AX.X, op=ALU.add), R=[b_eg], W=[b_gwc])
            P.op("dve", lambda e: e.reciprocal(out=gwc, in_=gwc), R=[b_gwc], W=[b_gwc])
            P.op("dve", lambda e: e.tensor_scalar(out=ohgB, in0=dg, scalar1=0.0, scalar2=None, op0=ALU.is_ge), R=[b_dg], W=[b_ohgB])
            P.op("dve", lambda e: e.tensor_tensor(out=tmpB, in0=LGT[:, :, 4:20].rearrange("p t (g e) -> p t g e", e=4),
                                                  in1=ohgB.unsqueeze(3).to_broadcast([128, NG, 4, 4]), op=ALU.mult), R=[b_LGT, b_ohgB], W=[b_tmpB])
            P.op("dve", lambda e: e.tensor_reduce(out=flsB, in_=tmpB.rearrange("p t g e -> p t e g"), axis=AX.X, op=ALU.add), R=[b_tmpB], W=[b_flsB])
            P.op("dve", lambda e: e.tensor_reduce(out=m1B, in_=flsB, axis=AX.X, op=ALU.max), R=[b_flsB], W=[b_m1B])
            P.op("dve", lambda e: e.tensor_tensor(out=ohB[:, 0], in0=flsB, in1=B3(m1B, 4), op=ALU.is_ge), R=[b_flsB, b_m1B], W=[b_ohB])
            P.op("dve", lambda e: e.scalar_tensor_tensor(out=fl2B, in0=ohB[:, 0], scalar=NEG, in1=flsB, op0=ALU.mult, op1=ALU.add), R=[b_ohB, b_flsB], W=[b_fl2B])
            P.op("dve", lambda e: e.tensor_reduce(out=m2B, in_=fl2B, axis=AX.X, op=ALU.max), R=[b_fl2B], W=[b_m2B])
            P.op("dve", lambda e: e.tensor_tensor(out=ohB[:, 1], in0=fl2B, in1=B3(m2B, 4), op=ALU.is_ge), R=[b_fl2B, b_m2B], W=[b_ohB])
            P.op("dve", lambda e: e.tensor_tensor(out=m2B, in0=m2B, in1=m1B, op=ALU.subtract), R=[b_m2B, b_m1B], W=[b_m2B])
            P.op("act", lambda e: e.activation(out=m2B, in_=m2B, func=AF.Exp), R=[b_m2B], W=[b_m2B])
            P.op("dve", lambda e: e.tensor_scalar(out=m1B, in0=m2B, scalar1=1.0, scalar2=None, op0=ALU.add), R=[b_m2B], W=[b_m1B])
            P.op("dve", lambda e: e.reciprocal(out=m1B, in_=m1B), R=[b_m1B], W=[b_m1B])
            P.op("dve", lambda e: e.tensor_tensor(out=GW[:, :, 0], in0=m1B, in1=gwc, op=ALU.mult), R=[b_m1B, b_gwc], W=[b_GW])
            P.op("dve", lambda e: e.tensor_tensor(out=m2B, in0=m2B, in1=m1B, op=ALU.mult), R=[b_m2B, b_m1B], W=[b_m2B])
            P.op("dve", lambda e: e.tensor_tensor(out=GW[:, :, 1], in0=m2B, in1=gwc, op=ALU.mult), R=[b_m2B, b_gwc], W=[b_GW])
            for k in range(2):
                P.op("dve", (lambda k: lambda e: e.tensor_tensor(out=OH[:, :, k * 16:(k + 1) * 16].rearrange("p t (g e) -> p t g e", e=4),
                                                                in0=ohgB.unsqueeze(3).to_broadcast([128, NG, 4, 4]),
                                                                in1=ohB[:, k].unsqueeze(2).to_broadcast([128, NG, 4, 4]), op=ALU.mult))(k),
                     R=[b_ohgB, b_ohB], W=[b_OH])
            STRI, b_STRI = A.alloc([128, 128], BF16, name="STRI")
            ONESM, b_ONESM = A.alloc([128, 128], BF16, name="ONESM")
            P.op("pool", lambda e: e.memset(ONESM, 1.0), W=[b_ONESM])
            P.op("pool", lambda e: e.memset(STRI, 1.0), W=[b_STRI])
            P.op("pool", lambda e: e.affine_select(out=STRI, in_=STRI, pattern=[[1, 128]], compare_op=ALU.is_ge, fill=0.0, base=-1,
                                                   channel_multiplier=-1), R=[b_STRI], W=[b_STRI])
            PC, b_PC = A.alloc([128, NG, 64], F32, name="PC")
            for gi in range(NG):
                bk_ = gi % 2
                P.op("pe", (lambda gi, bk_: lambda e: e.matmul(out=pbanks[bk_][:, 0:32], lhsT=STRI, rhs=OH[:, gi, :], start=True, stop=True))(gi, bk_),
                     R=[b_STRI, b_OH], W=[pbuf[bk_]])
                P.op("pe", (lambda gi, bk_: lambda e: e.matmul(out=pbanks[bk_][:, 32:64], lhsT=ONESM, rhs=OH[:, gi, :], start=True, stop=True))(gi, bk_),
                     R=[b_ONESM, b_OH], W=[pbuf[bk_]])
                copy_rr(PC[:, gi, :], pbanks[bk_][:, 0:64], [pbuf[bk_]], [b_PC])
            BASE, b_BASE = A.alloc([128, NG * 2, 16], F32, name="BASE")
            run, b_run = A.alloc([128, 16], F32, name="run")
            P.op("pool", lambda e: e.memset(run, 0.0), W=[b_run])
            for gi in range(NG):
                for k in range(2):
                    P.op("dve", (lambda gi, k: lambda e: e.tensor_copy(out=BASE[:, gi * 2 + k, :], in_=run))(gi, k), R=[b_run], W=[b_BASE])
                    P.op("dve", (lambda gi, k: lambda e: e.tensor_tensor(out=run, in0=run, in1=PC[:, gi, 32 + k * 16:48 + k * 16], op=ALU.add))(gi, k),
                         R=[b_run, b_PC], W=[b_run])
            npad, b_npad = A.alloc([128, 16], F32, name="npad")
            cum, b_cum = A.alloc([128, 16], F32, name="cum")
            ones16, b_ones16 = A.alloc([128, 16], F32, name="ones16")
            offs, b_offs = A.alloc([128, 16], F32, name="offs")
            P.op("pool", lambda e: e.memset(ones16, 1.0), W=[b_ones16])
            P.op("dve", lambda e: e.tensor_scalar(out=npad, in0=run, scalar1=0.0, scalar2=512.0, op0=ALU.is_gt, op1=ALU.mult), R=[b_run], W=[b_npad])
            for m_ in range(1, 8):
                P.op("dve", (lambda m_: lambda e: e.tensor_scalar(out=cum, in0=run, scalar1=512.0 * m_, scalar2=512.0, op0=ALU.is_gt, op1=ALU.mult))(m_),
                     R=[b_run], W=[b_cum])
                P.op("dve", lambda e: e.tensor_tensor(out=npad, in0=npad, in1=cum, op=ALU.add), R=[b_npad, b_cum], W=[b_npad])
            P.op("dve", lambda e: e.tensor_tensor_scan(out=cum, data0=ones16, data1=npad, initial=0.0, op0=ALU.mult, op1=ALU.add),
                 R=[b_ones16, b_npad], W=[b_cum])
            P.op("dve", lambda e: e.tensor_tensor(out=offs, in0=cum, in1=npad, op=ALU.subtract), R=[b_cum, b_npad], W=[b_offs])
            P.op("dve", lambda e: e.tensor_tensor(out=BASE, in0=BASE, in1=offs.unsqueeze(1).to_broadcast([128, NG * 2, 16]), op=ALU.add),
                 R=[b_BASE, b_offs], W=[b_BASE])
            T1, b_T1 = A.alloc([128, NG, 32], F32, name="T1")
            P.op("dve", lambda e: e.tensor_tensor(out=T1, in0=PC[:, :, 0:32], in1=BASE.rearrange("p (a k) e -> p a (k e)", k=2), op=ALU.add),
                 R=[b_PC, b_BASE], W=[b_T1])
            P.op("dve", lambda e: e.tensor_tensor(out=T1, in0=T1, in1=OH, op=ALU.mult), R=[b_T1, b_OH], W=[b_T1])
            POSf, b_POSf = A.alloc([128, NG * 2], F32, name="POSf")
            P.op("dve", lambda e: e.tensor_reduce(out=POSf, in_=T1.rearrange("p a (k e) -> p (a k) e", k=2), axis=AX.X, op=ALU.add), R=[b_T1], W=[b_POSf])
            P.op("dve", lambda e: e.tensor_copy(out=POSi, in_=POSf), R=[b_POSf], W=[b_POSi])
            EJ, b_EJ = A.alloc([128, NTILE], F32, name="EJ")
            for j in range(NTILE):
                P.op("dve", (lambda j: lambda e: e.tensor_scalar(out=ones16, in0=cum, scalar1=float(j * 512) + 0.5, scalar2=None, op0=ALU.is_le,
                                                                 op1=ALU.add, accum_out=EJ[:, j:j + 1]))(j), R=[b_cum], W=[b_ones16, b_EJ])
            EJu, b_EJu = A.alloc([128, NTILE], F32, name="EJu")
            P.op("dve", lambda e: e.tensor_scalar(out=EJu, in0=EJ, scalar1=15.5, scalar2=1.0e7, op0=ALU.is_ge, op1=ALU.mult), R=[b_EJ], W=[b_EJu])
            P.op("dve", lambda e: e.tensor_scalar(out=EJ, in0=EJ, scalar1=15.0, scalar2=float(layer * 16), op0=ALU.min, op1=ALU.add), R=[b_EJ], W=[b_EJ])
            P2, b_P2 = A.alloc([128, 2], F32, name="P2")
            P4, b_P4 = A.alloc([128, 4], F32, name="P4")
            P.op("pool", lambda e: e.iota(out=P2, pattern=[[1, 2]], base=0, channel_multiplier=2, allow_small_or_imprecise_dtypes=True), W=[b_P2])
            P.op("pool", lambda e: e.iota(out=P4, pattern=[[128, 4]], base=0, channel_multiplier=1, allow_small_or_imprecise_dtypes=True), W=[b_P4])
            IGf, b_IGf = A.alloc([128, NTILE, 2], F32, name="IGf")
            IDf, b_IDf = A.alloc([128, NTILE, 4], F32, name="IDf")
            P.op("dve", lambda e: e.tensor_scalar(out=IGf, in0=EJ.unsqueeze(2).to_broadcast([128, NTILE, 2]), scalar1=256.0, scalar2=None, op0=ALU.mult),
                 R=[b_EJ], W=[b_IGf])
            P.op("dve", lambda e: e.tensor_tensor(out=IGf, in0=IGf, in1=P2.unsqueeze(1).to_broadcast([128, NTILE, 2]), op=ALU.add), R=[b_IGf, b_P2], W=[b_IGf])
            P.op("dve", lambda e: e.tensor_scalar(out=IDf, in0=EJ.unsqueeze(2).to_broadcast([128, NTILE, 4]), scalar1=512.0, scalar2=None, op0=ALU.mult),
                 R=[b_EJ], W=[b_IDf])
            P.op("dve", lambda e: e.tensor_tensor(out=IDf, in0=IDf, in1=P4.unsqueeze(1).to_broadcast([128, NTILE, 4]), op=ALU.add), R=[b_IDf, b_P4], W=[b_IDf])
            if os.environ.get("OOB", "1") == "1":
                P.op("dve", lambda e: e.tensor_tensor(out=IGf, in0=IGf, in1=EJu.unsqueeze(2).to_broadcast([128, NTILE, 2]), op=ALU.add), R=[b_IGf, b_EJu], W=[b_IGf])
                P.op("dve", lambda e: e.tensor_tensor(out=IDf, in0=IDf, in1=EJu.unsqueeze(2).to_broadcast([128, NTILE, 4]), op=ALU.add), R=[b_IDf, b_EJu], W=[b_IDf])
            P.op("dve", lambda e: e.tensor_copy(out=IDXG, in_=IGf.rearrange("p a b -> p (a b)")), R=[b_IGf], W=[b_IDXG])
            P.op("dve", lambda e: e.tensor_copy(out=IDXD, in_=IDf.rearrange("p a b -> p (a b)")), R=[b_IDf], W=[b_IDXD])
            if debug == "moe_route":
                P.dma(y[0, 0:128, 0:64], POSf, R=[b_POSf], W=[y_b])
                P.dma(y[0, 0:128, 64:128], GW.rearrange("p a b -> p (a b)"), R=[b_GW], W=[y_b])
                P.dma(y[0, 0:128, 128:128 + NTILE], EJ, R=[b_EJ], W=[y_b])
                P.dma(y[0, 0:128, 256:272], cum, R=[b_cum], W=[y_b])
            hb, b_hb = A.alloc([128, 2, 1024], BF16, nbufs=2, name="hb")
            for gi in range(NG):
                par = gi % 2
                P.dma(ht[:, par, :], hres[gi * 128:(gi + 1) * 128, :], R=[hres_b[gi]], W=[b_ht[par]])
                P.op("act", (lambda par: lambda e: e.copy(out=hb[:, par, :], in_=ht[:, par, :]))(par), R=[b_ht[par]], W=[b_hb[par]])
                for k in range(2):
                    P.op("pool", (lambda gi, k, par: lambda e: e.indirect_dma_start(
                        out=xs_dram[:, :], out_offset=bass.IndirectOffsetOnAxis(ap=POSi[:, gi * 2 + k:gi * 2 + k + 1], axis=0),
                        in_=hb[:, par, :], in_offset=None))(gi, k, par), R=[b_hb[par], b_POSi], W=[xs_b], dma=True)
            A.release(m_r)
            wg, b_wg = A.alloc([128, 2, 8 * 512], BF16, nbufs=2, name="wg")
            wu, b_wu = A.alloc([128, 2, 8 * 512], BF16, nbufs=2, name="wu")
            wd, b_wd = A.alloc([128, 2, 4 * 1024], BF16, nbufs=2, name="wd")
            xsb, b_xsb = A.alloc([128, 2, 4 * 1024], BF16, nbufs=2, name="xsb")
            xT2, b_xT2 = A.alloc([128, 2, 8, 512], BF16, nbufs=2, name="xT")
            hid, b_hid = A.alloc([128, 4, 512], BF16, name="hid")
            sg, b_sg = A.alloc([128, 2, 512], F32, nbufs=2, name="sg")
            ysb, b_ysb = A.alloc([128, 4, 1024], F32, name="ysb")
            PC_ = os.environ.get("PRECAST", "1") == "1"
            WGv = (wgb if PC_ else moe_w_gate).rearrange("l g e (p two r) n -> (l g e p two) (r n)", p=128, two=2)
            WUv = (wub if PC_ else moe_w_up).rearrange("l g e (p two r) n -> (l g e p two) (r n)", p=128, two=2)
            WDv = (wdb if PC_ else moe_w_down).rearrange("l g e f n -> (l g e f) n")
            wc_dep = list(wcast_b[layer]) if PC_ else []
            def mxload(j):
                par = j % 2
                P.dma(xsb[:, par, :].rearrange("p (a d) -> p a d", a=4), xs_dram[j * 512:(j + 1) * 512, :].rearrange("(a p) d -> p a d", p=128),
                      R=[xs_b], W=[b_xsb[par]])

            def mfront(j):
                par = j % 2
                xT, b_xT = xT2[:, par], b_xT2[par]
                for hf in range(2):
                    P.op("pool", (lambda j, hf, par: lambda e: e.indirect_dma_start(
                        out=wg[:, par, hf * 2048:(hf + 1) * 2048], out_offset=None, in_=WGv,
                        in_offset=bass.IndirectOffsetOnAxis(ap=IDXG[:, j * 2 + hf:j * 2 + hf + 1], axis=0), **oobkw(e, 2 * 16 * 256 - 1)))(j, hf, par),
                        R=[b_IDXG] + wc_dep, W=[b_wg[par]], dma=True)
                    P.op("pool", (lambda j, hf, par: lambda e: e.indirect_dma_start(
                        out=wu[:, par, hf * 2048:(hf + 1) * 2048], out_offset=None, in_=WUv,
                        in_offset=bass.IndirectOffsetOnAxis(ap=IDXG[:, j * 2 + hf:j * 2 + hf + 1], axis=0), **oobkw(e, 2 * 16 * 256 - 1)))(j, hf, par),
                        R=[b_IDXG] + wc_dep, W=[b_wu[par]], dma=True)
                for fc in range(4):
                    P.op("pool", (lambda j, fc, par: lambda e: e.indirect_dma_start(
                        out=wd[:, par, fc * 1024:(fc + 1) * 1024], out_offset=None, in_=WDv,
                        in_offset=bass.IndirectOffsetOnAxis(ap=IDXD[:, j * 4 + fc:j * 4 + fc + 1], axis=0), **oobkw(e, 2 * 16 * 512 - 1)))(j, fc, par),
                        R=[b_IDXD] + wc_dep, W=[b_wd[par]], dma=True)
                for st_ in range(4):
                    bk_ = st_ % 2
                    src = xsb[:, par, st_ * 1024:(st_ + 1) * 1024].rearrange("s (p k) -> s k p", k=8)
                    for kc in range(8):
                        P.op("pe", (lambda src, kc, bk_: lambda e: e.transpose(out=pbf(bk_)[:, kc * 128:(kc + 1) * 128], in_=src[:, kc, :], identity=identb))(src, kc, bk_),
                             R=[b_xsb[par], b_identb], W=[pbuf[bk_]])
                    copy_rr(xT[:, :, st_ * 128:(st_ + 1) * 128], pbf(bk_).rearrange("p (a b) -> p a b", b=128), [pbuf[bk_]], [b_xT])
            def mback(j):
                par = j % 2
                xT, b_xT = xT2[:, par], b_xT2[par]
                wgv = wg[:, par, :].rearrange("p (k n) -> p k n", n=512)
                wuv = wu[:, par, :].rearrange("p (k n) -> p k n", n=512)
                wdv = wd[:, par, :].rearrange("p (k n) -> p k n", n=1024)
                for fc in range(4):
                    bg_, bu_ = 2 + (fc % 2) * 2, 3 + (fc % 2) * 2
                    for kc in range(8):
                        P.op("pe", (lambda fc, kc, bg_, wgv: lambda e: e.matmul(out=pbanks[bg_], lhsT=wgv[:, kc, fc * 128:(fc + 1) * 128], rhs=xT[:, kc, :],
                                                                              start=(kc == 0), stop=(kc == 7)))(fc, kc, bg_, wgv), R=[b_wg[par], b_xT], W=[pbuf[bg_]])
                    for kc in range(8):
                        P.op("pe", (lambda fc, kc, bu_, wuv: lambda e: e.matmul(out=pbanks[bu_], lhsT=wuv[:, kc, fc * 128:(fc + 1) * 128], rhs=xT[:, kc, :],
                                                                              start=(kc == 0), stop=(kc == 7)))(fc, kc, bu_, wuv), R=[b_wu[par], b_xT], W=[pbuf[bu_]])
                    sp_ = fc % 2
                    P.op("act", (lambda bg_, sp_: lambda e: e.activation(out=sg[:, sp_, :], in_=pbanks[bg_], func=AF.Silu))(bg_, sp_), R=[pbuf[bg_]], W=[b_sg[sp_]])
                    P.op("dve", (lambda fc, bu_, sp_: lambda e: e.tensor_tensor(out=hid[:, fc, :], in0=pbanks[bu_], in1=sg[:, sp_, :], op=ALU.mult))(fc, bu_, sp_),
                         R=[pbuf[bu_], b_sg[sp_]], W=[b_hid])
                for st_ in range(4):
                    for hh in range(2):
                        bk_ = 6 + hh
                        for fc in range(4):
                            P.op("pe", (lambda st_, hh, fc, bk_, wdv: lambda e: e.matmul(out=pbanks[bk_], lhsT=hid[:, fc, st_ * 128:(st_ + 1) * 128],
                                                                                        rhs=wdv[:, fc, hh * 512:(hh + 1) * 512], start=(fc == 0), stop=(fc == 3)))(st_, hh, fc, bk_, wdv),
                                 R=[b_hid, b_wd[par]], W=[pbuf[bk_]])
                        copy_rr(ysb[:, st_, hh * 512:(hh + 1) * 512], pbanks[bk_], [pbuf[bk_]], [b_ysb])
                P.dma(ys_dram[j * 512:(j + 1) * 512, :].rearrange("(a p) d -> p a d", p=128), ysb, R=[b_ysb], W=[ys_b])

            mxload(0)
            mxload(1)
            mfront(0)
            for j in range(NTILE):
                if j + 2 < NTILE:
                    mxload(j + 2)
                if j + 1 < NTILE:
                    P.fork(2)
                    P.stream(0)
                    mfront(j + 1)
                    P.stream(1)
                    mback(j)
                    n0, n1 = len(P.streams[0]), len(P.streams[1])
                    P.join([max(1, n0 // n1), max(1, n1 // n0)])
                else:
                    mback(j)
            A.release(m_r)
            ht5, b_ht5 = A.alloc([128, 2, 1024], F32, nbufs=2, name="ht2")
            Y1, b_Y1 = A.alloc([128, 2, 1024], F32, nbufs=2, name="Y1")
            Y2, b_Y2 = A.alloc([128, 2, 1024], F32, nbufs=2, name="Y2")
            rr2_, b_rr2_ = A.alloc([128, 2, 1024], F32, nbufs=2, name="rr2")
            lnt = []
            for _ in range(2):
                a_, ba_ = A.alloc([128, 2, 6], F32, name="lnst")
                c_, bc_ = A.alloc([128, 2], F32, name="lnmv")
                d_, bd_ = A.alloc([128, 2], F32, name="lnsd")
                lnt.append((a_, ba_, c_, bc_, d_, bd_))
            y_bs = [Buf("y%d" % i) for i in range(NG)]
            h2, b_h2 = A.alloc([128, 2, 1024], F32, nbufs=2, name="h2")
            def r5(gi):
                par = gi % 2
                rr2, b_rr2 = rr2_[:, par, :], b_rr2_[par]
                P.dma(ht5[:, par, :], hres[gi * 128:(gi + 1) * 128, :], R=[hres_b[gi]], W=[b_ht5[par]])
                for k, (Y_, bY) in enumerate(((Y1, b_Y1), (Y2, b_Y2))):
                    P.op("pool", (lambda gi, k, par, Y_: lambda e: e.indirect_dma_start(
                        out=Y_[:, par, :], out_offset=None, in_=ys_dram[:, :],
                        in_offset=bass.IndirectOffsetOnAxis(ap=POSi[:, gi * 2 + k:gi * 2 + k + 1], axis=0)))(gi, k, par, Y_),
                        R=[ys_b, b_POSi], W=[bY[par]], dma=True)
                P.op("dve", (lambda gi, par: lambda e: e.tensor_scalar(out=rr2, in0=Y1[:, par, :], scalar1=GW[:, gi, 0:1], scalar2=None, op0=ALU.mult))(gi, par),
                     R=[b_Y1[par], b_GW], W=[b_rr2])
                P.op("dve", (lambda gi, par: lambda e: e.scalar_tensor_tensor(out=rr2, in0=Y2[:, par, :], scalar=GW[:, gi, 1:2], in1=rr2, op0=ALU.mult, op1=ALU.add))(gi, par),
                     R=[b_Y2[par], b_GW, b_rr2], W=[b_rr2])
                P.op("dve", (lambda par: lambda e: e.scalar_tensor_tensor(out=rr2, in0=ht5[:, par, :], scalar=ALPHA, in1=rr2, op0=ALU.mult, op1=ALU.add))(par),
                     R=[b_ht5[par], b_rr2], W=[b_rr2])
                layer_norm_tile(rr2, b_rr2, LGf, LBf, b_LGf, h2[:, par, :], b_h2[par], b_LBf, tmps=lnt[par])
                if final:
                    s_, qt_ = gi // NT, gi % NT
                    P.dma(y[s_, qt_ * 128:(qt_ + 1) * 128, :], h2[:, par, :], R=[b_h2[par]], W=[y_bs[gi]])
                else:
                    P.dma(hres[gi * 128:(gi + 1) * 128, :], h2[:, par, :], R=[b_h2[par]], W=[hres_b[gi]])
            P.fork(2)
            for gi in range(NG):
                P.stream(gi % 2)
                r5(gi)
            P.join([1, 1])
            A.release(m_moe)


        def s5_stage():
            rr["mode"] = "act"
            m_s5 = A.mark()
            gT_b = [Buf("gTd%d" % i) for i in range(SEQ_PER_CORE)]
            Win, b_Win = A.alloc([128, 8, 1024], BF16, name="Win")
            wdma(Win, b_Win, s5_w_in[0])
            DV, b_DV = A.alloc([128, 1024], F32, name="DV")
            P.dma(DV, s5_d[0].partition_broadcast(128), W=[b_DV])
            TA, b_TA = A.alloc([128, 4096], F32, name="TA")
            TB, b_TB = A.alloc([128, 4096], F32, name="TB")
            Pre, b_Pre = A.alloc([128, 32, 128], F32, name="Pre")
            Pim, b_Pim = A.alloc([128, 32, 128], F32, name="Pim")
            BDr, b_BDr = A.alloc([128, 8, 512], BF16, name="BDr")
            BDi, b_BDi = A.alloc([128, 8, 512], BF16, name="BDi")
            CmR, b_CmR = A.alloc([128, 1024], BF16, name="CmR")
            CmI, b_CmI = A.alloc([128, 1024], BF16, name="CmI")
            TRIc, b_TRIc = A.alloc([128, 128], BF16, name="TRIc")
            a1, b_a1 = A.alloc([128, 2, 32], F32, name="a1")
            P.op("pool", lambda e: e.memset(TRIc, 1.0), W=[b_TRIc])
            P.op("pool", lambda e: e.affine_select(out=TRIc, in_=TRIc, pattern=[[1, 128]], compare_op=ALU.is_ge, fill=0.0, base=0,
                                                   channel_multiplier=-1), R=[b_TRIc], W=[b_TRIc])
            m_p = A.mark()
            P.fork(2)
            P.stream(0)
            lr, b_lr = A.alloc([128, 32], F32, name="lr")
            li, b_li = A.alloc([128, 32], F32, name="li")
            ldt, b_ldt = A.alloc([128, 32], F32, name="ldt")
            for two in range(2):
                ps_ = slice(two * 64, (two + 1) * 64)
                P.dma(lr[ps_, :], s5_lam_re[0].rearrange("(q two) p -> two p q", two=2)[two], W=[b_lr], allow_slow_non_contiguous=True)
                P.dma(li[ps_, :], s5_lam_im[0].rearrange("(q two) p -> two p q", two=2)[two], W=[b_li], allow_slow_non_contiguous=True)
                P.dma(ldt[ps_, :], s5_log_dt[0].rearrange("(q two) -> two q", two=2)[two].partition_broadcast(64), W=[b_ldt],
                      allow_slow_non_contiguous=True)
            sc_, b_sc_ = A.alloc([128, 16, 32], F32, name="s5sc")
            V = lambda k: sc_[:, k, :]
            def dv(fn, R=(), W=()):
                P.op("dve", fn, R=[b_sc_, b_lr, b_li, b_ldt] + list(R), W=[b_sc_] + list(W))
            def ac(fn, R=(), W=()):
                P.op("act", fn, R=[b_sc_, b_lr, b_li, b_ldt] + list(R), W=[b_sc_] + list(W))
            dv(lambda e: e.tensor_scalar(out=lr, in0=lr, scalar1=-1e-4, scalar2=None, op0=ALU.min), W=[b_lr])
            ac(lambda e: e.activation(out=V(0), in_=ldt, func=AF.Exp))
            dv(lambda e: e.tensor_tensor(out=V(1), in0=lr, in1=V(0), op=ALU.mult))
            dv(lambda e: e.tensor_tensor(out=V(2), in0=li, in1=V(0), op=ALU.mult))
            dv(lambda e: e.tensor_scalar(out=V(3), in0=V(2), scalar1=1.0 / 64, scalar2=1.5, op0=ALU.mult, op1=ALU.min))
            dv(lambda e: e.tensor_scalar(out=V(3), in0=V(3), scalar1=-1.5, scalar2=None, op0=ALU.max))
            ac(lambda e: e.activation(out=V(4), in_=V(3), func=AF.Sin))
            ac(lambda e: e.activation(out=V(5), in_=V(3), func=AF.Sin, bias=halfpi[:, 0:1], scale=1.0), R=[b_hp])
            ac(lambda e: e.activation(out=V(6), in_=V(1), func=AF.Exp, scale=1.0 / 64))
            dv(lambda e: e.tensor_tensor(out=V(7), in0=V(5), in1=V(6), op=ALU.mult))
            dv(lambda e: e.tensor_tensor(out=V(8), in0=V(4), in1=V(6), op=ALU.mult))
            for _ in range(6):
                dv(lambda e: e.tensor_tensor(out=V(9), in0=V(7), in1=V(7), op=ALU.mult))
                dv(lambda e: e.tensor_tensor(out=V(10), in0=V(8), in1=V(8), op=ALU.mult))
                dv(lambda e: e.tensor_tensor(out=V(11), in0=V(7), in1=V(8), op=ALU.mult))
                dv(lambda e: e.tensor_tensor(out=V(7), in0=V(9), in1=V(10), op=ALU.subtract))
                dv(lambda e: e.tensor_scalar(out=V(8), in0=V(11), scalar1=2.0, scalar2=None, op0=ALU.mult))
            dv(lambda e: e.tensor_copy(out=a1[:, 0, :], in_=V(7)), W=[b_a1])
            dv(lambda e: e.tensor_copy(out=a1[:, 1, :], in_=V(8)), W=[b_a1])
            ac(lambda e: e.activation(out=V(9), in_=V(1), func=AF.Exp, scale=-2.0))
            dv(lambda e: e.tensor_tensor(out=V(10), in0=V(7), in1=V(9), op=ALU.mult))
            dv(lambda e: e.scalar_tensor_tensor(out=V(11), in0=V(8), scalar=-1.0, in1=V(9), op0=ALU.mult, op1=ALU.mult))
            dv(lambda e: e.tensor_tensor(out=V(12), in0=lr, in1=lr, op=ALU.mult))
            dv(lambda e: e.tensor_tensor(out=V(13), in0=li, in1=li, op=ALU.mult))
            dv(lambda e: e.tensor_tensor(out=V(12), in0=V(12), in1=V(13), op=ALU.add))
            dv(lambda e: e.reciprocal(out=V(12), in_=V(12)))
            dv(lambda e: e.tensor_scalar(out=V(13), in0=V(7), scalar1=-1.0, scalar2=None, op0=ALU.add))
            dv(lambda e: e.tensor_tensor(out=V(14), in0=V(13), in1=lr, op=ALU.mult))
            dv(lambda e: e.tensor_tensor(out=V(15), in0=V(8), in1=li, op=ALU.mult))
            dv(lambda e: e.tensor_tensor(out=V(14), in0=V(14), in1=V(15), op=ALU.add))
            dv(lambda e: e.tensor_tensor(out=V(14), in0=V(14), in1=V(12), op=ALU.mult))
            dv(lambda e: e.tensor_tensor(out=V(15), in0=V(8), in1=lr, op=ALU.mult))
            dv(lambda e: e.tensor_tensor(out=V(13), in0=V(13), in1=li, op=ALU.mult))
            dv(lambda e: e.tensor_tensor(out=V(15), in0=V(15), in1=V(13), op=ALU.subtract))
            dv(lambda e: e.tensor_tensor(out=V(15), in0=V(15), in1=V(12), op=ALU.mult))
            Qre, b_Qre = A.alloc([128, 32, 128], F32, name="Qre")
            Qim, b_Qim = A.alloc([128, 32, 128], F32, name="Qim")
            tq1, b_tq1 = A.alloc([128, 32, 64], F32, name="tq1")
            tq2, b_tq2 = A.alloc([128, 32, 64], F32, name="tq2")
            pw, b_pw = A.alloc([128, 2, 32], F32, name="pw")
            sq, b_sq = A.alloc([128, 3, 32], F32, name="sq")
            for (Tr, bTr, Ti, bTi, i0r, i0i, br_, bi_) in ((Pre, b_Pre, Pim, b_Pim, None, None, 7, 8), (Qre, b_Qre, Qim, b_Qim, 14, 15, 10, 11)):
                if i0r is None:
                    P.op("pool", (lambda Tr: lambda e: e.memset(Tr[:, :, 0:1], 1.0))(Tr), W=[bTr])
                    P.op("pool", (lambda Ti: lambda e: e.memset(Ti[:, :, 0:1], 0.0))(Ti), W=[bTi])
                else:
                    P.op("dve", (lambda Tr, i0r: lambda e: e.tensor_copy(out=Tr[:, :, 0], in_=V(i0r)))(Tr, i0r), R=[b_sc_], W=[bTr])
                    P.op("dve", (lambda Ti, i0i: lambda e: e.tensor_copy(out=Ti[:, :, 0], in_=V(i0i)))(Ti, i0i), R=[b_sc_], W=[bTi])
                P.op("dve", (lambda br_: lambda e: e.tensor_copy(out=pw[:, 0, :], in_=V(br_)))(br_), R=[b_sc_], W=[b_pw])
                P.op("dve", (lambda bi_: lambda e: e.tensor_copy(out=pw[:, 1, :], in_=V(bi_)))(bi_), R=[b_sc_], W=[b_pw])
                n_ = 1
                while n_ < 128:
                    pr = pw[:, 0, :].unsqueeze(2).to_broadcast([128, 32, n_])
                    pi_ = pw[:, 1, :].unsqueeze(2).to_broadcast([128, 32, n_])
                    Rb = [bTr, bTi, b_pw, b_tq1, b_tq2, b_sq]
                    P.op("dve", (lambda Tr, pr, n_: lambda e: e.tensor_tensor(out=tq1[:, :, 0:n_], in0=Tr[:, :, 0:n_], in1=pr, op=ALU.mult))(Tr, pr, n_), R=Rb, W=[b_tq1])
                    P.op("dve", (lambda Ti, pi_, n_: lambda e: e.tensor_tensor(out=tq2[:, :, 0:n_], in0=Ti[:, :, 0:n_], in1=pi_, op=ALU.mult))(Ti, pi_, n_), R=Rb, W=[b_tq2])
                    P.op("dve", (lambda Tr, n_: lambda e: e.tensor_tensor(out=Tr[:, :, n_:2 * n_], in0=tq1[:, :, 0:n_], in1=tq2[:, :, 0:n_], op=ALU.subtract))(Tr, n_), R=Rb, W=[bTr])
                    P.op("dve", (lambda Tr, pi_, n_: lambda e: e.tensor_tensor(out=tq1[:, :, 0:n_], in0=Tr[:, :, 0:n_], in1=pi_, op=ALU.mult))(Tr, pi_, n_), R=Rb, W=[b_tq1])
                    P.op("dve", (lambda Ti, pr, n_: lambda e: e.tensor_tensor(out=tq2[:, :, 0:n_], in0=Ti[:, :, 0:n_], in1=pr, op=ALU.mult))(Ti, pr, n_), R=Rb, W=[b_tq2])
                    P.op("dve", (lambda Ti, n_: lambda e: e.tensor_tensor(out=Ti[:, :, n_:2 * n_], in0=tq1[:, :, 0:n_], in1=tq2[:, :, 0:n_], op=ALU.add))(Ti, n_), R=Rb, W=[bTi])
                    P.op("dve", lambda e: e.tensor_tensor(out=sq[:, 0, :], in0=pw[:, 0, :], in1=pw[:, 0, :], op=ALU.mult), R=Rb, W=[b_sq])
                    P.op("dve", lambda e: e.tensor_tensor(out=sq[:, 1, :], in0=pw[:, 1, :], in1=pw[:, 1, :], op=ALU.mult), R=Rb, W=[b_sq])
                    P.op("dve", lambda e: e.tensor_tensor(out=sq[:, 2, :], in0=pw[:, 0, :], in1=pw[:, 1, :], op=ALU.mult), R=Rb, W=[b_sq])
                    P.op("dve", lambda e: e.tensor_tensor(out=pw[:, 0, :], in0=sq[:, 0, :],
                                                          in1=sq[:, 1, :], op=ALU.subtract), R=Rb, W=[b_pw])
                    P.op("dve", lambda e: e.tensor_scalar(out=pw[:, 1, :], in0=sq[:, 2, :], scalar1=2.0, scalar2=None, op0=ALU.mult),
                         R=Rb, W=[b_pw])
                    n_ *= 2
            for (Q_, bQ, T_, bT) in ((Qre, b_Qre, TA, b_TA), (Qim, b_Qim, TB, b_TB)):
                for q4 in range(8):
                    bk_ = q4 % 2
                    for c in range(4):
                        q = q4 * 4 + c
                        P.op("pe", (lambda Q_, q, c, bk_: lambda e: e.transpose(out=pbanks[bk_][:, c * 128:(c + 1) * 128], in_=Q_[:, q, :], identity=ident))(Q_, q, c, bk_),
                             R=[bQ, b_ident], W=[pbuf[bk_]])
                    copy_rr(T_[:, q4 * 512:(q4 + 1) * 512], pbanks[bk_], [pbuf[bk_]], [bT])
            P.stream(1)
            m_bc = A.mark()
            Xb, b_Xb = A.alloc([128, 4, 128], F32, nbufs=4, name="Xb")
            Xbb, b_Xbb = A.alloc([128, 4, 128], BF16, nbufs=4, name="Xbb")
            cnt_ = 0
            for (src, BD_, bBD) in ((s5_b_re, BDr, b_BDr), (s5_b_im, BDi, b_BDi)):
                for kc in range(8):
                    for pair in range(4):
                        sl = cnt_ % 4
                        cnt_ += 1
                        P.op("pool", (lambda sl: lambda e: e.memset(Xb[:, sl, :], 0.0))(sl), W=[b_Xb[sl]])
                        for two in range(2):
                            g = 8 * kc + 2 * pair + two
                            c0 = (2 * pair + two) * 16
                            P.dma(Xb[two * 64:(two + 1) * 64, sl, c0:c0 + 16], src[0, g], W=[b_Xb[sl]])
                        P.op("act", (lambda sl: lambda e: e.copy(out=Xbb[:, sl, :], in_=Xb[:, sl, :]))(sl), R=[b_Xb[sl]], W=[b_Xbb[sl]])
                        bk_ = 2 + cnt_ % 2
                        P.op("pe", (lambda sl, bk_: lambda e: e.transpose(out=pbf(bk_)[:, 0:128], in_=Xbb[:, sl, :], identity=identb))(sl, bk_),
                             R=[b_Xbb[sl], b_identb], W=[pbuf[bk_]])
                        copy_rr(BD_[:, kc, pair * 128:(pair + 1) * 128], pbf(bk_)[:, 0:128], [pbuf[bk_]], [bBD])
            for (src, Cm_, bCm, sgn) in ((s5_c_re, CmR, b_CmR, 1.0), (s5_c_im, CmI, b_CmI, -1.0)):
                for kc in range(8):
                    sl = cnt_ % 4
                    cnt_ += 1
                    P.op("pool", (lambda sl: lambda e: e.memset(Xb[:, sl, :], 0.0))(sl), W=[b_Xb[sl]])
                    for gl in range(8):
                        g = 8 * kc + gl
                        two = g % 2
                        P.dma(Xb[gl * 16:(gl + 1) * 16, sl, two * 64:(two + 1) * 64], src[0, g], W=[b_Xb[sl]])
                    P.op("act", (lambda sl: lambda e: e.copy(out=Xbb[:, sl, :], in_=Xb[:, sl, :]))(sl), R=[b_Xb[sl]], W=[b_Xbb[sl]])
                    bk_ = 2 + cnt_ % 2
                    P.op("pe", (lambda sl, bk_: lambda e: e.transpose(out=pbf(bk_)[:, 0:128], in_=Xbb[:, sl, :], identity=identb))(sl, bk_),
                         R=[b_Xbb[sl], b_identb], W=[pbuf[bk_]])
                    P.op("act", (lambda Cm_, kc, bk_, sgn: lambda e: e.activation(out=Cm_[:, kc * 128:(kc + 1) * 128], in_=pbf(bk_)[:, 0:128], func=AF.Copy, scale=sgn))(Cm_, kc, bk_, sgn),
                         R=[pbuf[bk_]], W=[bCm])
            n0, n1 = len(P.streams[0]), len(P.streams[1])
            P.join([max(1, n0 // n1), max(1, n1 // n0)])
            A.release(m_p)
            m_scan = A.mark()
            ht, b_ht = A.alloc([128, 2, 1024], F32, nbufs=2, name="s5ht")
            hTt, b_hTt = A.alloc([128, 8, 128], BF16, name="hTt")
            uTt, b_uTt = A.alloc([128, 8, 128], BF16, name="uTt")
            ud2, b_ud2 = A.alloc([128, 2, 1024], F32, nbufs=2, name="ud")
            wre2, b_wre2 = A.alloc([128, 2, 4096], BF16, nbufs=2, name="wre")
            wim2, b_wim2 = A.alloc([128, 2, 4096], BF16, nbufs=2, name="wim")
            sre, b_sre = A.alloc([128, 32, 128], BF16, name="sre")
            sim_, b_sim = A.alloc([128, 32, 128], BF16, name="sim")
            ttf, b_ttf = A.alloc([128, 4, 512], F32, nbufs=4, name="ttf")
            ttb, b_ttb = A.alloc([128, 4, 512], F32, nbufs=4, name="ttb")
            zz, b_zz = A.alloc([128, 2, 2, 4 * 128], F32, nbufs=2, name="zz")
            zz = zz.rearrange("p z r (c i) -> p z r c i", i=128)
            cre, b_cre = A.alloc([128, 2, 32], F32, name="cre")
            send, b_send = A.alloc([128, 2, 32], F32, name="send")
            ctmp, b_ctmp = A.alloc([128, 4, 32], F32, name="ctmp")
            yy, b_yy = A.alloc([128, 1024], F32, name="yy")
            gb, b_gb = A.alloc([128, 1024], BF16, name="gb")
            gTt, b_gTt = A.alloc([128, 8, 128], BF16, name="gTt")

            def front(gi):
                par = gi % 2
                ud, b_ud, wre, b_wre, wim, b_wim = ud2[:, par, :], b_ud2[par], wre2[:, par, :], b_wre2[par], wim2[:, par, :], b_wim2[par]
                for g2 in range(2):
                    for c in range(4):
                        kc = 4 * g2 + c
                        P.op("pe", (lambda par, kc, c, g2: lambda e: e.transpose(out=pbanks[g2][:, c * 128:(c + 1) * 128], in_=ht[:, par, kc * 128:(kc + 1) * 128],
                                                                               identity=ident))(par, kc, c, g2), R=[b_ht[par], b_ident], W=[pbuf[g2]])
                    copy_rr(hTt[:, 4 * g2:4 * g2 + 4, :], pbanks[g2].rearrange("p (a b) -> p a b", b=128), [pbuf[g2]], [b_hTt])
                for g2 in range(2):
                    for c in range(4):
                        fc = 4 * g2 + c
                        for kc in range(8):
                            P.op("pe", (lambda fc, c, kc, g2: lambda e: e.matmul(out=pbanks[g2][:, c * 128:(c + 1) * 128], lhsT=Win[:, kc, fc * 128:(fc + 1) * 128], rhs=hTt[:, kc, :],
                                                                                 start=(kc == 0), stop=(kc == 7)))(fc, c, kc, g2), R=[b_Win, b_hTt], W=[pbuf[g2]])
                    copy_rr(uTt[:, 4 * g2:4 * g2 + 4, :], pbanks[g2].rearrange("p (a b) -> p a b", b=128), [pbuf[g2]], [b_uTt])
                for hh in range(2):
                    for kc in range(8):
                        P.op("pe", (lambda hh, kc: lambda e: e.matmul(out=pbanks[2 + hh], lhsT=hTt[:, kc, :], rhs=Win[:, kc, hh * 512:(hh + 1) * 512],
                                                                    start=(kc == 0), stop=(kc == 7)))(hh, kc), R=[b_Win, b_hTt], W=[pbuf[2 + hh]])
                    P.op("dve", (lambda hh: lambda e: e.tensor_tensor(out=ud[:, hh * 512:(hh + 1) * 512], in0=pbanks[2 + hh], in1=DV[:, hh * 512:(hh + 1) * 512], op=ALU.mult))(hh),
                         R=[pbuf[2 + hh], b_DV], W=[b_ud])
                for kc in range(8):
                    br_, bi_ = (kc % 2) * 2, 1 + (kc % 2) * 2
                    cs = slice(kc * 512, (kc + 1) * 512)
                    P.op("pe", (lambda kc, br_: lambda e: e.matmul(out=pbanks[br_], lhsT=uTt[:, kc, :], rhs=BDr[:, kc, :], start=True, stop=True))(kc, br_),
                         R=[b_uTt, b_BDr], W=[pbuf[br_]])
                    P.op("pe", (lambda kc, bi_: lambda e: e.matmul(out=pbanks[bi_], lhsT=uTt[:, kc, :], rhs=BDi[:, kc, :], start=True, stop=True))(kc, bi_),
                         R=[b_uTt, b_BDi], W=[pbuf[bi_]])
                    for (k_, bnk, T_, bT_) in ((0, br_, TA, b_TA), (1, bi_, TB, b_TB), (2, bi_, TA, b_TA), (3, br_, TB, b_TB)):
                        P.op("dve", (lambda k_, bnk, T_, cs: lambda e: e.tensor_tensor(out=ttf[:, k_, :], in0=pbanks[bnk], in1=T_[:, cs], op=ALU.mult))(k_, bnk, T_, cs),
                             R=[pbuf[bnk], bT_], W=[b_ttf[k_]])
                    P.op("pool", (lambda cs: lambda e: e.tensor_tensor(out=wre[:, cs], in0=ttf[:, 0, :], in1=ttf[:, 1, :], op=ALU.subtract))(cs),
                         R=[b_ttf[0], b_ttf[1]], W=[b_wre])
                    P.op("pool", (lambda cs: lambda e: e.tensor_tensor(out=wim[:, cs], in0=ttf[:, 2, :], in1=ttf[:, 3, :], op=ALU.add))(cs),
                         R=[b_ttf[2], b_ttf[3]], W=[b_wim])

            def back(gi):
                par = gi % 2
                s_, qt = gi // NT, gi % NT
                ud, b_ud, wre, b_wre, wim, b_wim = ud2[:, par, :], b_ud2[par], wre2[:, par, :], b_wre2[par], wim2[:, par, :], b_wim2[par]
                if qt == 0:
                    P.op("pool", lambda e: e.memset(cre, 0.0), W=[b_cre])
                for q4 in range(8):
                    br_, bi_ = 4 + (q4 % 2) * 2, 5 + (q4 % 2) * 2
                    zp = q4 % 2
                    for c in range(4):
                        q = q4 * 4 + c
                        P.op("pe", (lambda q, c, br_: lambda e: e.matmul(out=pbanks[br_][:, c * 128:(c + 1) * 128], lhsT=wre[:, q * 128:(q + 1) * 128], rhs=TRIc,
                                                                       start=True, stop=True))(q, c, br_), R=[b_wre, b_TRIc], W=[pbuf[br_]])
                        P.op("pe", (lambda q, c, bi_: lambda e: e.matmul(out=pbanks[bi_][:, c * 128:(c + 1) * 128], lhsT=wim[:, q * 128:(q + 1) * 128], rhs=TRIc,
                                                                       start=True, stop=True))(q, c, bi_), R=[b_wim, b_TRIc], W=[pbuf[bi_]])
                    qs = slice(q4 * 4, q4 * 4 + 4)
                    P.op("dve", (lambda br_, qs, zp: lambda e: e.tensor_tensor(out=zz[:, zp, 0, :, :], in0=pbanks[br_].rearrange("p (a b) -> p a b", b=128),
                                                                          in1=cre[:, 0, qs].unsqueeze(2).to_broadcast([128, 4, 128]), op=ALU.add))(br_, qs, zp), R=[pbuf[br_], b_cre], W=[b_zz[zp]])
                    P.op("dve", (lambda bi_, qs, zp: lambda e: e.tensor_tensor(out=zz[:, zp, 1, :, :], in0=pbanks[bi_].rearrange("p (a b) -> p a b", b=128),
                                                                          in1=cre[:, 1, qs].unsqueeze(2).to_broadcast([128, 4, 128]), op=ALU.add))(bi_, qs, zp), R=[pbuf[bi_], b_cre], W=[b_zz[zp]])
                    tv = lambda k_: ttb[:, k_, :].rearrange("p (a b) -> p a b", b=128)
                    t0v, t1v, t2v, t3v = tv(0), tv(1), tv(2), tv(3)
                    zrv, ziv = zz[:, zp, 0, :, :], zz[:, zp, 1, :, :]
                    P.op("pool", (lambda qs, t0v, zrv: lambda e: e.tensor_tensor(out=t0v, in0=zrv, in1=Pre[:, qs, :], op=ALU.mult))(qs, t0v, zrv), R=[b_zz[zp], b_Pre], W=[b_ttb[0]])
                    P.op("pool", (lambda qs, t1v, ziv: lambda e: e.tensor_tensor(out=t1v, in0=ziv, in1=Pim[:, qs, :], op=ALU.mult))(qs, t1v, ziv), R=[b_zz[zp], b_Pim], W=[b_ttb[1]])
                    P.op("pool", (lambda qs, t2v, ziv: lambda e: e.tensor_tensor(out=t2v, in0=ziv, in1=Pre[:, qs, :], op=ALU.mult))(qs, t2v, ziv), R=[b_zz[zp], b_Pre], W=[b_ttb[2]])
                    P.op("dve", (lambda qs, t3v, zrv: lambda e: e.tensor_tensor(out=t3v, in0=zrv, in1=Pim[:, qs, :], op=ALU.mult))(qs, t3v, zrv), R=[b_zz[zp], b_Pim], W=[b_ttb[3]])
                    P.op("dve", (lambda qs, t0v, t1v: lambda e: e.tensor_tensor(out=sre[:, qs, :], in0=t0v, in1=t1v, op=ALU.subtract))(qs, t0v, t1v), R=[b_ttb[0], b_ttb[1]], W=[b_sre])
                    P.op("dve", (lambda qs, t0v, t1v: lambda e: e.tensor_tensor(out=send[:, 0, qs], in0=t0v[:, :, 127], in1=t1v[:, :, 127], op=ALU.subtract))(qs, t0v, t1v),
                         R=[b_ttb[0], b_ttb[1]], W=[b_send])
                    P.op("dve", (lambda qs, t2v, t3v: lambda e: e.tensor_tensor(out=sim_[:, qs, :], in0=t2v, in1=t3v, op=ALU.add))(qs, t2v, t3v), R=[b_ttb[2], b_ttb[3]], W=[b_sim])
                    P.op("dve", (lambda qs, t2v, t3v: lambda e: e.tensor_tensor(out=send[:, 1, qs], in0=t2v[:, :, 127], in1=t3v[:, :, 127], op=ALU.add))(qs, t2v, t3v),
                         R=[b_ttb[2], b_ttb[3]], W=[b_send])
                P.op("dve", lambda e: e.tensor_tensor(out=ctmp[:, 0, :], in0=send[:, 0, :], in1=a1[:, 0, :], op=ALU.mult), R=[b_send, b_a1], W=[b_ctmp])
                P.op("dve", lambda e: e.tensor_tensor(out=ctmp[:, 1, :], in0=send[:, 1, :], in1=a1[:, 1, :], op=ALU.mult), R=[b_send, b_a1], W=[b_ctmp])
                P.op("dve", lambda e: e.tensor_tensor(out=ctmp[:, 2, :], in0=send[:, 0, :], in1=a1[:, 1, :], op=ALU.mult), R=[b_send, b_a1], W=[b_ctmp])
                P.op("dve", lambda e: e.tensor_tensor(out=ctmp[:, 3, :], in0=send[:, 1, :], in1=a1[:, 0, :], op=ALU.mult), R=[b_send, b_a1], W=[b_ctmp])
                P.op("dve", lambda e: e.tensor_tensor(out=cre[:, 0, :], in0=ctmp[:, 0, :], in1=ctmp[:, 1, :], op=ALU.subtract), R=[b_ctmp], W=[b_cre])
                P.op("dve", lambda e: e.tensor_tensor(out=cre[:, 1, :], in0=ctmp[:, 2, :], in1=ctmp[:, 3, :], op=ALU.add), R=[b_ctmp], W=[b_cre])
                for q in range(32):
                    bk_ = 4 + q // 16
                    col = (q % 16) * 32
                    P.op("pe", (lambda q, bk_, col: lambda e: e.matmul(out=pbanks[bk_][:, col:col + 32], lhsT=sre[:, q, :], rhs=CmR[:, q * 32:(q + 1) * 32], start=True, stop=False))(q, bk_, col),
                         R=[b_sre, b_CmR], W=[pbuf[bk_]])
                    P.op("pe", (lambda q, bk_, col: lambda e: e.matmul(out=pbanks[bk_][:, col:col + 32], lhsT=sim_[:, q, :], rhs=CmI[:, q * 32:(q + 1) * 32], start=False, stop=True))(q, bk_, col),
                         R=[b_sim, b_CmI], W=[pbuf[bk_]])
                for hh in range(2):
                    P.op("dve", (lambda hh: lambda e: e.tensor_tensor(out=yy[:, hh * 512:(hh + 1) * 512], in0=pbanks[4 + hh], in1=ud[:, hh * 512:(hh + 1) * 512], op=ALU.add))(hh),
                         R=[pbuf[4 + hh], b_ud], W=[b_yy])
                P.op("act", lambda e: e.activation(out=gb, in_=yy, func=AF.Gelu), R=[b_yy], W=[b_gb])
                for kc in range(8):
                    P.op("pe", (lambda kc: lambda e: e.transpose(out=pbf(6)[:, kc * 128:(kc + 1) * 128], in_=gb[:, kc * 128:(kc + 1) * 128], identity=identb))(kc),
                         R=[b_gb, b_identb], W=[pbuf[6]])
                copy_rr(gTt, pbf(6).rearrange("p (a b) -> p a b", b=128), [pbuf[6]], [b_gTt])
                P.dma(gT_dram[s_, :, :, qt * 128:(qt + 1) * 128], gTt, R=[b_gTt], W=[gT_b[s_]])

            NGT = SEQ_PER_CORE * NT

            def hload(gi):
                P.dma(ht[:, gi % 2, :], hres[gi * 128:(gi + 1) * 128, :], R=[hres_b[gi]], W=[b_ht[gi % 2]])
            hload(0)
            hload(1)
            front(0)
            for gi in range(NGT):
                if gi + 2 < NGT:
                    hload(gi + 2)
                if gi + 1 < NGT:
                    P.fork(2)
                    P.stream(0)
                    front(gi + 1)
                    P.stream(1)
                    back(gi)
                    n0, n1 = len(P.streams[0]), len(P.streams[1])
                    P.join([max(1, n0 // n1), max(1, n1 // n0)])
                else:
                    back(gi)
            A.release(m_scan)
            A.release(m_s5)
            rr["mode"] = "alt"
            m_g = A.mark()
            LG1b, b_LG1b = A.alloc([128, 1024], F32, name="LG1b")
            LB1b, b_LB1b = A.alloc([128, 1024], F32, name="LB1b")
            P.dma(LG1b, ln_mix_g[1].partition_broadcast(128), W=[b_LG1b])
            P.dma(LB1b, ln_mix_b[1].partition_broadcast(128), W=[b_LB1b])
            W1, b_W1 = A.alloc([128, 8, 1024], BF16, name="W1")
            W2, b_W2 = A.alloc([128, 8, 1024], BF16, name="W2")
            Wo1, b_Wo1 = A.alloc([128, 8, 1024], BF16, name="Wo1")
            wdma(W1, b_W1, s5_glu_w1[0])
            wdma(W2, b_W2, s5_glu_w2[0])
            wdma(Wo1, b_Wo1, s5_w_out[0])
            gTs2, b_gTs2 = A.alloc([128, 2, 8, 512], BF16, nbufs=2, name="gTs")
            zT2, b_zT2 = A.alloc([128, 2, 8, 512], BF16, nbufs=2, name="zT")
            sgm, b_sgm = A.alloc([128, 2, 512], F32, nbufs=2, name="sgm")
            ght, b_ght = A.alloc([128, 2, 1024], F32, nbufs=2, name="ght")
            rr3, b_rr3 = A.alloc([128, 1024], F32, name="rr3")
            h3, b_h3 = A.alloc([128, 2, 1024], F32, nbufs=2, name="h3")
            if True:
                def gfront(bi_):
                    s_, nb_ = bi_ // 4, bi_ % 4
                    gTs, b_gTs, zT, b_zT = gTs2[:, bi_ % 2], b_gTs2[bi_ % 2], zT2[:, bi_ % 2], b_zT2[bi_ % 2]
                    for fc in range(8):
                        b1_, b2_ = (fc % 2) * 2, 1 + (fc % 2) * 2
                        for kc in range(8):
                            P.op("pe", (lambda fc, kc, b1_: lambda e: e.matmul(out=pbanks[b1_], lhsT=W1[:, kc, fc * 128:(fc + 1) * 128], rhs=gTs[:, kc, :], start=(kc == 0), stop=(kc == 7)))(fc, kc, b1_),
                                 R=[b_W1, b_gTs], W=[pbuf[b1_]])
                        for kc in range(8):
                            P.op("pe", (lambda fc, kc, b2_: lambda e: e.matmul(out=pbanks[b2_], lhsT=W2[:, kc, fc * 128:(fc + 1) * 128], rhs=gTs[:, kc, :], start=(kc == 0), stop=(kc == 7)))(fc, kc, b2_),
                                 R=[b_W2, b_gTs], W=[pbuf[b2_]])
                        sp_ = fc % 2
                        P.op("act", (lambda b2_, sp_: lambda e: e.activation(out=sgm[:, sp_, :], in_=pbanks[b2_], func=AF.Sigmoid))(b2_, sp_), R=[pbuf[b2_]], W=[b_sgm[sp_]])
                        P.op("dve", (lambda fc, b1_, sp_: lambda e: e.tensor_tensor(out=zT[:, fc, :], in0=pbanks[b1_], in1=sgm[:, sp_, :], op=ALU.mult))(fc, b1_, sp_),
                             R=[pbuf[b1_], b_sgm[sp_]], W=[b_zT])
                def gback(bi_):
                    s_, nb_ = bi_ // 4, bi_ % 4
                    gTs, b_gTs, zT, b_zT = gTs2[:, bi_ % 2], b_gTs2[bi_ % 2], zT2[:, bi_ % 2], b_zT2[bi_ % 2]
                    for t4 in range(4):
                        gi = s_ * NT + nb_ * 4 + t4
                        par = gi % 2
                        for hh in range(2):
                            for fc in range(8):
                                P.op("pe", (lambda t4, hh, fc: lambda e: e.matmul(out=pbanks[4 + hh], lhsT=zT[:, fc, t4 * 128:(t4 + 1) * 128], rhs=Wo1[:, fc, hh * 512:(hh + 1) * 512],
                                                                                start=(fc == 0), stop=(fc == 7)))(t4, hh, fc), R=[b_zT, b_Wo1], W=[pbuf[4 + hh]])
                        P.dma(ght[:, par, :], hres[gi * 128:(gi + 1) * 128, :], R=[hres_b[gi]], W=[b_ght[par]])
                        for hh in range(2):
                            P.op("dve", (lambda hh, par: lambda e: e.scalar_tensor_tensor(out=rr3[:, hh * 512:(hh + 1) * 512], in0=ght[:, par, hh * 512:(hh + 1) * 512], scalar=ALPHA,
                                                                                       in1=pbanks[4 + hh], op0=ALU.mult, op1=ALU.add))(hh, par), R=[b_ght[par], pbuf[4 + hh]], W=[b_rr3])
                        m_ln = A.mark()
                        layer_norm_tile(rr3, b_rr3, LG1b, LB1b, b_LG1b, h3[:, par, :], b_h3[par], b_LB1b)
                        A.release(m_ln)
                        P.dma(hres[gi * 128:(gi + 1) * 128, :], h3[:, par, :], R=[b_h3[par]], W=[hres_b[gi]])
            NB_ = SEQ_PER_CORE * 4

            def gload(bi_):
                P.dma(gTs2[:, bi_ % 2], gT_dram[bi_ // 4, :, :, (bi_ % 4) * 512:(bi_ % 4 + 1) * 512], R=[gT_b[bi_ // 4]], W=[b_gTs2[bi_ % 2]])
            gload(0)
            gload(1)
            gfront(0)
            for bi_ in range(NB_):
                if bi_ + 2 < NB_:
                    gload(bi_ + 2)
                if bi_ + 1 < NB_:
                    P.fork(2)
                    P.stream(0)
                    gfront(bi_ + 1)
                    P.stream(1)
                    gback(bi_)
                    n0, n1 = len(P.streams[0]), len(P.streams[1])
                    P.join([max(1, n0 // n1), max(1, n1 // n0)])
                else:
                    gback(bi_)
            A.release(m_g)

        if os.environ.get("PRECAST", "1") == "1":
            while precast_state["i"] < 32:
                precast_next()
        if debug != "l0":
            moe_stage(0, final=(debug == "moe0"))
        if debug is None or debug in ("s5", "full", "s5dbg"):
            s5_stage()
            if debug == "s5dbg":
                pass
            elif debug == "s5":
                for gi in range(SEQ_PER_CORE * NT):
                    P.dma(y[gi // NT, (gi % NT) * 128:(gi % NT + 1) * 128, :], hres[gi * 128:(gi + 1) * 128, :], R=[hres_b[gi]], W=[y_b])
            else:
                moe_stage(1, final=True)
        if debug == "l0":
            for gi in range(SEQ_PER_CORE * NT):
                s, qt = gi // NT, gi % NT
                if s >= nseq or qt >= nqt or stop:
                    continue
                P.dma(y[s, qt * 128:(qt + 1) * 128, :], hres[gi * 128:(gi + 1) * 128, :], R=[hres_b[gi]], W=[y_b])
        P.emit()
        print("ops", len(P.ops), P.stats, flush=True)
    return nc


_NC_CACHE = {}


def kernel(**inputs):
    n = 8
    if "nc" not in _NC_CACHE:
        _NC_CACHE["nc"] = build()
    nc = _NC_CACHE["nc"]
    x = np.ascontiguousarray(inputs["x"], dtype=np.float32)
    in_maps = []
    for c in range(n):
        m = {k: np.ascontiguousarray(v) for k, v in inputs.items() if k != "x"}
        m["x"] = x[c * SEQ_PER_CORE:(c + 1) * SEQ_PER_CORE]
        in_maps.append(m)
    res = run_bass_kernel_spmd(nc, in_maps, core_ids=list(range(n)))
    return np.concatenate([r["y"] for r in res.results], axis=0)
```

```python
import math
import os
import contextlib
import numpy as np
import concourse.bass as bass
import concourse.mybir as mybir
from concourse.bass_utils import run_bass_kernel_spmd

F32 = mybir.dt.float32
BF16 = mybir.dt.bfloat16
I32 = mybir.dt.int32
ALU = mybir.AluOpType
AF = mybir.ActivationFunctionType
AX = mybir.AxisListType

ENGS = ("pe", "act", "dve", "pool", "sp")
N_DMA_SEMS = 56
NSW = int(os.environ.get('NSW', '3'))

D = 1024
L = 2048
NT = 16
SEQ_PER_CORE = 2
ALPHA = (2.0 * 2) ** 0.25
EPS = 1e-5
NEG = -1e30
NTILE = 31
NG_TOK = SEQ_PER_CORE * NT
T5_STARTS = [1, 2, 3, 4, 5, 6, 7, 8, 9, 10, 11, 12, 13, 14, 15, 16, 19, 21, 24, 27, 31, 35, 40,
             46, 52, 59, 67, 77, 87, 99, 113]


class Buf:
    __slots__ = ("name", "lw", "rd")

    def __init__(self, name=""):
        self.name = name
        self.lw = None
        self.rd = []


class Prog:
    def __init__(self, nc):
        self.nc = nc
        self.ops = []
        self.cur = self.ops
        self.nuid = 0
        self.dma_rr = 0
        self.dma_rr_sw = 0

    def op(self, eng, fn, R=(), W=(), dma=False):
        if getattr(self, "mute", False):
            return None
        deps = set()
        raw = set()
        for b in R:
            if b.lw is not None:
                deps.add(b.lw)
                raw.add(b.lw)
        for b in W:
            if b.lw is not None:
                deps.add(b.lw)
            deps.update(b.rd)
        oid = self.nuid
        self.nuid += 1
        self.cur.append(dict(uid=oid, eng=eng, fn=fn, deps=deps, raw=raw, dma=dma))
        for b in R:
            b.rd.append(oid)
        for b in W:
            b.lw = oid
            b.rd = []
        return oid

    def fork(self, n):
        self.streams = [[] for _ in range(n)]

    def stream(self, k):
        self.cur = self.streams[k] if k is not None else self.ops

    def join(self, ratio=None):
        st = self.streams
        ratio = ratio or [1] * len(st)
        idx = [0] * len(st)
        while any(idx[k] < len(st[k]) for k in range(len(st))):
            for k in range(len(st)):
                for _ in range(ratio[k]):
                    if idx[k] < len(st[k]):
                        self.ops.append(st[k][idx[k]])
                        idx[k] += 1
        self.cur = self.ops
        self.streams = None

    def dma(self, out, in_, R=(), W=(), q="sp", **kw):
        return self.op(q, lambda e: e.dma_start(out=out, in_=in_, **kw), R, W, dma=True)

    def emit(self):
        nc = self.nc
        ops = self.ops
        pos = {o["uid"]: i for i, o in enumerate(ops)}
        for i, o in enumerate(ops):
            o["deps"] = set(pos[d] for d in o["deps"])
            o["raw"] = set(pos[d] for d in o["raw"])
            assert all(d < i for d in o["deps"]), "stream merge broke dependency order"
        eng_idx = {e: 0 for e in ENGS}
        dma_cnt = [0] * N_DMA_SEMS
        dma_last = [None] * N_DMA_SEMS
        for i, o in enumerate(ops):
            if o["dma"]:
                if o["eng"] == "pool":
                    k = N_DMA_SEMS - NSW + self.dma_rr_sw % NSW
                    self.dma_rr_sw += 1
                else:
                    k = self.dma_rr % (N_DMA_SEMS - NSW)
                    self.dma_rr += 1
                if dma_last[k] is not None:
                    o["deps"].add(dma_last[k])
                dma_last[k] = i
                dma_cnt[k] += 1
                o["tok"] = ("d%d" % k, dma_cnt[k])
            else:
                eng_idx[o["eng"]] += 1
                o["tok"] = (o["eng"], eng_idx[o["eng"]])
        eclock = {e: {} for e in ENGS}
        for i, o in enumerate(ops):
            e = o["eng"]
            ck = eclock[e]
            wm = {}
            for d in sorted(o["deps"]):
                od = ops[d]
                s, v = od["tok"]
                if (not od["dma"]) and od["eng"] == e and (e == "pe" or d not in o["raw"]):
                    continue
                if ck.get(s, 0) >= v:
                    continue
                if wm.get(s, 0) < v:
                    wm[s] = v
                for s2, v2 in od["clock"].items():
                    if ck.get(s2, 0) < v2:
                        ck[s2] = v2
            o["waits"] = wm
            c2 = dict(ck)
            c2[o["tok"][0]] = o["tok"][1]
            o["clock"] = c2
        need = {e: set() for e in ENGS}
        for o in ops:
            for s, v in o["waits"].items():
                if s in need:
                    need[s].add(v)
        remap = {e: {v: k + 1 for k, v in enumerate(sorted(need[e]))} for e in ENGS}
        self.stats = {e: (eng_idx[e], len(need[e])) for e in ENGS}
        for o in ops:
            o.pop("clock", None)
        with contextlib.ExitStack() as st:
            sems = {}
            for e in ENGS:
                sems[e] = st.enter_context(nc.semaphore("s_" + e))
            for k in range(N_DMA_SEMS):
                if dma_cnt[k]:
                    sems["d%d" % k] = st.enter_context(nc.semaphore("s_d%d" % k))
            block = st.enter_context(nc.Block())
            per = {e: [o for o in ops if o["eng"] == e] for e in ENGS}
            final_dma = [("d%d" % k, dma_cnt[k] * 16) for k in range(N_DMA_SEMS) if dma_cnt[k]]

            def run(engname, eng):
                for o in per[engname]:
                    for s, v in o["waits"].items():
                        if s in remap:
                            eng.wait_ge(sems[s], remap[s][v])
                        else:
                            eng.wait_ge(sems[s], v * 16)
                    ins = o["fn"](eng)
                    s, v = o["tok"]
                    if o["dma"]:
                        ins.then_inc(sems[s], 16)
                    elif v in remap[s]:
                        ins.then_inc(sems[s], 1)
                if engname == "sp":
                    for s, v in final_dma:
                        eng.wait_ge(sems[s], v)

            @block.tensor
            def _(eng):
                run("pe", eng)

            @block.scalar
            def _(eng):
                run("act", eng)

            @block.vector
            def _(eng):
                run("dve", eng)

            @block.gpsimd
            def _(eng):
                run("pool", eng)

            @block.sync
            def _(eng):
                run("sp", eng)


class Arena:
    def __init__(self, ap, words):
        self.ap = ap
        self.words = words
        self.top = 0
        self.live = []
        self.dead = []

    def mark(self):
        return (self.top, len(self.live))

    def release(self, m):
        top, n = m
        self.dead.extend(self.live[n:])
        del self.live[n:]
        self.top = top

    def alloc(self, shape, dt=F32, nbufs=1, name=""):
        per = int(np.prod(shape[1:]))
        words = (per * (2 if dt == BF16 else 4) + 3) // 4
        words = (words + 7) // 8 * 8
        s, e = self.top, self.top + words
        assert e <= self.words, "arena overflow %s %d > %d" % (name, e, self.words)
        self.top = e
        bufs = [Buf(name) for _ in range(nbufs)]
        keep = []
        for (ds, de, db) in self.dead:
            if ds < e and s < de:
                for ob in db:
                    for nb in bufs:
                        nb.rd.extend(ob.rd)
                        if ob.lw is not None:
                            nb.rd.append(ob.lw)
            keep.append((ds, de, db))
        self.dead = keep
        self.live.append((s, e, bufs))
        v = self.ap[0:shape[0], s:e]
        if dt == BF16:
            v = v.bitcast(BF16)[:, 0:per]
        elif dt == I32:
            v = v.bitcast(I32)[:, 0:per]
        else:
            v = v[:, 0:per]
        if len(shape) == 3:
            v = v.rearrange("p (a b) -> p a b", b=shape[2])
        elif len(shape) == 4:
            v = v.rearrange("p (a b c) -> p a b c", b=shape[2], c=shape[3])
        return (v, bufs[0]) if nbufs == 1 else (v, bufs)


def build(debug=None, stop=None, nqt=NT, nseq=SEQ_PER_CORE, gstop=0):
    nc = bass.Bass("TRN2", target_bir_lowering=False)

    def din(name, shape, dt=F32):
        return nc.dram_tensor(name, list(shape), dt, kind="ExternalInput").ap()

    x = din("x", [SEQ_PER_CORE, L, D])
    rel_bias = din("rel_bias", [32, 8])
    ab_w_in = din("ab_w_in", [1, D, 2644])
    gla_gate_w2 = din("gla_gate_w2", [1, 16, 256])
    gla_gate_b = din("gla_gate_b", [1, 256])
    gla_norm_g = din("gla_norm_g", [1, 512])
    ab_w_out = din("ab_w_out", [1, D, D])
    ln_mix_g = din("ln_mix_g", [2, D])
    ln_mix_b = din("ln_mix_b", [2, D])
    s5_w_in = din("s5_w_in", [1, D, D])
    s5_lam_re = din("s5_lam_re", [1, 64, 64])
    s5_lam_im = din("s5_lam_im", [1, 64, 64])
    s5_log_dt = din("s5_log_dt", [1, 64])
    s5_b_re = din("s5_b_re", [1, 64, 64, 16])
    s5_b_im = din("s5_b_im", [1, 64, 64, 16])
    s5_c_re = din("s5_c_re", [1, 64, 16, 64])
    s5_c_im = din("s5_c_im", [1, 64, 16, 64])
    s5_d = din("s5_d", [1, D])
    s5_glu_w1 = din("s5_glu_w1", [1, D, D])
    s5_glu_w2 = din("s5_glu_w2", [1, D, D])
    s5_w_out = din("s5_w_out", [1, D, D])
    gT_dram = nc.dram_tensor("gT_dram", [SEQ_PER_CORE, 128, 8, L], BF16, kind="Internal").ap()
    ln_ffn_g = din("ln_ffn_g", [2, D])
    ln_ffn_b = din("ln_ffn_b", [2, D])
    moe_r_coarse = din("moe_r_coarse", [2, D, 4])
    moe_rb_coarse = din("moe_rb_coarse", [2, 4])
    moe_r_fine = din("moe_r_fine", [2, 4, D, 4])
    moe_rb_fine = din("moe_rb_fine", [2, 4, 4])
    moe_w_gate = din("moe_w_gate", [2, 4, 4, D, 512])
    moe_w_up = din("moe_w_up", [2, 4, 4, D, 512])
    moe_w_down = din("moe_w_down", [2, 4, 4, 512, D])
    wgb = nc.dram_tensor("wgb", [2, 4, 4, D, 512], BF16, kind="Internal").ap()
    wub = nc.dram_tensor("wub", [2, 4, 4, D, 512], BF16, kind="Internal").ap()
    wdb = nc.dram_tensor("wdb", [2, 4, 4, 512, D], BF16, kind="Internal").ap()
    NSLOT = NTILE * 512
    xs_dram = nc.dram_tensor("xs_dram", [NSLOT, D], BF16, kind="Internal").ap()
    ys_dram = nc.dram_tensor("ys_dram", [NSLOT, D], F32, kind="Internal").ap()
    y = nc.dram_tensor("y", [SEQ_PER_CORE, L, D], F32, kind="ExternalOutput").ap()
    hres = nc.dram_tensor("hres", [SEQ_PER_CORE * L, D], F32, kind="Internal").ap()

    st = contextlib.ExitStack()
    with st:
        AW = 53200
        arena_t = st.enter_context(nc.sbuf_tensor("arena", [128, AW], F32))
        A = Arena(arena_t[:, :], AW)
        pbanks = [st.enter_context(nc.psum_tensor("pb%d" % i, [128, 512], F32))[:, :] for i in range(8)]
        pbuf = [Buf("pb%d" % i) for i in range(8)]
        P = Prog(nc)
        hres_b = [Buf("hres%d" % i) for i in range(SEQ_PER_CORE * NT)]
        y_b = Buf("y")

        def pbf(i):
            return pbanks[i].bitcast(BF16)

        rr = {"cp": 0}

        def copy_rr(out, in_, R, W):
            rr["cp"] += 1
            if rr["cp"] % 2:
                P.op("act", lambda e: e.copy(out=out, in_=in_), R=R, W=W)
            else:
                P.op("dve", lambda e: e.tensor_copy(out=out, in_=in_), R=R, W=W)

        ident, b_ident = A.alloc([128, 128], F32, name="ident")
        identb, b_identb = A.alloc([128, 128], BF16, name="identb")
        P.op("pool", lambda e: e.memset(ident, 1.0), W=[b_ident])
        P.op("pool", lambda e: e.affine_select(out=ident, in_=ident, pattern=[[-1, 128]], compare_op=ALU.is_equal,
                                               fill=0.0, base=0, channel_multiplier=1), R=[b_ident], W=[b_ident])
        P.op("dve", lambda e: e.tensor_copy(out=identb, in_=ident), R=[b_ident], W=[b_identb])

        def wload(shape_cols, src, name):
            t, b = A.alloc([128, 8, shape_cols], BF16, name=name)
            return t, b

        def wdma(dst, b, src2d):
            P.dma(dst, src2d.rearrange("(kc p) n -> p kc n", p=128), W=[b], q="pool")

        def layer_norm_tile(r, b_r, g_bc, b_bc, b_gb, out, b_out, b_bb=None, tmps=None):
            if tmps is not None:
                stt, b_st, mv, b_mv, sd, b_sd = tmps
            else:
                stt, b_st = A.alloc([128, 2, 6], F32, name="lnst")
                mv, b_mv = A.alloc([128, 2], F32, name="lnmv")
            for hh in range(2):
                P.op("dve", (lambda hh: lambda e: e.bn_stats(out=stt[:, hh, :], in_=r[:, hh * 512:(hh + 1) * 512]))(hh),
                     R=[b_r], W=[b_st])
            P.op("dve", lambda e: e.bn_aggr(out=mv, in_=stt.rearrange("p a b -> p (a b)")), R=[b_st], W=[b_mv])
            if tmps is None:
                sd, b_sd = A.alloc([128, 2], F32, name="lnsd")
            P.op("act", lambda e: e.activation(out=sd[:, 0:1], in_=mv[:, 1:2], func=AF.Sqrt, bias=eps_t[:, 0:1], scale=1.0),
                 R=[b_mv, b_eps], W=[b_sd])
            P.op("dve", lambda e: e.reciprocal(out=sd[:, 1:2], in_=sd[:, 0:1]), R=[b_sd], W=[b_sd])
            P.op("dve", lambda e: e.tensor_scalar(out=r, in0=r, scalar1=mv[:, 0:1], scalar2=sd[:, 1:2],
                                                  op0=ALU.subtract, op1=ALU.mult), R=[b_r, b_mv, b_sd], W=[b_r])
            P.op("pool", lambda e: e.tensor_tensor(out=r, in0=r, in1=g_bc, op=ALU.mult), R=[b_r, b_gb], W=[b_r])
            P.op("pool", lambda e: e.tensor_tensor(out=out, in0=r, in1=b_bc, op=ALU.add), R=[b_r, b_gb] + ([b_bb] if b_bb is not None else []), W=[b_out])

        bar_t, b_bar = A.alloc([128, 8], F32, name="bar")

        def barrier():
            bufs = [b for (_, _, bs) in A.live + A.dead for b in bs] + pbuf + hres_b + [b_bar]
            seen = {}
            for b in bufs:
                seen[id(b)] = b
            bufs = list(seen.values())
            P.op("dve", lambda e: e.memset(bar_t, 0.0), R=bufs, W=bufs)

        eps_t, b_eps = A.alloc([128, 1], F32, name="eps")
        P.op("pool", lambda e: e.memset(eps_t, EPS), W=[b_eps])
        halfpi, b_hp = A.alloc([128, 1], F32, name="halfpi")
        P.op("pool", lambda e: e.memset(halfpi, math.pi / 2), W=[b_hp])

        m_l0 = A.mark()
        W0 = ab_w_in[0]
        Wq, b_Wq = wload(512, None, "Wq")
        Wiq, b_Wiq = wload(256, None, "Wiq")
        Wiw, b_Wiw = wload(4, None, "Wiw")
        Wk, b_Wk = wload(256, None, "Wk")
        Wik, b_Wik = wload(128, None, "Wik")
        Wv, b_Wv = wload(128, None, "Wv")
        Wbq, b_Wbq = wload(256, None, "Wbq")
        Wbk, b_Wbk = wload(256, None, "Wbk")
        Wbv, b_Wbv = wload(512, None, "Wbv")
        Wbg, b_Wbg = wload(16, None, "Wbg")
        Wbr, b_Wbr = wload(512, None, "Wbr")
        Wo, b_Wo = wload(1024, None, "Wo")
        wdma(Wq, b_Wq, W0[:, 0:512])
        for g in range(2):
            for r2 in range(2):
                c0 = (2 * g + r2) * 64
                P.dma(Wk[:, :, c0:c0 + 64], W0[:, 512 + g * 64:512 + (g + 1) * 64].rearrange("(kc p) n -> p kc n", p=128),
                      W=[b_Wk], q="pool")
        wdma(Wv, b_Wv, W0[:, 640:768])
        wdma(Wiq, b_Wiq, W0[:, 768:1024])
        for r2 in range(2):
            P.dma(Wik[:, :, r2 * 64:(r2 + 1) * 64], W0[:, 1024:1088].rearrange("(kc p) n -> p kc n", p=128),
                  W=[b_Wik], q="pool")
        wdma(Wiw, b_Wiw, W0[:, 1088:1092])
        wdma(Wbq, b_Wbq, W0[:, 1092:1348])
        wdma(Wbk, b_Wbk, W0[:, 1348:1604])
        wdma(Wbv, b_Wbv, W0[:, 1604:2116])
        wdma(Wbg, b_Wbg, W0[:, 2116:2132])
        wdma(Wbr, b_Wbr, W0[:, 2132:2644])
        wdma(Wo, b_Wo, ab_w_out[0])

        LG, b_LG = A.alloc([128, 1024], F32, name="LG")
        LB, b_LB = A.alloc([128, 1024], F32, name="LB")
        P.dma(LG, ln_mix_g[0].partition_broadcast(128), W=[b_LG])
        P.dma(LB, ln_mix_b[0].partition_broadcast(128), W=[b_LB])
        NG, b_NG = A.alloc([128, 512], F32, name="NG")
        P.dma(NG, gla_norm_g[0].partition_broadcast(128), W=[b_NG])
        G2, b_G2 = A.alloc([32, 256], F32, name="G2")
        P.op("pool", lambda e: e.memset(G2, 0.0), W=[b_G2])
        P.dma(G2[0:16, :], gla_gate_w2[0], W=[b_G2])
        P.dma(G2[16:17, :], gla_gate_b[0:1, :], W=[b_G2])

        TRI, b_TRI = A.alloc([128, 128], F32, name="TRI")
        TRI2, b_TRI2 = A.alloc([128, 128], F32, name="TRI2")
        MASKT, b_MASKT = A.alloc([128, 128], F32, name="MASKT")
        for (T_, b_, val, strict) in ((TRI, b_TRI, -1.0 / 16, False), (MASKT, b_MASKT, 1.0, False), (TRI2, b_TRI2, -1.0 / 16, True)):
            P.op("pool", (lambda T_, val: lambda e: e.memset(T_, val))(T_, val), W=[b_])
            if not strict:
                P.op("pool", (lambda T_: lambda e: e.affine_select(out=T_, in_=T_, pattern=[[1, 128]], compare_op=ALU.is_ge,
                                                                   fill=0.0, base=0, channel_multiplier=-1))(T_), R=[b_], W=[b_])
                P.op("pool", (lambda T_: lambda e: e.memset(T_[0:64, 64:128], 0.0))(T_), R=[b_], W=[b_])
            else:
                P.op("pool", (lambda T_: lambda e: e.affine_select(out=T_, in_=T_, pattern=[[-1, 128]], compare_op=ALU.is_ge,
                                                                   fill=0.0, base=-1, channel_multiplier=1))(T_), R=[b_], W=[b_])
                P.op("pool", (lambda T_: lambda e: e.memset(T_[64:128, 0:64], 0.0))(T_), R=[b_], W=[b_])

        Bp, b_Bp = A.alloc([128, 8, 256], F32, name="Bp")
        m_tmp = A.mark()
        RB, b_RB = A.alloc([128, 32, 8], F32, name="RB")
        DL, b_DL = A.alloc([128, 32, 8], F32, name="DL")
        P.dma(RB.rearrange("p a b -> p (a b)"), rel_bias.rearrange("a b -> (a b)").partition_broadcast(128), W=[b_RB])
        P.op("dve", lambda e: e.tensor_tensor(out=DL[:, 1:32, :], in0=RB[:, 1:32, :], in1=RB[:, 0:31, :], op=ALU.subtract),
             R=[b_RB], W=[b_DL])
        P.op("dve", lambda e: e.tensor_tensor(out=DL[:, 0:1, :], in0=RB[:, 0:1, :], in1=RB[:, 31:32, :], op=ALU.subtract),
             R=[b_RB], W=[b_DL])
        Dt, b_Dt = A.alloc([128, 256], F32, name="Dt")
        Ib, b_Ib = A.alloc([128, 31, 256], BF16, name="Ib")
        P.op("pool", lambda e: e.iota(out=Dt, pattern=[[-1, 256]], base=128, channel_multiplier=1,
                                      allow_small_or_imprecise_dtypes=True), W=[b_Dt])
        for bi, sv in enumerate(T5_STARTS):
            P.op("dve", (lambda bi, sv: lambda e: e.tensor_scalar(out=Ib[:, bi, :], in0=Dt, scalar1=float(sv) - 0.5, scalar2=None,
                                                                  op0=ALU.is_ge))(bi, sv), R=[b_Dt], W=[b_Ib])
        for h in range(8):
            P.op("dve", (lambda h: lambda e: e.tensor_scalar(out=Bp[:, h, :], in0=Ib[:, 0, :], scalar1=DL[:, 1, h:h + 1],
                                                             scalar2=DL[:, 0, h:h + 1], op0=ALU.mult, op1=ALU.add))(h),
                 R=[b_Ib, b_DL], W=[b_Bp])
            for bi in range(1, 31):
                P.op("dve", (lambda h, bi: lambda e: e.scalar_tensor_tensor(out=Bp[:, h, :], in0=Ib[:, bi, :],
                                                                           scalar=DL[:, bi + 1, h:h + 1], in1=Bp[:, h, :],
                                                                           op0=ALU.mult, op1=ALU.add))(h, bi),
                     R=[b_Ib, b_DL, b_Bp], W=[b_Bp])
        A.release(m_tmp)

        bg1, b_bg1 = A.alloc([32, 128], F32, name="bg1")
        P.op("pool", lambda e: e.memset(bg1, 1.0), W=[b_bg1])

        hT, b_hT = A.alloc([128, 8, L], BF16, nbufs=NT, name="hT")
        kTd, b_kTd = A.alloc([128, 2, L], BF16, name="kTd")
        ikT2, b_ikT2 = A.alloc([128, L], BF16, name="ikT2")
        vtok, b_vtok = A.alloc([128, NT, 128], BF16, name="vtok")
        xs, b_xs = A.alloc([128, 1, 1024], F32, nbufs=1, name="xs")
        b_xs = [b_xs, b_xs]
        sc, b_sc = A.alloc([128, L], F32, name="sc")
        rt, b_rt = A.alloc([128, 2, 512], F32, nbufs=2, name="rt")
        selm2, b_selm2 = A.alloc([128, 2, L], BF16, nbufs=2, name="selm1")
        lg2, b_lg2 = A.alloc([128, 2, L], F32, nbufs=2, name="lg")
        pp2, b_pp2 = A.alloc([128, 2, L], BF16, nbufs=2, name="pp")
        pT2, b_pT2 = A.alloc([128, 1, L], BF16, nbufs=1, name="pT")
        b_pT2 = [b_pT2, b_pT2]
        qTt2, b_qTt2 = A.alloc([128, 2, 4, 128], BF16, nbufs=2, name="qTt")
        iqTt, b_iqTt = A.alloc([128, 2, 128], BF16, name="iqTt")
        iwt, b_iwt = A.alloc([128, 4], F32, name="iwt")
        m8, b_m8 = A.alloc([128, 8], F32, name="m8")
        MB = 22
        TOPK_BISECT = os.environ.get("TOPK", "bisect") == "bisect"
        tkb, b_tkb = A.alloc([128, 16], F32, name="tkb")
        Wbis, b_Wbis = A.alloc([128, 24], F32, name="Wbis")
        POW2, b_POW2 = A.alloc([128, 24], F32, name="POW2")
        for k in range(MB):
            P.op("pool", (lambda k: lambda e: e.memset(POW2[:, k:k + 1], 2.0 ** -(k + 1)))(k), W=[b_POW2])
        sm2, b_sm2 = A.alloc([128, 2, 4], F32, nbufs=2, name="sm")
        oa, b_oa = A.alloc([128, 1024], BF16, name="oa")
        oT, b_oT = A.alloc([128, 8, 128], BF16, name="oT")
        lap, b_lap = A.alloc([128, 256], F32, name="lap")
        EcT, b_EcT = A.alloc([128, 2, 128], F32, name="EcT")
        EnT, b_EnT = A.alloc([128, 2, 128], F32, name="EnT")
        Er, b_Er = A.alloc([128, 256], F32, name="Er")
        qdT, b_qdT = A.alloc([128, 2, 128], BF16, name="qdT")
        kiT, b_kiT = A.alloc([128, 2, 128], BF16, name="kiT")
        kend, b_kend = A.alloc([128, 256], BF16, name="kend")
        vt, b_vt = A.alloc([128, 512], BF16, name="vt")
        sbr, b_sbr = A.alloc([128, 512], F32, name="sbr")
        scT, b_scT = A.alloc([128, 4, 128], BF16, name="scT")
        Sf, b_Sf = A.alloc([128, 2, 128], F32, name="Sf")
        Sb, b_Sb = A.alloc([128, 2, 128], BF16, name="Sb")
        on, b_on = A.alloc([128, 512], F32, name="on")
        gst, b_gst = A.alloc([128, 4, 6], F32, name="gst")
        gmv, b_gmv = A.alloc([128, 4, 2], F32, name="gmv")
        gsd, b_gsd = A.alloc([128, 4, 2], F32, name="gsd")
        rr_, b_rr = A.alloc([128, 1024], F32, name="r")

        wcast_b = [[Buf("wc%d_%d" % (l, k)) for k in range(48)] for l in range(2)]
        precast_state = {"i": 0}

        def precast_next():
            i = precast_state["i"]
            if i >= 32:
                return
            precast_state["i"] = i + 1
            l_, g_, e_ = i // 16, (i % 16) // 4, i % 4
            k_ = (i % 16) * 3
            P.dma(wgb[l_, g_, e_], moe_w_gate[l_, g_, e_], W=[wcast_b[l_][k_]], q="pool")
            P.dma(wub[l_, g_, e_], moe_w_up[l_, g_, e_], W=[wcast_b[l_][k_ + 1]], q="pool")
            P.dma(wdb[l_, g_, e_], moe_w_down[l_, g_, e_], W=[wcast_b[l_][k_ + 2]], q="pool")

        for s in range(nseq):
            if stop == "consts":
                break
            for tt in range(NT):
                par = 0
                P.dma(xs[:, par, :], x[s, tt * 128:(tt + 1) * 128, :], W=[b_xs[par]])
                for g in range(2):
                    bk_ = (2 * tt + g) % 2
                    for c in range(4):
                        kc = 4 * g + c
                        P.op("pe", (lambda par, kc, c, bk_: lambda e: e.transpose(out=pbanks[bk_][:, c * 128:(c + 1) * 128],
                                                                                in_=xs[:, par, kc * 128:(kc + 1) * 128], identity=ident))(par, kc, c, bk_),
                             R=[b_xs[par], b_ident], W=[pbuf[bk_]])
                    copy_rr(hT[:, 4 * g:4 * g + 4, tt * 128:(tt + 1) * 128],
                            pbanks[bk_].rearrange("p (a b) -> p a b", b=128), [pbuf[bk_]], [b_hT[tt]])
            if stop == "s1":
                break
            for n in range(4):
                tsl = slice(n * 512, (n + 1) * 512)
                hb = b_hT[4 * n:4 * n + 4]
                for g in range(3):
                    bk_ = 2 + (n * 3 + g) % 2
                    Wt, bW, c0 = (Wk, b_Wk, g * 128) if g < 2 else (Wik, b_Wik, 0)
                    for kc in range(8):
                        P.op("pe", (lambda Wt, c0, kc, bk_, tsl: lambda e: e.matmul(out=pbanks[bk_], lhsT=Wt[:, kc, c0:c0 + 128], rhs=hT[:, kc, tsl],
                                                                                 start=(kc == 0), stop=(kc == 7)))(Wt, c0, kc, bk_, tsl),
                             R=[bW] + hb, W=[pbuf[bk_]])
                    dst = kTd[:, g, tsl] if g < 2 else ikT2[:, tsl]
                    copy_rr(dst, pbanks[bk_], [pbuf[bk_]], [b_kTd if g < 2 else b_ikT2])
            for g4 in range(4):
                bk_ = 2 + g4 % 2
                for c in range(4):
                    tt = 4 * g4 + c
                    for kc in range(8):
                        P.op("pe", (lambda tt, c, kc, bk_: lambda e: e.matmul(out=pbanks[bk_][:, c * 128:(c + 1) * 128], lhsT=hT[:, kc, tt * 128:(tt + 1) * 128],
                                                                           rhs=Wv[:, kc, :], start=(kc == 0), stop=(kc == 7)))(tt, c, kc, bk_),
                             R=[b_Wv, b_hT[tt]], W=[pbuf[bk_]])
                copy_rr(vtok[:, 4 * g4:4 * g4 + 4, :], pbanks[bk_].rearrange("p (a b) -> p a b", b=128), [pbuf[bk_]], [b_vtok])
            P.op("pool", lambda e: e.memset(Sf, 0.0), W=[b_Sf])
            P.op("pool", lambda e: e.memset(Sb, 0.0), W=[b_Sb])

            if stop == "s2":
                break
            def pre(qt):
                t0 = qt * 128
                S = (qt + 1) * 128
                tq = slice(t0, t0 + 128)
                bh = b_hT[qt]
                nch = (S + 511) // 512
                qTt, b_qTt = qTt2[:, qt % 2], b_qTt2[qt % 2]
                selm1, b_selm1 = selm2[:, qt % 2, :], b_selm2[qt % 2]
                for c in range(4):
                    for kc in range(8):
                        P.op("pe", (lambda c, kc, tq: lambda e: e.matmul(out=pbanks[0][:, c * 128:(c + 1) * 128], lhsT=Wq[:, kc, c * 128:(c + 1) * 128],
                                                                      rhs=hT[:, kc, tq], start=(kc == 0), stop=(kc == 7)))(c, kc, tq),
                             R=[b_Wq, bh], W=[pbuf[0]])
                P.op("act", lambda e: e.activation(out=qTt, in_=pbanks[0].rearrange("p (a b) -> p a b", b=128), func=AF.Copy, scale=0.125),
                     R=[pbuf[0]], W=[b_qTt])
                for c in range(2):
                    for kc in range(8):
                        P.op("pe", (lambda c, kc, tq: lambda e: e.matmul(out=pbanks[1][:, c * 128:(c + 1) * 128], lhsT=Wiq[:, kc, c * 128:(c + 1) * 128],
                                                                      rhs=hT[:, kc, tq], start=(kc == 0), stop=(kc == 7)))(c, kc, tq),
                             R=[b_Wiq, bh], W=[pbuf[1]])
                for kc in range(8):
                    P.op("pe", (lambda kc, tq: lambda e: e.matmul(out=pbanks[1][:, 256:260], lhsT=hT[:, kc, tq], rhs=Wiw[:, kc, :],
                                                               start=(kc == 0), stop=(kc == 7)))(kc, tq), R=[b_Wiw, bh], W=[pbuf[1]])
                P.op("act", lambda e: e.activation(out=iqTt, in_=pbanks[1][:, 0:256].rearrange("p (a b) -> p a b", b=128), func=AF.Copy, scale=0.125),
                     R=[pbuf[1]], W=[b_iqTt])
                P.op("act", lambda e: e.activation(out=iwt, in_=pbanks[1][:, 256:260], func=AF.Copy, scale=0.5), R=[pbuf[1]], W=[b_iwt])
                nch = (S + 511) // 512
                k_ = 0
                for j in range(nch):
                    w_ = min(512, S - j * 512)
                    cs = slice(j * 512, j * 512 + w_)
                    for hi in range(4):
                        c, base = hi // 2, (hi % 2) * 64
                        bk_ = 2 + hi % 2
                        par = k_ % 2
                        k_ += 1
                        P.op("pe", (lambda c, base, bk_, w_, cs: lambda e: e.matmul(out=pbanks[bk_][:, 0:w_], lhsT=iqTt[base:base + 64, c, :],
                                                                                 rhs=ikT2[base:base + 64, cs], start=True, stop=True))(c, base, bk_, w_, cs),
                             R=[b_iqTt, b_ikT2], W=[pbuf[bk_]])
                        P.op("act", (lambda par, bk_, w_: lambda e: e.activation(out=rt[:, par, 0:w_], in_=pbanks[bk_][:, 0:w_], func=AF.Relu))(par, bk_, w_),
                             R=[pbuf[bk_]], W=[b_rt[par]])
                        if hi == 0:
                            P.op("dve", (lambda par, w_, cs: lambda e: e.tensor_scalar(out=sc[:, cs], in0=rt[:, par, 0:w_], scalar1=iwt[:, 0:1], scalar2=None,
                                                                                    op0=ALU.mult))(par, w_, cs), R=[b_rt[par], b_iwt], W=[b_sc])
                        else:
                            P.op("dve", (lambda par, w_, cs, hi: lambda e: e.scalar_tensor_tensor(out=sc[:, cs], in0=rt[:, par, 0:w_], scalar=iwt[:, hi:hi + 1],
                                                                                               in1=sc[:, cs], op0=ALU.mult, op1=ALU.add))(par, w_, cs, hi),
                                 R=[b_rt[par], b_iwt, b_sc], W=[b_sc])
                P.op("pool", (lambda S: lambda e: e.affine_select(out=sc[:, S - 128:S], in_=sc[:, S - 128:S], pattern=[[-1, 128]], compare_op=ALU.is_ge,
                                                                  fill=NEG, base=0, channel_multiplier=1))(S), R=[b_sc], W=[b_sc])
            def topk(qt):
                t0 = qt * 128
                S = (qt + 1) * 128
                tq = slice(t0, t0 + 128)
                bh = b_hT[qt]
                nch = (S + 511) // 512
                qTt, b_qTt = qTt2[:, qt % 2], b_qTt2[qt % 2]
                selm1, b_selm1 = selm2[:, qt % 2, :], b_selm2[qt % 2]
                if qt >= 2 and TOPK_BISECT:
                    rtb = rt.rearrange("p a b -> p (a b)").bitcast(BF16)
                    Wrt = [b_rt[0], b_rt[1]]
                    T = lambda k: tkb[:, k:k + 1]
                    P.op("dve", (lambda S: lambda e: e.tensor_reduce(out=T(0), in_=sc[:, 0:S], axis=AX.X, op=ALU.max))(S), R=[b_sc], W=[b_tkb])
                    P.op("dve", (lambda S: lambda e: e.tensor_reduce(out=T(1), in_=sc[:, 0:S - 128], axis=AX.X, op=ALU.min))(S), R=[b_sc], W=[b_tkb])
                    P.op("dve", lambda e: e.tensor_scalar(out=T(2), in0=T(1), scalar1=-1.0, scalar2=None, op0=ALU.add), R=[b_tkb], W=[b_tkb])
                    P.op("dve", lambda e: e.tensor_tensor(out=T(3), in0=T(0), in1=T(2), op=ALU.subtract), R=[b_tkb], W=[b_tkb])
                    P.op("dve", lambda e: e.tensor_scalar(out=Wbis, in0=POW2, scalar1=T(3), scalar2=None, op0=ALU.mult), R=[b_tkb, b_POW2], W=[b_Wbis])
                    for k in range(MB):
                        P.op("dve", (lambda k: lambda e: e.tensor_tensor(out=T(4), in0=T(2), in1=Wbis[:, k:k + 1], op=ALU.add))(k), R=[b_tkb, b_Wbis], W=[b_tkb])
                        P.op("dve", (lambda S: lambda e: e.tensor_scalar(out=rtb[:, 0:S], in0=sc[:, 0:S], scalar1=T(4), scalar2=None, op0=ALU.is_gt, op1=ALU.add,
                                                                         accum_out=T(5)))(S), R=[b_sc, b_tkb], W=Wrt + [b_tkb])
                        P.op("dve", lambda e: e.tensor_scalar(out=T(6), in0=T(5), scalar1=255.5, scalar2=None, op0=ALU.is_ge), R=[b_tkb], W=[b_tkb])
                        P.op("dve", (lambda k: lambda e: e.scalar_tensor_tensor(out=T(2), in0=T(6), scalar=Wbis[:, k:k + 1], in1=T(2), op0=ALU.mult, op1=ALU.add))(k),
                             R=[b_tkb, b_Wbis], W=[b_tkb])
                    P.op("dve", lambda e: e.tensor_tensor(out=T(7), in0=T(2), in1=Wbis[:, MB - 1:MB], op=ALU.add), R=[b_tkb, b_Wbis], W=[b_tkb])
                    P.op("dve", (lambda S: lambda e: e.tensor_scalar(out=selm1[:, 0:S], in0=sc[:, 0:S], scalar1=T(7), scalar2=None, op0=ALU.is_gt, op1=ALU.add,
                                                                     accum_out=T(8)))(S), R=[b_sc, b_tkb], W=[b_selm1, b_tkb])
                    P.op("dve", (lambda S: lambda e: e.tensor_scalar(out=rtb[:, 0:S], in0=sc[:, 0:S], scalar1=T(2), scalar2=None, op0=ALU.is_gt))(S),
                         R=[b_sc, b_tkb], W=Wrt)
                    P.op("dve", (lambda S: lambda e: e.tensor_tensor(out=rtb[:, 0:S], in0=rtb[:, 0:S], in1=selm1[:, 0:S], op=ALU.subtract))(S), R=Wrt + [b_selm1], W=Wrt)
                    P.op("dve", lambda e: e.tensor_scalar(out=T(9), in0=T(8), scalar1=-1.0, scalar2=256.0, op0=ALU.mult, op1=ALU.add), R=[b_tkb], W=[b_tkb])
                    P.op("dve", (lambda S: lambda e: e.tensor_tensor_scan(out=sc[:, 0:S], data0=rtb[:, 0:S], data1=rtb[:, 0:S], initial=0.0, op0=ALU.add, op1=ALU.bypass))(S),
                         R=Wrt, W=[b_sc])
                    P.op("dve", (lambda S: lambda e: e.scalar_tensor_tensor(out=rtb[:, 0:S], in0=sc[:, 0:S], scalar=T(9), in1=rtb[:, 0:S], op0=ALU.is_le, op1=ALU.mult))(S),
                         R=[b_sc, b_tkb] + Wrt, W=Wrt)
                    P.op("dve", (lambda S: lambda e: e.scalar_tensor_tensor(out=selm1[:, 0:S], in0=rtb[:, 0:S], scalar=-1.0, in1=selm1[:, 0:S], op0=ALU.add, op1=ALU.add))(S),
                         R=Wrt + [b_selm1], W=[b_selm1])
                elif qt >= 2:
                    for it in range(32):
                        P.op("dve", (lambda S: lambda e: e.max(out=m8, in_=sc[:, 0:S]))(S), R=[b_sc], W=[b_m8])
                        P.op("dve", (lambda S: lambda e: e.match_replace(out=sc[:, 0:S], in_to_replace=m8, in_values=sc[:, 0:S], imm_value=-3e38))(S),
                             R=[b_sc, b_m8], W=[b_sc])
                    P.op("dve", (lambda S: lambda e: e.tensor_scalar(out=selm1[:, 0:S], in0=sc[:, 0:S], scalar1=-1e37, scalar2=1.0,
                                                                     op0=ALU.is_le, op1=ALU.subtract))(S), R=[b_sc], W=[b_selm1])
                else:
                    P.op("dve", (lambda S: lambda e: e.tensor_scalar(out=selm1[:, 0:S], in0=sc[:, 0:S], scalar1=-1e29, scalar2=1.0,
                                                                     op0=ALU.is_ge, op1=ALU.subtract))(S), R=[b_sc], W=[b_selm1])
            def rest(qt):
                t0 = qt * 128
                S = (qt + 1) * 128
                tq = slice(t0, t0 + 128)
                bh = b_hT[qt]
                nch = (S + 511) // 512
                qTt, b_qTt = qTt2[:, qt % 2], b_qTt2[qt % 2]
                selm1, b_selm1 = selm2[:, qt % 2, :], b_selm2[qt % 2]
                if os.environ.get("PRECAST", "1") == "1":
                    precast_next()
                P.dma(xs[:, 0, :], x[s, t0:t0 + 128, :], W=[b_xs[0]])
                def hbufs(h):
                    hp = h % 2
                    return (lg2[:, hp, :], b_lg2[hp], pp2[:, hp, :], b_pp2[hp], pT2[:, 0, :], b_pT2[hp], sm2[:, hp, :], b_sm2[hp])

                def stageA(h):
                    c, base, g = h // 2, (h % 2) * 64, h // 4
                    hp = h % 2
                    lg, b_lg, pp, b_pp, pT, b_pT, sm, b_sm = hbufs(h)
                    for j in range(nch):
                        w_ = min(512, S - j * 512)
                        cs = slice(j * 512, j * 512 + w_)
                        bk_ = (2 if j % 2 == 0 else 0) + (h % 2)
                        P.op("pe", (lambda c, base, g, bk_, w_, cs: lambda e: e.matmul(out=pbanks[bk_][:, 0:w_], lhsT=qTt[base:base + 64, c, :],
                                                                                    rhs=kTd[base:base + 64, g, cs], start=True, stop=True))(c, base, g, bk_, w_, cs),
                             R=[b_qTt, b_kTd], W=[pbuf[bk_]])
                        P.op("dve", (lambda bk_, w_, cs, lg=lg: lambda e: e.scalar_tensor_tensor(out=lg[:, cs], in0=selm1[:, cs], scalar=1e30, in1=pbanks[bk_][:, 0:w_],
                                                                                       op0=ALU.mult, op1=ALU.add))(bk_, w_, cs),
                             R=[b_selm1, pbuf[bk_]], W=[b_lg])
                    if qt == 0:
                        P.op("pool", (lambda h, lg=lg: lambda e: e.tensor_tensor(out=lg[:, 0:128], in0=lg[:, 0:128], in1=Bp[:, h, 128:256], op=ALU.add))(h),
                             R=[b_lg, b_Bp], W=[b_lg])
                    else:
                        P.op("pool", (lambda h, S, lg=lg: lambda e: e.tensor_tensor(out=lg[:, S - 256:S], in0=lg[:, S - 256:S], in1=Bp[:, h, :], op=ALU.add))(h, S),
                             R=[b_lg, b_Bp], W=[b_lg])
                    P.op("dve", (lambda S, lg=lg, sm=sm: lambda e: e.tensor_reduce(out=sm[:, 0:1], in_=lg[:, 0:S], axis=AX.X, op=ALU.max, negate=True))(S),
                         R=[b_lg], W=[b_sm])
                    P.op("act", (lambda S, lg=lg, sm=sm, pp=pp: lambda e: e.activation(out=pp[:, 0:S], in_=lg[:, 0:S], func=AF.Exp, bias=sm[:, 0:1], scale=1.0,
                                                                  accum_out=sm[:, 1:2]))(S), R=[b_lg, b_sm], W=[b_pp, b_sm])
                    P.op("dve", (lambda sm=sm: lambda e: e.reciprocal(out=sm[:, 2:3], in_=sm[:, 1:2]))(), R=[b_sm], W=[b_sm])
                def stageB(h):
                    c, base, g = h // 2, (h % 2) * 64, h // 4
                    hp = h % 2
                    lg, b_lg, pp, b_pp, pT, b_pT, sm, b_sm = hbufs(h)
                    nb = qt + 1
                    for g8 in range((nb + 7) // 8):
                        bk_ = 4 + hp
                        n8 = min(8, nb - g8 * 8)
                        for u in range(n8):
                            kb = g8 * 8 + u
                            P.op("pe", (lambda bk_, u, kb, pp=pp: lambda e: e.transpose(out=pbf(bk_)[:, u * 128:(u + 1) * 128], in_=pp[:, kb * 128:(kb + 1) * 128],
                                                                              identity=identb))(bk_, u, kb), R=[b_pp, b_identb], W=[pbuf[bk_]])
                        copy_rr(pT[:, g8 * 1024:g8 * 1024 + n8 * 128], pbf(bk_)[:, 0:n8 * 128], [pbuf[bk_]], [b_pT])
                    for kb in range(nb):
                        P.op("pe", (lambda h, g, kb, nb, pT=pT: lambda e: e.matmul(out=pbanks[6][:, h * 64:(h + 1) * 64], lhsT=pT[:, kb * 128:(kb + 1) * 128],
                                                                         rhs=vtok[:, kb, g * 64:(g + 1) * 64], start=(h == 0 and kb == 0), stop=(kb == nb - 1),
                                                                         skip_group_check=True))(h, g, kb, nb),
                             R=[b_pT, b_vtok], W=[pbuf[6]])
                    P.op("act", (lambda h, sm=sm: lambda e: e.activation(out=oa[:, h * 64:(h + 1) * 64], in_=pbanks[6][:, h * 64:(h + 1) * 64], func=AF.Copy,
                                                                  scale=sm[:, 2:3]))(h), R=[pbuf[6], b_sm], W=[b_oa])

                stageA(0)
                for h in range(8):
                    if h + 1 < 8:
                        stageA(h + 1)
                    stageB(h)

                if stop == "dsa":
                    return
                def ck(k):
                    if gstop == k:
                        P.mute = True
                for c in range(2):
                    for kc in range(8):
                        P.op("pe", (lambda c, kc, tq: lambda e: e.matmul(out=pbanks[0][:, c * 128:(c + 1) * 128], lhsT=Wbq[:, kc, c * 128:(c + 1) * 128],
                                                                      rhs=hT[:, kc, tq], start=(kc == 0), stop=(kc == 7)))(c, kc, tq), R=[b_Wbq, bh], W=[pbuf[0]])
                for c in range(2):
                    for kc in range(8):
                        P.op("pe", (lambda c, kc, tq: lambda e: e.matmul(out=pbanks[0][:, 256 + c * 128:256 + (c + 1) * 128], lhsT=Wbk[:, kc, c * 128:(c + 1) * 128],
                                                                      rhs=hT[:, kc, tq], start=(kc == 0), stop=(kc == 7)))(c, kc, tq), R=[b_Wbk, bh], W=[pbuf[0]])
                for kc in range(8):
                    P.op("pe", (lambda kc, tq: lambda e: e.matmul(out=pbanks[1][:, 0:256], lhsT=hT[:, kc, tq], rhs=Wbk[:, kc, :],
                                                               start=(kc == 0), stop=(kc == 7)))(kc, tq), R=[b_Wbk, bh], W=[pbuf[1]])
                for kc in range(8):
                    P.op("pe", (lambda kc, tq: lambda e: e.matmul(out=pbanks[1][0:16, 256:384], lhsT=Wbg[:, kc, :], rhs=hT[:, kc, tq],
                                                               start=(kc == 0), stop=(kc == 7)))(kc, tq), R=[b_Wbg, bh], W=[pbuf[1]])
                for kc in range(8):
                    P.op("pe", (lambda kc, tq: lambda e: e.matmul(out=pbanks[2], lhsT=hT[:, kc, tq], rhs=Wbv[:, kc, :],
                                                               start=(kc == 0), stop=(kc == 7)))(kc, tq), R=[b_Wbv, bh], W=[pbuf[2]])
                for kc in range(8):
                    P.op("pe", (lambda kc, tq: lambda e: e.matmul(out=pbanks[3], lhsT=hT[:, kc, tq], rhs=Wbr[:, kc, :],
                                                               start=(kc == 0), stop=(kc == 7)))(kc, tq), R=[b_Wbr, bh], W=[pbuf[3]])
                ck(1)
                P.op("act", lambda e: e.copy(out=bg1[0:16, :], in_=pbanks[1][0:16, 256:384]), R=[pbuf[1]], W=[b_bg1])
                P.op("act", lambda e: e.copy(out=vt, in_=pbanks[2]), R=[pbuf[2]], W=[b_vt])
                P.op("act", lambda e: e.activation(out=sbr, in_=pbanks[3], func=AF.Silu), R=[pbuf[3]], W=[b_sbr])
                ck(2)
                P.op("pe", lambda e: e.matmul(out=pbanks[4][:, 0:256], lhsT=bg1[0:17, :], rhs=G2[0:17, :], start=True, stop=True),
                     R=[b_bg1, b_G2], W=[pbuf[4]])
                P.op("act", lambda e: e.activation(out=lap, in_=pbanks[4][:, 0:256], func=AF.Exp, scale=-1.0), R=[pbuf[4]], W=[b_lap])
                P.op("act", lambda e: e.activation(out=lap, in_=lap, func=AF.Ln, bias=1.0, scale=1.0), R=[b_lap], W=[b_lap])
                ck(3)
                for c in range(2):
                    P.op("pe", (lambda c: lambda e: e.matmul(out=pbanks[5][:, c * 128:(c + 1) * 128], lhsT=lap[:, c * 128:(c + 1) * 128], rhs=TRI,
                                                             start=True, stop=True))(c), R=[b_lap, b_TRI], W=[pbuf[5]])
                P.op("pe", lambda e: e.matmul(out=pbanks[5][:, 256:512], lhsT=TRI2, rhs=lap, start=True, stop=True), R=[b_lap, b_TRI2], W=[pbuf[5]])
                ck(4)
                P.op("act", lambda e: e.activation(out=EcT, in_=pbanks[5][:, 0:256].rearrange("p (a b) -> p a b", b=128), func=AF.Exp), R=[pbuf[5]], W=[b_EcT])
                P.op("act", lambda e: e.activation(out=EnT, in_=pbanks[5][:, 0:256].rearrange("p (a b) -> p a b", b=128), func=AF.Exp, scale=-1.0),
                     R=[pbuf[5]], W=[b_EnT])
                P.op("act", lambda e: e.activation(out=Er, in_=pbanks[5][:, 256:512], func=AF.Exp), R=[pbuf[5]], W=[b_Er])
                P.op("dve", lambda e: e.scalar_tensor_tensor(out=qdT, in0=pbanks[0][:, 0:256].rearrange("p (a b) -> p a b", b=128), scalar=0.125, in1=EcT,
                                                             op0=ALU.mult, op1=ALU.mult), R=[pbuf[0], b_EcT], W=[b_qdT])
                P.op("dve", lambda e: e.tensor_tensor(out=kiT, in0=pbanks[0][:, 256:512].rearrange("p (a b) -> p a b", b=128), in1=EnT, op=ALU.mult),
                     R=[pbuf[0], b_EnT], W=[b_kiT])
                P.op("dve", lambda e: e.tensor_tensor(out=kend, in0=pbanks[1][:, 0:256], in1=Er, op=ALU.mult), R=[pbuf[1], b_Er], W=[b_kend])
                ck(5)
                for h in range(4):
                    c, base = h // 2, (h % 2) * 64
                    sb_ = 4 if h % 2 == 0 else 3
                    P.op("pe", (lambda h, c, base, sb_: lambda e: e.matmul(out=pbanks[sb_][:, c * 128:(c + 1) * 128], lhsT=kiT[base:base + 64, c, :],
                                                                          rhs=qdT[base:base + 64, c, :], start=True, stop=True))(h, c, base, sb_),
                         R=[b_kiT, b_qdT], W=[pbuf[sb_]])
                ck(55)
                for h in range(4):
                    sb_ = 4 if h % 2 == 0 else 3
                    P.op("dve", (lambda h, sb_: lambda e: e.tensor_tensor(out=scT[:, h, :], in0=pbanks[sb_][:, (h // 2) * 128:(h // 2 + 1) * 128],
                                                                         in1=MASKT, op=ALU.mult))(h, sb_), R=[pbuf[sb_], b_MASKT], W=[b_scT])
                ck(6)
                for h in range(4):
                    ob_ = 7 if h % 2 == 0 else 6
                    P.op("pe", (lambda h, ob_: lambda e: e.matmul(out=pbanks[ob_][:, (h // 2) * 128:(h // 2 + 1) * 128], lhsT=scT[:, h, :], rhs=vt[:, h * 128:(h + 1) * 128],
                                                                  start=(h < 2), stop=False, skip_group_check=True))(h, ob_), R=[b_scT, b_vt], W=[pbuf[ob_]])
                ck(7)
                for u in range(2):
                    us = slice(u * 64, (u + 1) * 64)
                    for h in range(4):
                        c, base = h // 2, (h % 2) * 64
                        ob_ = 7 if h % 2 == 0 else 6
                        P.op("pe", (lambda h, c, base, us, ob_: lambda e: e.matmul(out=pbanks[ob_][us, c * 128:(c + 1) * 128], lhsT=qdT[base:base + 64, c, us],
                                                                                  rhs=Sb[base:base + 64, c, :], start=False, stop=True,
                                                                                  skip_group_check=True))(h, c, base, us, ob_), R=[b_qdT, b_Sb], W=[pbuf[ob_]])
                    for h in range(4):
                        c, base = h // 2, (h % 2) * 64
                        P.op("pe", (lambda h, c, base, us: lambda e: e.matmul(out=pbanks[5][base:base + 64, c * 128:(c + 1) * 128], lhsT=kend[us, h * 64:(h + 1) * 64],
                                                                             rhs=vt[us, h * 128:(h + 1) * 128], start=True, stop=True))(h, c, base, us),
                             R=[b_kend, b_vt], W=[pbuf[5]])
                    for c in range(2):
                        P.op("dve", (lambda c, u: lambda e: e.scalar_tensor_tensor(out=Sf[:, c, :], in0=Sf[:, c, :], scalar=EcT[:, c, u * 64 + 63:u * 64 + 64],
                                                                                   in1=pbanks[5][:, c * 128:(c + 1) * 128], op0=ALU.mult, op1=ALU.add))(c, u),
                             R=[b_Sf, b_EcT, pbuf[5]], W=[b_Sf])
                    P.op("act", lambda e: e.copy(out=Sb, in_=Sf), R=[b_Sf], W=[b_Sb])
                ck(8)
                for h in range(4):
                    ob_ = 7 if h % 2 == 0 else 6
                    P.op("dve", (lambda h, ob_: lambda e: e.bn_stats(out=gst[:, h, :], in_=pbanks[ob_][:, (h // 2) * 128:(h // 2 + 1) * 128]))(h, ob_), R=[pbuf[ob_]], W=[b_gst])
                for h in range(4):
                    P.op("dve", (lambda h: lambda e: e.bn_aggr(out=gmv[:, h, :], in_=gst[:, h, :]))(h), R=[b_gst], W=[b_gmv])
                P.op("act", lambda e: e.activation(out=gsd[:, :, 0], in_=gmv[:, :, 1], func=AF.Sqrt, bias=eps_t[:, 0:1], scale=1.0),
                     R=[b_gmv, b_eps], W=[b_gsd])
                P.op("dve", lambda e: e.reciprocal(out=gsd[:, :, 1], in_=gsd[:, :, 0]), R=[b_gsd], W=[b_gsd])
                for h in range(4):
                    ob_ = 7 if h % 2 == 0 else 6
                    P.op("dve", (lambda h, ob_: lambda e: e.tensor_scalar(out=on[:, h * 128:(h + 1) * 128], in0=pbanks[ob_][:, (h // 2) * 128:(h // 2 + 1) * 128],
                                                                         scalar1=gmv[:, h, 0:1], scalar2=gsd[:, h, 1:2], op0=ALU.subtract, op1=ALU.mult))(h, ob_),
                         R=[pbuf[ob_], b_gmv, b_gsd], W=[b_on])
                P.op("pool", lambda e: e.tensor_tensor(out=on, in0=on, in1=NG, op=ALU.mult), R=[b_on, b_NG], W=[b_on])
                P.op("pool", lambda e: e.tensor_tensor(out=oa[:, 512:1024], in0=on, in1=sbr, op=ALU.mult), R=[b_on, b_sbr], W=[b_oa])

                P.mute = False
                if stop == "gla":
                    return
                for kc in range(8):
                    P.op("pe", (lambda kc: lambda e: e.transpose(out=pbf(4)[:, kc * 128:(kc + 1) * 128], in_=oa[:, kc * 128:(kc + 1) * 128], identity=identb))(kc),
                         R=[b_oa, b_identb], W=[pbuf[4]])
                copy_rr(oT, pbf(4).rearrange("p (a b) -> p a b", b=128), [pbuf[4]], [b_oT])
                for hh in range(2):
                    for kc in range(8):
                        P.op("pe", (lambda hh, kc: lambda e: e.matmul(out=pbanks[2 + hh], lhsT=oT[:, kc, :], rhs=Wo[:, kc, hh * 512:(hh + 1) * 512],
                                                                    start=(kc == 0), stop=(kc == 7)))(hh, kc), R=[b_oT, b_Wo], W=[pbuf[2 + hh]])
                par = 0
                for hh in range(2):
                    P.op("dve", (lambda hh, par: lambda e: e.scalar_tensor_tensor(out=rr_[:, hh * 512:(hh + 1) * 512], in0=xs[:, par, hh * 512:(hh + 1) * 512],
                                                                               scalar=ALPHA, in1=pbanks[2 + hh], op0=ALU.mult, op1=ALU.add))(hh, par),
                         R=[b_xs[par], pbuf[2 + hh]], W=[b_rr])
                m_ln = A.mark()
                layer_norm_tile(rr_, b_rr, LG, LB, b_LG, rr_, b_rr, b_LB)
                A.release(m_ln)
                gi = s * NT + qt
                P.dma(hres[gi * 128:(gi + 1) * 128, :], rr_, R=[b_rr], W=[hres_b[gi]])

            pre(0)
            topk(0)
            for qt in range(nqt):
                if qt + 1 < nqt:
                    pre(qt + 1)
                    P.fork(2)
                    P.stream(0)
                    topk(qt + 1)
                    P.stream(1)
                    rest(qt)
                    n0, n1 = len(P.streams[0]), len(P.streams[1])
                    P.join([1, max(1, n1 // max(1, n0))])
                else:
                    rest(qt)

        A.release(m_l0)


        xs_b = Buf("xs_dram")
        ys_b = Buf("ys_dram")

        _oob_regs = {}

        def oobkw(e, mx):
            if os.environ.get("OOB", "1") != "1":
                return {}
            if mx not in _oob_regs:
                _oob_regs[mx] = e.to_reg(mx)
            return dict(bounds_check=_oob_regs[mx], oob_is_err=False)

        def moe_stage(layer, final):
            m_moe = A.mark()
            NG = NG_TOK
            LGf, b_LGf = A.alloc([128, 1024], F32, name="LGf")
            LBf, b_LBf = A.alloc([128, 1024], F32, name="LBf")
            P.dma(LGf, ln_ffn_g[layer].partition_broadcast(128), W=[b_LGf])
            P.dma(LBf, ln_ffn_b[layer].partition_broadcast(128), W=[b_LBf])
            Wr, b_Wr = A.alloc([128, 8, 20], F32, name="Wr")
            P.dma(Wr[:, :, 0:4], moe_r_coarse[layer].rearrange("(kc p) n -> p kc n", p=128), W=[b_Wr])
            for g in range(4):
                P.dma(Wr[:, :, 4 + 4 * g:8 + 4 * g], moe_r_fine[layer, g].rearrange("(kc p) n -> p kc n", p=128), W=[b_Wr])
            RBb, b_RBb = A.alloc([128, 20], F32, name="RBb")
            P.dma(RBb[:, 0:4], moe_rb_coarse[layer].partition_broadcast(128), W=[b_RBb])
            P.dma(RBb[:, 4:20], moe_rb_fine[layer].rearrange("a b -> (a b)").partition_broadcast(128), W=[b_RBb])
            OH, b_OH = A.alloc([128, NG, 32], BF16, name="OH")
            GW, b_GW = A.alloc([128, NG, 2], F32, name="GW")
            POSi, b_POSi = A.alloc([128, NG * 2], I32, name="POSi")
            IDXG, b_IDXG = A.alloc([128, NTILE * 2], I32, name="IDXG")
            IDXD, b_IDXD = A.alloc([128, NTILE * 4], I32, name="IDXD")
            m_r = A.mark()
            ht, b_ht = A.alloc([128, 2, 1024], F32, nbufs=2, name="ht")
            h1T, b_h1T = A.alloc([128, 8, 128], F32, name="h1T")
            LGT, b_LGT = A.alloc([128, NG, 20], F32, name="LGT")
            rmx, b_rmx = A.alloc([128, NG], F32, name="rmx")
            dg, b_dg = A.alloc([128, NG, 4], F32, name="dg")
            eg, b_eg = A.alloc([128, NG, 4], F32, name="eg")
            gwc, b_gwc = A.alloc([128, NG], F32, name="gwc")
            ohgB, b_ohgB = A.alloc([128, NG, 4], F32, name="ohgB")
            tmpB, b_tmpB = A.alloc([128, NG, 4, 4], F32, name="tmpB")
            flsB, b_flsB = A.alloc([128, NG, 4], F32, name="flsB")
            fl2B, b_fl2B = A.alloc([128, NG, 4], F32, name="fl2B")
            m1B, b_m1B = A.alloc([128, NG], F32, name="m1B")
            m2B, b_m2B = A.alloc([128, NG], F32, name="m2B")
            ohB, b_ohB = A.alloc([128, 2, NG, 4], F32, name="ohB")
            lgt, b_lgt = A.alloc([128, 20], F32, name="lgt")
            sm_, b_sm_ = A.alloc([128, 16], F32, name="rsm")
            ohg, b_ohg = A.alloc([128, 4], F32, name="ohg")
            tmp16, b_tmp16 = A.alloc([128, 4, 4], F32, name="tmp16")
            fls, b_fls = A.alloc([128, 4], F32, name="fls")
            fl2, b_fl2 = A.alloc([128, 4], F32, name="fl2")
            oh12, b_oh12 = A.alloc([128, 2, 4], F32, name="oh12")
            zt, b_zt = A.alloc([128, 4, 1024], BF16, name="zt")
            P.op("pool", lambda e: e.memset(zt, 0.0), W=[b_zt])
            for gi in range(NG):
                par = gi % 2
                if gi < NTILE:
                    P.dma(xs_dram[gi * 512:(gi + 1) * 512, :].rearrange("(p a) d -> p a d", a=4), zt, R=[b_zt], W=[xs_b])
                P.dma(ht[:, par, :], hres[gi * 128:(gi + 1) * 128, :], R=[hres_b[gi]], W=[b_ht[par]])
                for g in range(2):
                    bk_ = g
                    for c in range(4):
                        kc = 4 * g + c
                        P.op("pe", (lambda par, kc, c, bk_: lambda e: e.transpose(out=pbanks[bk_][:, c * 128:(c + 1) * 128],
                                                                                in_=ht[:, par, kc * 128:(kc + 1) * 128], identity=ident))(par, kc, c, bk_),
                             R=[b_ht[par], b_ident], W=[pbuf[bk_]])
                    copy_rr(h1T[:, 4 * g:4 * g + 4, :], pbanks[bk_].rearrange("p (a b) -> p a b", b=128), [pbuf[bk_]], [b_h1T])
                for kc in range(8):
                    P.op("pe", (lambda kc: lambda e: e.matmul(out=pbanks[2][:, 0:20], lhsT=h1T[:, kc, :], rhs=Wr[:, kc, :], start=(kc == 0), stop=(kc == 7)))(kc),
                         R=[b_h1T, b_Wr], W=[pbuf[2]])
                P.op("dve", (lambda gi: lambda e: e.tensor_tensor(out=LGT[:, gi, :], in0=pbanks[2][:, 0:20], in1=RBb, op=ALU.add))(gi), R=[pbuf[2], b_RBb], W=[b_LGT])
            B3 = lambda ap2, n: ap2.unsqueeze(2).to_broadcast([128, NG, n])
            gl = LGT[:, :, 0:4]
            P.op("dve", lambda e: e.tensor_reduce(out=rmx, in_=gl, axis=AX.X, op=ALU.max), R=[b_LGT], W=[b_rmx])
            P.op("dve", lambda e: e.tensor_tensor(out=dg, in0=gl, in1=B3(rmx, 4), op=ALU.subtract), R=[b_LGT, b_rmx], W=[b_dg])
            P.op("act", lambda e: e.activation(out=eg, in_=dg, func=AF.Exp), R=[b_dg], W=[b_eg])
            P.op("dve", lambda e: e.tensor_reduce(out=gwc, in_=eg, axis=AX.X, op=ALU.add), R=[b_eg], W=[b_gwc])
            P.op("dve", lambda e: e.reciprocal(out=gwc, in_=gwc), R=[b_gwc], W=[b_gwc])
            P.op("dve", lambda e: e.tensor_scalar(out=ohgB, in0=dg, scalar1=0.0, scalar2=None, op0=ALU.is_ge), R=[b_dg], W=[b_ohgB])
            P.op("dve", lambda e: e.tensor_tensor(out=tmpB, in0=LGT[:, :, 4:20].rearrange("p t (g e) -> p t g e", e=4),
                                                  in1=ohgB.unsqueeze(3).to_broadcast([128, NG, 4, 4]), op=ALU.mult), R=[b_LGT, b_ohgB], W=[b_tmpB])
            P.op("dve", lambda e: e.tensor_reduce(out=flsB, in_=tmpB.rearrange("p t g e -> p t e g"), axis=AX.X, op=ALU.add), R=[b_tmpB], W=[b_flsB])
            P.op("dve", lambda e: e.tensor_reduce(out=m1B, in_=flsB, axis=AX.X, op=ALU.max), R=[b_flsB], W=[b_m1B])
            P.op("dve", lambda e: e.tensor_tensor(out=ohB[:, 0], in0=flsB, in1=B3(m1B, 4), op=ALU.is_ge), R=[b_flsB, b_m1B], W=[b_ohB])
            P.op("dve", lambda e: e.scalar_tensor_tensor(out=fl2B, in0=ohB[:, 0], scalar=NEG, in1=flsB, op0=ALU.mult, op1=ALU.add), R=[b_ohB, b_flsB], W=[b_fl2B])
            P.op("dve", lambda e: e.tensor_reduce(out=m2B, in_=fl2B, axis=AX.X, op=ALU.max), R=[b_fl2B], W=[b_m2B])
            P.op("dve", lambda e: e.tensor_tensor(out=ohB[:, 1], in0=fl2B, in1=B3(m2B, 4), op=ALU.is_ge), R=[b_fl2B, b_m2B], W=[b_ohB])
            P.op("dve", lambda e: e.tensor_tensor(out=m2B, in0=m2B, in1=m1B, op=ALU.subtract), R=[b_m2B, b_m1B], W=[b_m2B])
            P.op("act", lambda e: e.activation(out=m2B, in_=m2B, func=AF.Exp), R=[b_m2B], W=[b_m2B])
            P.op("dve", lambda e: e.tensor_scalar(out=m1B, in0=m2B, scalar1=1.0, scalar2=None, op0=ALU.add), R=[b_m2B], W=[b_m1B])
            P.op("dve", lambda e: e.reciprocal(out=m1B, in_=m1B), R=[b_m1B], W=[b_m1B])
            P.op("dve", lambda e: e.tensor_tensor(out=GW[:, :, 0], in0=m1B, in1=gwc, op=ALU.mult), R=[b_m1B, b_gwc], W=[b_GW])
            P.op("dve", lambda e: e.tensor_tensor(out=m2B, in0=m2B, in1=m1B, op=ALU.mult), R=[b_m2B, b_m1B], W=[b_m2B])
            P.op("dve", lambda e: e.tensor_tensor(out=GW[:, :, 1], in0=m2B, in1=gwc, op=ALU.mult), R=[b_m2B, b_gwc], W=[b_GW])
            for k in range(2):
                P.op("dve", (lambda k: lambda e: e.tensor_tensor(out=OH[:, :, k * 16:(k + 1) * 16].rearrange("p t (g e) -> p t g e", e=4),
                                                                in0=ohgB.unsqueeze(3).to_broadcast([128, NG, 4, 4]),
                                                                in1=ohB[:, k].unsqueeze(2).to_broadcast([128, NG, 4, 4]), op=ALU.mult))(k),
                     R=[b_ohgB, b_ohB], W=[b_OH])
            STRI, b_STRI = A.alloc([128, 128], BF16, name="STRI")
            ONESM, b_ONESM = A.alloc([128, 128], BF16, name="ONESM")
            P.op("pool", lambda e: e.memset(ONESM, 1.0), W=[b_ONESM])
            P.op("pool", lambda e: e.memset(STRI, 1.0), W=[b_STRI])
            P.op("pool", lambda e: e.affine_select(out=STRI, in_=STRI, pattern=[[1, 128]], compare_op=ALU.is_ge, fill=0.0, base=-1,
                                                   channel_multiplier=-1), R=[b_STRI], W=[b_STRI])
            PC, b_PC = A.alloc([128, NG, 64], F32, name="PC")
            for gi in range(NG):
                bk_ = gi % 2
                P.op("pe", (lambda gi, bk_: lambda e: e.matmul(out=pbanks[bk_][:, 0:32], lhsT=STRI, rhs=OH[:, gi, :], start=True, stop=True))(gi, bk_),
                     R=[b_STRI, b_OH], W=[pbuf[bk_]])
                P.op("pe", (lambda gi, bk_: lambda e: e.matmul(out=pbanks[bk_][:, 32:64], lhsT=ONESM, rhs=OH[:, gi, :], start=True, stop=True))(gi, bk_),
                     R=[b_ONESM, b_OH], W=[pbuf[bk_]])
                copy_rr(PC[:, gi, :], pbanks[bk_][:, 0:64], [pbuf[bk_]], [b_PC])
            BASE, b_BASE = A.alloc([128, NG * 2, 16], F32, name="BASE")
            run, b_run = A.alloc([128, 16], F32, name="run")
            P.op("pool", lambda e: e.memset(run, 0.0), W=[b_run])
            for gi in range(NG):
                for k in range(2):
                    P.op("dve", (lambda gi, k: lambda e: e.tensor_copy(out=BASE[:, gi * 2 + k, :], in_=run))(gi, k), R=[b_run], W=[b_BASE])
                    P.op("dve", (lambda gi, k: lambda e: e.tensor_tensor(out=run, in0=run, in1=PC[:, gi, 32 + k * 16:48 + k * 16], op=ALU.add))(gi, k),
                         R=[b_run, b_PC], W=[b_run])
            npad, b_npad = A.alloc([128, 16], F32, name="npad")
            cum, b_cum = A.alloc([128, 16], F32, name="cum")
            ones16, b_ones16 = A.alloc([128, 16], F32, name="ones16")
            offs, b_offs = A.alloc([128, 16], F32, name="offs")
            P.op("pool", lambda e: e.memset(ones16, 1.0), W=[b_ones16])
            P.op("dve", lambda e: e.tensor_scalar(out=npad, in0=run, scalar1=0.0, scalar2=512.0, op0=ALU.is_gt, op1=ALU.mult), R=[b_run], W=[b_npad])
            for m_ in range(1, 8):
                P.op("dve", (lambda m_: lambda e: e.tensor_scalar(out=cum, in0=run, scalar1=512.0 * m_, scalar2=512.0, op0=ALU.is_gt, op1=ALU.mult))(m_),
                     R=[b_run], W=[b_cum])
                P.op("dve", lambda e: e.tensor_tensor(out=npad, in0=npad, in1=cum, op=ALU.add), R=[b_npad, b_cum], W=[b_npad])
            P.op("dve", lambda e: e.tensor_tensor_scan(out=cum, data0=ones16, data1=npad, initial=0.0, op0=ALU.mult, op1=ALU.add),
                 R=[b_ones16, b_npad], W=[b_cum])
            P.op("dve", lambda e: e.tensor_tensor(out=offs, in0=cum, in1=npad, op=ALU.subtract), R=[b_cum, b_npad], W=[b_offs])
            P.op("dve", lambda e: e.tensor_tensor(out=BASE, in0=BASE, in1=offs.unsqueeze(1).to_broadcast([128, NG * 2, 16]), op=ALU.add),
                 R=[b_BASE, b_offs], W=[b_BASE])
            T1, b_T1 = A.alloc([128, NG, 32], F32, name="T1")
            P.op("dve", lambda e: e.tensor_tensor(out=T1, in0=PC[:, :, 0:32], in1=BASE.rearrange("p (a k) e -> p a (k e)", k=2), op=ALU.add),
                 R=[b_PC, b_BASE], W=[b_T1])
            P.op("dve", lambda e: e.tensor_tensor(out=T1, in0=T1, in1=OH, op=ALU.mult), R=[b_T1, b_OH], W=[b_T1])
            POSf, b_POSf = A.alloc([128, NG * 2], F32, name="POSf")
            P.op("dve", lambda e: e.tensor_reduce(out=POSf, in_=T1.rearrange("p a (k e) -> p (a k) e", k=2), axis=AX.X, op=ALU.add), R=[b_T1], W=[b_POSf])
            P.op("dve", lambda e: e.tensor_copy(out=POSi, in_=POSf), R=[b_POSf], W=[b_POSi])
            EJ, b_EJ = A.alloc([128, NTILE], F32, name="EJ")
            for j in range(NTILE):
                P.op("dve", (lambda j: lambda e: e.tensor_scalar(out=ones16, in0=cum, scalar1=float(j * 512) + 0.5, scalar2=None, op0=ALU.is_le,
                                                                 op1=ALU.add, accum_out=EJ[:, j:j + 1]))(j), R=[b_cum], W=[b_ones16, b_EJ])
            EJu, b_EJu = A.alloc([128, NTILE], F32, name="EJu")
            P.op("dve", lambda e: e.tensor_scalar(out=EJu, in0=EJ, scalar1=15.5, scalar2=1.0e7, op0=ALU.is_ge, op1=ALU.mult), R=[b_EJ], W=[b_EJu])
            P.op("dve", lambda e: e.tensor_scalar(out=EJ, in0=EJ, scalar1=15.0, scalar2=float(layer * 16), op0=ALU.min, op1=ALU.add), R=[b_EJ], W=[b_EJ])
            P2, b_P2 = A.alloc([128, 2], F32, name="P2")
            P4, b_P4 = A.alloc([128, 4], F32, name="P4")
            P.op("pool", lambda e: e.iota(out=P2, pattern=[[1, 2]], base=0, channel_multiplier=2, allow_small_or_imprecise_dtypes=True), W=[b_P2])
            P.op("pool", lambda e: e.iota(out=P4, pattern=[[128, 4]], base=0, channel_multiplier=1, allow_small_or_imprecise_dtypes=True), W=[b_P4])
            IGf, b_IGf = A.alloc([128, NTILE, 2], F32, name="IGf")
            IDf, b_IDf = A.alloc([128, NTILE, 4], F32, name="IDf")
            P.op("dve", lambda e: e.tensor_scalar(out=IGf, in0=EJ.unsqueeze(2).to_broadcast([128, NTILE, 2]), scalar1=256.0, scalar2=None, op0=ALU.mult),
                 R=[b_EJ], W=[b_IGf])
            P.op("dve", lambda e: e.tensor_tensor(out=IGf, in0=IGf, in1=P2.unsqueeze(1).to_broadcast([128, NTILE, 2]), op=ALU.add), R=[b_IGf, b_P2], W=[b_IGf])
            P.op("dve", lambda e: e.tensor_scalar(out=IDf, in0=EJ.unsqueeze(2).to_broadcast([128, NTILE, 4]), scalar1=512.0, scalar2=None, op0=ALU.mult),
                 R=[b_EJ], W=[b_IDf])
            P.op("dve", lambda e: e.tensor_tensor(out=IDf, in0=IDf, in1=P4.unsqueeze(1).to_broadcast([128, NTILE, 4]), op=ALU.add), R=[b_IDf, b_P4], W=[b_IDf])
            if os.environ.get("OOB", "1") == "1":
                P.op("dve", lambda e: e.tensor_tensor(out=IGf, in0=IGf, in1=EJu.unsqueeze(2).to_broadcast([128, NTILE, 2]), op=ALU.add), R=[b_IGf, b_EJu], W=[b_IGf])
                P.op("dve", lambda e: e.tensor_tensor(out=IDf, in0=IDf, in1=EJu.unsqueeze(2).to_broadcast([128, NTILE, 4]), op=ALU.add), R=[b_IDf, b_EJu], W=[b_IDf])
            P.op("dve", lambda e: e.tensor_copy(out=IDXG, in_=IGf.rearrange("p a b -> p (a b)")), R=[b_IGf], W=[b_IDXG])
            P.op("dve", lambda e: e.tensor_copy(out=IDXD, in_=IDf.rearrange("p a b -> p (a b)")), R=[b_IDf], W=[b_IDXD])
            if debug == "moe_route":
                P.dma(y[0, 0:128, 0:64], POSf, R=[b_POSf], W=[y_b])
                P.dma(y[0, 0:128, 64:128], GW.rearrange("p a b -> p (a b)"), R=[b_GW], W=[y_b])
                P.dma(y[0, 0:128, 128:128 + NTILE], EJ, R=[b_EJ], W=[y_b])
                P.dma(y[0, 0:128, 256:272], cum, R=[b_cum], W=[y_b])
            hb, b_hb = A.alloc([128, 2, 1024], BF16, nbufs=2, name="hb")
            for gi in range(NG):
                par = gi % 2
                P.dma(ht[:, par, :], hres[gi * 128:(gi + 1) * 128, :], R=[hres_b[gi]], W=[b_ht[par]])
                P.op("act", (lambda par: lambda e: e.copy(out=hb[:, par, :], in_=ht[:, par, :]))(par), R=[b_ht[par]], W=[b_hb[par]])
                for k in range(2):
                    P.op("pool", (lambda gi, k, par: lambda e: e.indirect_dma_start(
                        out=xs_dram[:, :], out_offset=bass.IndirectOffsetOnAxis(ap=POSi[:, gi * 2 + k:gi * 2 + k + 1], axis=0),
                        in_=hb[:, par, :], in_offset=None))(gi, k, par), R=[b_hb[par], b_POSi], W=[xs_b], dma=True)
            A.release(m_r)
            wg, b_wg = A.alloc([128, 2, 8 * 512], BF16, nbufs=2, name="wg")
            wu, b_wu = A.alloc([128, 2, 8 * 512], BF16, nbufs=2, name="wu")
            wd, b_wd = A.alloc([128, 2, 4 * 1024], BF16, nbufs=2, name="wd")
            xsb, b_xsb = A.alloc([128, 2, 4 * 1024], BF16, nbufs=2, name="xsb")
            xT2, b_xT2 = A.alloc([128, 2, 8, 512], BF16, nbufs=2, name="xT")
            hid, b_hid = A.alloc([128, 4, 512], BF16, name="hid")
            sg, b_sg = A.alloc([128, 2, 512], F32, nbufs=2, name="sg")
            ysb, b_ysb = A.alloc([128, 4, 1024], F32, name="ysb")
            PC_ = os.environ.get("PRECAST", "1") == "1"
            WGv = (wgb if PC_ else moe_w_gate).rearrange("l g e (p two r) n -> (l g e p two) (r n)", p=128, two=2)
            WUv = (wub if PC_ else moe_w_up).rearrange("l g e (p two r) n -> (l g e p two) (r n)", p=128, two=2)
            WDv = (wdb if PC_ else moe_w_down).rearrange("l g e f n -> (l g e f) n")
            wc_dep = list(wcast_b[layer]) if PC_ else []
            def mxload(j):
                par = j % 2
                P.dma(xsb[:, par, :].rearrange("p (a d) -> p a d", a=4), xs_dram[j * 512:(j + 1) * 512, :].rearrange("(a p) d -> p a d", p=128),
                      R=[xs_b], W=[b_xsb[par]])

            def mfront(j):
                par = j % 2
                xT, b_xT = xT2[:, par], b_xT2[par]
                for hf in range(2):
                    P.op("pool", (lambda j, hf, par: lambda e: e.indirect_dma_start(
                        out=wg[:, par, hf * 2048:(hf + 1) * 2048], out_offset=None, in_=WGv,
                        in_offset=bass.IndirectOffsetOnAxis(ap=IDXG[:, j * 2 + hf:j * 2 + hf + 1], axis=0), **oobkw(e, 2 * 16 * 256 - 1)))(j, hf, par),
                        R=[b_IDXG] + wc_dep, W=[b_wg[par]], dma=True)
                    P.op("pool", (lambda j, hf, par: lambda e: e.indirect_dma_start(
                        out=wu[:, par, hf * 2048:(hf + 1) * 2048], out_offset=None, in_=WUv,
                        in_offset=bass.IndirectOffsetOnAxis(ap=IDXG[:, j * 2 + hf:j * 2 + hf + 1], axis=0), **oobkw(e, 2 * 16 * 256 - 1)))(j, hf, par),
                        R=[b_IDXG] + wc_dep, W=[b_wu[par]], dma=True)
                for fc in range(4):
                    P.op("pool", (lambda j, fc, par: lambda e: e.indirect_dma_start(
                        out=wd[:, par, fc * 1024:(fc + 1) * 1024], out_offset=None, in_=WDv,
                        in_offset=bass.IndirectOffsetOnAxis(ap=IDXD[:, j * 4 + fc:j * 4 + fc + 1], axis=0), **oobkw(e, 2 * 16 * 512 - 1)))(j, fc, par),
                        R=[b_IDXD] + wc_dep, W=[b_wd[par]], dma=True)
                for st_ in range(4):
                    bk_ = st_ % 2
                    src = xsb[:, par, st_ * 1024:(st_ + 1) * 1024].rearrange("s (p k) -> s k p", k=8)
                    for kc in range(8):
                        P.op("pe", (lambda src, kc, bk_: lambda e: e.transpose(out=pbf(bk_)[:, kc * 128:(kc + 1) * 128], in_=src[:, kc, :], identity=identb))(src, kc, bk_),
                             R=[b_xsb[par], b_identb], W=[pbuf[bk_]])
                    copy_rr(xT[:, :, st_ * 128:(st_ + 1) * 128], pbf(bk_).rearrange("p (a b) -> p a b", b=128), [pbuf[bk_]], [b_xT])
            def mback(j):
                par = j % 2
                xT, b_xT = xT2[:, par], b_xT2[par]
                wgv = wg[:, par, :].rearrange("p (k n) -> p k n", n=512)
                wuv = wu[:, par, :].rearrange("p (k n) -> p k n", n=512)
                wdv = wd[:, par, :].rearrange("p (k n) -> p k n", n=1024)
                for fc in range(4):
                    bg_, bu_ = 2 + (fc % 2) * 2, 3 + (fc % 2) * 2
                    for kc in range(8):
                        P.op("pe", (lambda fc, kc, bg_, wgv: lambda e: e.matmul(out=pbanks[bg_], lhsT=wgv[:, kc, fc * 128:(fc + 1) * 128], rhs=xT[:, kc, :],
                                                                              start=(kc == 0), stop=(kc == 7)))(fc, kc, bg_, wgv), R=[b_wg[par], b_xT], W=[pbuf[bg_]])
                    for kc in range(8):
                        P.op("pe", (lambda fc, kc, bu_, wuv: lambda e: e.matmul(out=pbanks[bu_], lhsT=wuv[:, kc, fc * 128:(fc + 1) * 128], rhs=xT[:, kc, :],
                                                                              start=(kc == 0), stop=(kc == 7)))(fc, kc, bu_, wuv), R=[b_wu[par], b_xT], W=[pbuf[bu_]])
                    sp_ = fc % 2
                    P.op("act", (lambda bg_, sp_: lambda e: e.activation(out=sg[:, sp_, :], in_=pbanks[bg_], func=AF.Silu))(bg_, sp_), R=[pbuf[bg_]], W=[b_sg[sp_]])
                    P.op("dve", (lambda fc, bu_, sp_: lambda e: e.tensor_tensor(out=hid[:, fc, :], in0=pbanks[bu_], in1=sg[:, sp_, :], op=ALU.mult))(fc, bu_, sp_),
                         R=[pbuf[bu_], b_sg[sp_]], W=[b_hid])
                for st_ in range(4):
                    for hh in range(2):
                        bk_ = 6 + hh
                        for fc in range(4):
                            P.op("pe", (lambda st_, hh, fc, bk_, wdv: lambda e: e.matmul(out=pbanks[bk_], lhsT=hid[:, fc, st_ * 128:(st_ + 1) * 128],
                                                                                        rhs=wdv[:, fc, hh * 512:(hh + 1) * 512], start=(fc == 0), stop=(fc == 3)))(st_, hh, fc, bk_, wdv),
                                 R=[b_hid, b_wd[par]], W=[pbuf[bk_]])
                        copy_rr(ysb[:, st_, hh * 512:(hh + 1) * 512], pbanks[bk_], [pbuf[bk_]], [b_ysb])
                P.dma(ys_dram[j * 512:(j + 1) * 512, :].rearrange("(a p) d -> p a d", p=128), ysb, R=[b_ysb], W=[ys_b])

            mxload(0)
            mxload(1)
            mfront(0)
            for j in range(NTILE):
                if j + 2 < NTILE:
                    mxload(j + 2)
                if j + 1 < NTILE:
                    P.fork(2)
                    P.stream(0)
                    mfront(j + 1)
                    P.stream(1)
                    mback(j)
                    n0, n1 = len(P.streams[0]), len(P.streams[1])
                    P.join([max(1, n0 // n1), max(1, n1 // n0)])
                else:
                    mback(j)
            A.release(m_r)
            ht5, b_ht5 = A.alloc([128, 2, 1024], F32, nbufs=2, name="ht2")
            Y1, b_Y1 = A.alloc([128, 2, 1024], F32, nbufs=2, name="Y1")
            Y2, b_Y2 = A.alloc([128, 2, 1024], F32, nbufs=2, name="Y2")
            rr2_, b_rr2_ = A.alloc([128, 2, 1024], F32, nbufs=2, name="rr2")
            lnt = []
            for _ in range(2):
                a_, ba_ = A.alloc([128, 2, 6], F32, name="lnst")
                c_, bc_ = A.alloc([128, 2], F32, name="lnmv")
                d_, bd_ = A.alloc([128, 2], F32, name="lnsd")
                lnt.append((a_, ba_, c_, bc_, d_, bd_))
            y_bs = [Buf("y%d" % i) for i in range(NG)]
            h2, b_h2 = A.alloc([128, 2, 1024], F32, nbufs=2, name="h2")
            def r5(gi):
                par = gi % 2
                rr2, b_rr2 = rr2_[:, par, :], b_rr2_[par]
                P.dma(ht5[:, par, :], hres[gi * 128:(gi + 1) * 128, :], R=[hres_b[gi]], W=[b_ht5[par]])
                for k, (Y_, bY) in enumerate(((Y1, b_Y1), (Y2, b_Y2))):
                    P.op("pool", (lambda gi, k, par, Y_: lambda e: e.indirect_dma_start(
                        out=Y_[:, par, :], out_offset=None, in_=ys_dram[:, :],
                        in_offset=bass.IndirectOffsetOnAxis(ap=POSi[:, gi * 2 + k:gi * 2 + k + 1], axis=0)))(gi, k, par, Y_),
                        R=[ys_b, b_POSi], W=[bY[par]], dma=True)
                P.op("act", (lambda gi, par: lambda e: e.activation(out=rr2, in_=Y1[:, par, :], func=AF.Copy, scale=GW[:, gi, 0:1]))(gi, par),
                     R=[b_Y1[par], b_GW], W=[b_rr2])
                P.op("dve", (lambda gi, par: lambda e: e.scalar_tensor_tensor(out=rr2, in0=Y2[:, par, :], scalar=GW[:, gi, 1:2], in1=rr2, op0=ALU.mult, op1=ALU.add))(gi, par),
                     R=[b_Y2[par], b_GW, b_rr2], W=[b_rr2])
                P.op("dve", (lambda par: lambda e: e.scalar_tensor_tensor(out=rr2, in0=ht5[:, par, :], scalar=ALPHA, in1=rr2, op0=ALU.mult, op1=ALU.add))(par),
                     R=[b_ht5[par], b_rr2], W=[b_rr2])
                layer_norm_tile(rr2, b_rr2, LGf, LBf, b_LGf, h2[:, par, :], b_h2[par], b_LBf, tmps=lnt[par])
                if final:
                    s_, qt_ = gi // NT, gi % NT
                    P.dma(y[s_, qt_ * 128:(qt_ + 1) * 128, :], h2[:, par, :], R=[b_h2[par]], W=[y_bs[gi]])
                else:
                    P.dma(hres[gi * 128:(gi + 1) * 128, :], h2[:, par, :], R=[b_h2[par]], W=[hres_b[gi]])
            P.fork(2)
            for gi in range(NG):
                P.stream(gi % 2)
                r5(gi)
            P.join([1, 1])
            A.release(m_moe)


        def s5_stage():
            m_s5 = A.mark()
            gT_b = [Buf("gTd%d" % i) for i in range(SEQ_PER_CORE)]
            Win, b_Win = A.alloc([128, 8, 1024], BF16, name="Win")
            wdma(Win, b_Win, s5_w_in[0])
            DV, b_DV = A.alloc([128, 1024], F32, name="DV")
            P.dma(DV, s5_d[0].partition_broadcast(128), W=[b_DV])
            TA, b_TA = A.alloc([128, 4096], F32, name="TA")
            TB, b_TB = A.alloc([128, 4096], F32, name="TB")
            Pre, b_Pre = A.alloc([128, 32, 128], F32, name="Pre")
            Pim, b_Pim = A.alloc([128, 32, 128], F32, name="Pim")
            BDr, b_BDr = A.alloc([128, 8, 512], BF16, name="BDr")
            BDi, b_BDi = A.alloc([128, 8, 512], BF16, name="BDi")
            CmR, b_CmR = A.alloc([128, 1024], BF16, name="CmR")
            CmI, b_CmI = A.alloc([128, 1024], BF16, name="CmI")
            TRIc, b_TRIc = A.alloc([128, 128], BF16, name="TRIc")
            a1, b_a1 = A.alloc([128, 2, 32], F32, name="a1")
            P.op("pool", lambda e: e.memset(TRIc, 1.0), W=[b_TRIc])
            P.op("pool", lambda e: e.affine_select(out=TRIc, in_=TRIc, pattern=[[1, 128]], compare_op=ALU.is_ge, fill=0.0, base=0,
                                                   channel_multiplier=-1), R=[b_TRIc], W=[b_TRIc])
            m_p = A.mark()
            P.fork(2)
            P.stream(0)
            lr, b_lr = A.alloc([128, 32], F32, name="lr")
            li, b_li = A.alloc([128, 32], F32, name="li")
            ldt, b_ldt = A.alloc([128, 32], F32, name="ldt")
            for two in range(2):
                ps_ = slice(two * 64, (two + 1) * 64)
                P.dma(lr[ps_, :], s5_lam_re[0].rearrange("(q two) p -> two p q", two=2)[two], W=[b_lr], allow_slow_non_contiguous=True)
                P.dma(li[ps_, :], s5_lam_im[0].rearrange("(q two) p -> two p q", two=2)[two], W=[b_li], allow_slow_non_contiguous=True)
                P.dma(ldt[ps_, :], s5_log_dt[0].rearrange("(q two) -> two q", two=2)[two].partition_broadcast(64), W=[b_ldt],
                      allow_slow_non_contiguous=True)
            sc_, b_sc_ = A.alloc([128, 16, 32], F32, name="s5sc")
            V = lambda k: sc_[:, k, :]
            def dv(fn, R=(), W=()):
                P.op("dve", fn, R=[b_sc_, b_lr, b_li, b_ldt] + list(R), W=[b_sc_] + list(W))
            def ac(fn, R=(), W=()):
                P.op("act", fn, R=[b_sc_, b_lr, b_li, b_ldt] + list(R), W=[b_sc_] + list(W))
            dv(lambda e: e.tensor_scalar(out=lr, in0=lr, scalar1=-1e-4, scalar2=None, op0=ALU.min), W=[b_lr])
            ac(lambda e: e.activation(out=V(0), in_=ldt, func=AF.Exp))
            dv(lambda e: e.tensor_tensor(out=V(1), in0=lr, in1=V(0), op=ALU.mult))
            dv(lambda e: e.tensor_tensor(out=V(2), in0=li, in1=V(0), op=ALU.mult))
            dv(lambda e: e.tensor_scalar(out=V(3), in0=V(2), scalar1=1.0 / 64, scalar2=1.5, op0=ALU.mult, op1=ALU.min))
            dv(lambda e: e.tensor_scalar(out=V(3), in0=V(3), scalar1=-1.5, scalar2=None, op0=ALU.max))
            ac(lambda e: e.activation(out=V(4), in_=V(3), func=AF.Sin))
            ac(lambda e: e.activation(out=V(5), in_=V(3), func=AF.Sin, bias=halfpi[:, 0:1], scale=1.0), R=[b_hp])
            ac(lambda e: e.activation(out=V(6), in_=V(1), func=AF.Exp, scale=1.0 / 64))
            dv(lambda e: e.tensor_tensor(out=V(7), in0=V(5), in1=V(6), op=ALU.mult))
            dv(lambda e: e.tensor_tensor(out=V(8), in0=V(4), in1=V(6), op=ALU.mult))
            for _ in range(6):
                dv(lambda e: e.tensor_tensor(out=V(9), in0=V(7), in1=V(7), op=ALU.mult))
                dv(lambda e: e.tensor_tensor(out=V(10), in0=V(8), in1=V(8), op=ALU.mult))
                dv(lambda e: e.tensor_tensor(out=V(11), in0=V(7), in1=V(8), op=ALU.mult))
                dv(lambda e: e.tensor_tensor(out=V(7), in0=V(9), in1=V(10), op=ALU.subtract))
                dv(lambda e: e.tensor_scalar(out=V(8), in0=V(11), scalar1=2.0, scalar2=None, op0=ALU.mult))
            dv(lambda e: e.tensor_copy(out=a1[:, 0, :], in_=V(7)), W=[b_a1])
            dv(lambda e: e.tensor_copy(out=a1[:, 1, :], in_=V(8)), W=[b_a1])
            ac(lambda e: e.activation(out=V(9), in_=V(1), func=AF.Exp, scale=-2.0))
            dv(lambda e: e.tensor_tensor(out=V(10), in0=V(7), in1=V(9), op=ALU.mult))
            dv(lambda e: e.scalar_tensor_tensor(out=V(11), in0=V(8), scalar=-1.0, in1=V(9), op0=ALU.mult, op1=ALU.mult))
            dv(lambda e: e.tensor_tensor(out=V(12), in0=lr, in1=lr, op=ALU.mult))
            dv(lambda e: e.tensor_tensor(out=V(13), in0=li, in1=li, op=ALU.mult))
            dv(lambda e: e.tensor_tensor(out=V(12), in0=V(12), in1=V(13), op=ALU.add))
            dv(lambda e: e.reciprocal(out=V(12), in_=V(12)))
            dv(lambda e: e.tensor_scalar(out=V(13), in0=V(7), scalar1=-1.0, scalar2=None, op0=ALU.add))
            dv(lambda e: e.tensor_tensor(out=V(14), in0=V(13), in1=lr, op=ALU.mult))
            dv(lambda e: e.tensor_tensor(out=V(15), in0=V(8), in1=li, op=ALU.mult))
            dv(lambda e: e.tensor_tensor(out=V(14), in0=V(14), in1=V(15), op=ALU.add))
            dv(lambda e: e.tensor_tensor(out=V(14), in0=V(14), in1=V(12), op=ALU.mult))
            dv(lambda e: e.tensor_tensor(out=V(15), in0=V(8), in1=lr, op=ALU.mult))
            dv(lambda e: e.tensor_tensor(out=V(13), in0=V(13), in1=li, op=ALU.mult))
            dv(lambda e: e.tensor_tensor(out=V(15), in0=V(15), in1=V(13), op=ALU.subtract))
            dv(lambda e: e.tensor_tensor(out=V(15), in0=V(15), in1=V(12), op=ALU.mult))
            Qre, b_Qre = A.alloc([128, 32, 128], F32, name="Qre")
            Qim, b_Qim = A.alloc([128, 32, 128], F32, name="Qim")
            tq1, b_tq1 = A.alloc([128, 32, 64], F32, name="tq1")
            tq2, b_tq2 = A.alloc([128, 32, 64], F32, name="tq2")
            pw, b_pw = A.alloc([128, 2, 32], F32, name="pw")
            sq, b_sq = A.alloc([128, 3, 32], F32, name="sq")
            for (Tr, bTr, Ti, bTi, i0r, i0i, br_, bi_) in ((Pre, b_Pre, Pim, b_Pim, None, None, 7, 8), (Qre, b_Qre, Qim, b_Qim, 14, 15, 10, 11)):
                if i0r is None:
                    P.op("pool", (lambda Tr: lambda e: e.memset(Tr[:, :, 0:1], 1.0))(Tr), W=[bTr])
                    P.op("pool", (lambda Ti: lambda e: e.memset(Ti[:, :, 0:1], 0.0))(Ti), W=[bTi])
                else:
                    P.op("dve", (lambda Tr, i0r: lambda e: e.tensor_copy(out=Tr[:, :, 0], in_=V(i0r)))(Tr, i0r), R=[b_sc_], W=[bTr])
                    P.op("dve", (lambda Ti, i0i: lambda e: e.tensor_copy(out=Ti[:, :, 0], in_=V(i0i)))(Ti, i0i), R=[b_sc_], W=[bTi])
                P.op("dve", (lambda br_: lambda e: e.tensor_copy(out=pw[:, 0, :], in_=V(br_)))(br_), R=[b_sc_], W=[b_pw])
                P.op("dve", (lambda bi_: lambda e: e.tensor_copy(out=pw[:, 1, :], in_=V(bi_)))(bi_), R=[b_sc_], W=[b_pw])
                n_ = 1
                while n_ < 128:
                    pr = pw[:, 0, :].unsqueeze(2).to_broadcast([128, 32, n_])
                    pi_ = pw[:, 1, :].unsqueeze(2).to_broadcast([128, 32, n_])
                    Rb = [bTr, bTi, b_pw, b_tq1, b_tq2, b_sq]
                    P.op("dve", (lambda Tr, pr, n_: lambda e: e.tensor_tensor(out=tq1[:, :, 0:n_], in0=Tr[:, :, 0:n_], in1=pr, op=ALU.mult))(Tr, pr, n_), R=Rb, W=[b_tq1])
                    P.op("dve", (lambda Ti, pi_, n_: lambda e: e.tensor_tensor(out=tq2[:, :, 0:n_], in0=Ti[:, :, 0:n_], in1=pi_, op=ALU.mult))(Ti, pi_, n_), R=Rb, W=[b_tq2])
                    P.op("dve", (lambda Tr, n_: lambda e: e.tensor_tensor(out=Tr[:, :, n_:2 * n_], in0=tq1[:, :, 0:n_], in1=tq2[:, :, 0:n_], op=ALU.subtract))(Tr, n_), R=Rb, W=[bTr])
                    P.op("dve", (lambda Tr, pi_, n_: lambda e: e.tensor_tensor(out=tq1[:, :, 0:n_], in0=Tr[:, :, 0:n_], in1=pi_, op=ALU.mult))(Tr, pi_, n_), R=Rb, W=[b_tq1])
                    P.op("dve", (lambda Ti, pr, n_: lambda e: e.tensor_tensor(out=tq2[:, :, 0:n_], in0=Ti[:, :, 0:n_], in1=pr, op=ALU.mult))(Ti, pr, n_), R=Rb, W=[b_tq2])
                    P.op("dve", (lambda Ti, n_: lambda e: e.tensor_tensor(out=Ti[:, :, n_:2 * n_], in0=tq1[:, :, 0:n_], in1=tq2[:, :, 0:n_], op=ALU.add))(Ti, n_), R=Rb, W=[bTi])
                    P.op("dve", lambda e: e.tensor_tensor(out=sq[:, 0, :], in0=pw[:, 0, :], in1=pw[:, 0, :], op=ALU.mult), R=Rb, W=[b_sq])
                    P.op("dve", lambda e: e.tensor_tensor(out=sq[:, 1, :], in0=pw[:, 1, :], in1=pw[:, 1, :], op=ALU.mult), R=Rb, W=[b_sq])
                    P.op("dve", lambda e: e.tensor_tensor(out=sq[:, 2, :], in0=pw[:, 0, :], in1=pw[:, 1, :], op=ALU.mult), R=Rb, W=[b_sq])
                    P.op("dve", lambda e: e.tensor_tensor(out=pw[:, 0, :], in0=sq[:, 0, :],
                                                          in1=sq[:, 1, :], op=ALU.subtract), R=Rb, W=[b_pw])
                    P.op("dve", lambda e: e.tensor_scalar(out=pw[:, 1, :], in0=sq[:, 2, :], scalar1=2.0, scalar2=None, op0=ALU.mult),
                         R=Rb, W=[b_pw])
                    n_ *= 2
            for (Q_, bQ, T_, bT) in ((Qre, b_Qre, TA, b_TA), (Qim, b_Qim, TB, b_TB)):
                for q4 in range(8):
                    bk_ = q4 % 2
                    for c in range(4):
                        q = q4 * 4 + c
                        P.op("pe", (lambda Q_, q, c, bk_: lambda e: e.transpose(out=pbanks[bk_][:, c * 128:(c + 1) * 128], in_=Q_[:, q, :], identity=ident))(Q_, q, c, bk_),
                             R=[bQ, b_ident], W=[pbuf[bk_]])
                    copy_rr(T_[:, q4 * 512:(q4 + 1) * 512], pbanks[bk_], [pbuf[bk_]], [bT])
            P.stream(1)
            m_bc = A.mark()
            Xb, b_Xb = A.alloc([128, 4, 128], F32, nbufs=4, name="Xb")
            Xbb, b_Xbb = A.alloc([128, 4, 128], BF16, nbufs=4, name="Xbb")
            cnt_ = 0
            for (src, BD_, bBD) in ((s5_b_re, BDr, b_BDr), (s5_b_im, BDi, b_BDi)):
                for kc in range(8):
                    for pair in range(4):
                        sl = cnt_ % 4
                        cnt_ += 1
                        P.op("pool", (lambda sl: lambda e: e.memset(Xb[:, sl, :], 0.0))(sl), W=[b_Xb[sl]])
                        for two in range(2):
                            g = 8 * kc + 2 * pair + two
                            c0 = (2 * pair + two) * 16
                            P.dma(Xb[two * 64:(two + 1) * 64, sl, c0:c0 + 16], src[0, g], W=[b_Xb[sl]])
                        P.op("act", (lambda sl: lambda e: e.copy(out=Xbb[:, sl, :], in_=Xb[:, sl, :]))(sl), R=[b_Xb[sl]], W=[b_Xbb[sl]])
                        bk_ = 2 + cnt_ % 2
                        P.op("pe", (lambda sl, bk_: lambda e: e.transpose(out=pbf(bk_)[:, 0:128], in_=Xbb[:, sl, :], identity=identb))(sl, bk_),
                             R=[b_Xbb[sl], b_identb], W=[pbuf[bk_]])
                        copy_rr(BD_[:, kc, pair * 128:(pair + 1) * 128], pbf(bk_)[:, 0:128], [pbuf[bk_]], [bBD])
            for (src, Cm_, bCm, sgn) in ((s5_c_re, CmR, b_CmR, 1.0), (s5_c_im, CmI, b_CmI, -1.0)):
                for kc in range(8):
                    sl = cnt_ % 4
                    cnt_ += 1
                    P.op("pool", (lambda sl: lambda e: e.memset(Xb[:, sl, :], 0.0))(sl), W=[b_Xb[sl]])
                    for gl in range(8):
                        g = 8 * kc + gl
                        two = g % 2
                        P.dma(Xb[gl * 16:(gl + 1) * 16, sl, two * 64:(two + 1) * 64], src[0, g], W=[b_Xb[sl]])
                    P.op("act", (lambda sl: lambda e: e.copy(out=Xbb[:, sl, :], in_=Xb[:, sl, :]))(sl), R=[b_Xb[sl]], W=[b_Xbb[sl]])
                    bk_ = 2 + cnt_ % 2
                    P.op("pe", (lambda sl, bk_: lambda e: e.transpose(out=pbf(bk_)[:, 0:128], in_=Xbb[:, sl, :], identity=identb))(sl, bk_),
                         R=[b_Xbb[sl], b_identb], W=[pbuf[bk_]])
                    P.op("act", (lambda Cm_, kc, bk_, sgn: lambda e: e.activation(out=Cm_[:, kc * 128:(kc + 1) * 128], in_=pbf(bk_)[:, 0:128], func=AF.Copy, scale=sgn))(Cm_, kc, bk_, sgn),
                         R=[pbuf[bk_]], W=[bCm])
            n0, n1 = len(P.streams[0]), len(P.streams[1])
            P.join([max(1, n0 // n1), max(1, n1 // n0)])
            A.release(m_p)
            m_scan = A.mark()
            ht, b_ht = A.alloc([128, 2, 1024], F32, nbufs=2, name="s5ht")
            hTt, b_hTt = A.alloc([128, 8, 128], BF16, name="hTt")
            uTt, b_uTt = A.alloc([128, 8, 128], BF16, name="uTt")
            ud2, b_ud2 = A.alloc([128, 2, 1024], F32, nbufs=2, name="ud")
            wre2, b_wre2 = A.alloc([128, 2, 4096], BF16, nbufs=2, name="wre")
            wim2, b_wim2 = A.alloc([128, 2, 4096], BF16, nbufs=2, name="wim")
            sre, b_sre = A.alloc([128, 32, 128], BF16, name="sre")
            sim_, b_sim = A.alloc([128, 32, 128], BF16, name="sim")
            ttf, b_ttf = A.alloc([128, 4, 512], F32, nbufs=4, name="ttf")
            ttb, b_ttb = A.alloc([128, 4, 512], F32, nbufs=4, name="ttb")
            zz, b_zz = A.alloc([128, 2, 2, 4 * 128], F32, nbufs=2, name="zz")
            zz = zz.rearrange("p z r (c i) -> p z r c i", i=128)
            cre, b_cre = A.alloc([128, 2, 32], F32, name="cre")
            send, b_send = A.alloc([128, 2, 32], F32, name="send")
            ctmp, b_ctmp = A.alloc([128, 4, 32], F32, name="ctmp")
            yy, b_yy = A.alloc([128, 1024], F32, name="yy")
            gb, b_gb = A.alloc([128, 1024], BF16, name="gb")
            gTt, b_gTt = A.alloc([128, 8, 128], BF16, name="gTt")

            def front(gi):
                par = gi % 2
                ud, b_ud, wre, b_wre, wim, b_wim = ud2[:, par, :], b_ud2[par], wre2[:, par, :], b_wre2[par], wim2[:, par, :], b_wim2[par]
                for g2 in range(2):
                    for c in range(4):
                        kc = 4 * g2 + c
                        P.op("pe", (lambda par, kc, c, g2: lambda e: e.transpose(out=pbanks[g2][:, c * 128:(c + 1) * 128], in_=ht[:, par, kc * 128:(kc + 1) * 128],
                                                                               identity=ident))(par, kc, c, g2), R=[b_ht[par], b_ident], W=[pbuf[g2]])
                    copy_rr(hTt[:, 4 * g2:4 * g2 + 4, :], pbanks[g2].rearrange("p (a b) -> p a b", b=128), [pbuf[g2]], [b_hTt])
                for g2 in range(2):
                    for c in range(4):
                        fc = 4 * g2 + c
                        for kc in range(8):
                            P.op("pe", (lambda fc, c, kc, g2: lambda e: e.matmul(out=pbanks[g2][:, c * 128:(c + 1) * 128], lhsT=Win[:, kc, fc * 128:(fc + 1) * 128], rhs=hTt[:, kc, :],
                                                                                 start=(kc == 0), stop=(kc == 7)))(fc, c, kc, g2), R=[b_Win, b_hTt], W=[pbuf[g2]])
                    copy_rr(uTt[:, 4 * g2:4 * g2 + 4, :], pbanks[g2].rearrange("p (a b) -> p a b", b=128), [pbuf[g2]], [b_uTt])
                for hh in range(2):
                    for kc in range(8):
                        P.op("pe", (lambda hh, kc: lambda e: e.matmul(out=pbanks[2 + hh], lhsT=hTt[:, kc, :], rhs=Win[:, kc, hh * 512:(hh + 1) * 512],
                                                                    start=(kc == 0), stop=(kc == 7)))(hh, kc), R=[b_Win, b_hTt], W=[pbuf[2 + hh]])
                    P.op("dve", (lambda hh: lambda e: e.tensor_tensor(out=ud[:, hh * 512:(hh + 1) * 512], in0=pbanks[2 + hh], in1=DV[:, hh * 512:(hh + 1) * 512], op=ALU.mult))(hh),
                         R=[pbuf[2 + hh], b_DV], W=[b_ud])
                for kc in range(8):
                    br_, bi_ = (kc % 2) * 2, 1 + (kc % 2) * 2
                    cs = slice(kc * 512, (kc + 1) * 512)
                    P.op("pe", (lambda kc, br_: lambda e: e.matmul(out=pbanks[br_], lhsT=uTt[:, kc, :], rhs=BDr[:, kc, :], start=True, stop=True))(kc, br_),
                         R=[b_uTt, b_BDr], W=[pbuf[br_]])
                    P.op("pe", (lambda kc, bi_: lambda e: e.matmul(out=pbanks[bi_], lhsT=uTt[:, kc, :], rhs=BDi[:, kc, :], start=True, stop=True))(kc, bi_),
                         R=[b_uTt, b_BDi], W=[pbuf[bi_]])
                    for (k_, bnk, T_, bT_) in ((0, br_, TA, b_TA), (1, bi_, TB, b_TB), (2, bi_, TA, b_TA), (3, br_, TB, b_TB)):
                        P.op("dve", (lambda k_, bnk, T_, cs: lambda e: e.tensor_tensor(out=ttf[:, k_, :], in0=pbanks[bnk], in1=T_[:, cs], op=ALU.mult))(k_, bnk, T_, cs),
                             R=[pbuf[bnk], bT_], W=[b_ttf[k_]])
                    P.op("pool", (lambda cs: lambda e: e.tensor_tensor(out=wre[:, cs], in0=ttf[:, 0, :], in1=ttf[:, 1, :], op=ALU.subtract))(cs),
                         R=[b_ttf[0], b_ttf[1]], W=[b_wre])
                    P.op("pool", (lambda cs: lambda e: e.tensor_tensor(out=wim[:, cs], in0=ttf[:, 2, :], in1=ttf[:, 3, :], op=ALU.add))(cs),
                         R=[b_ttf[2], b_ttf[3]], W=[b_wim])

            def back(gi):
                par = gi % 2
                s_, qt = gi // NT, gi % NT
                ud, b_ud, wre, b_wre, wim, b_wim = ud2[:, par, :], b_ud2[par], wre2[:, par, :], b_wre2[par], wim2[:, par, :], b_wim2[par]
                if qt == 0:
                    P.op("pool", lambda e: e.memset(cre, 0.0), W=[b_cre])
                for q4 in range(8):
                    br_, bi_ = 4 + (q4 % 2) * 2, 5 + (q4 % 2) * 2
                    zp = q4 % 2
                    for c in range(4):
                        q = q4 * 4 + c
                        P.op("pe", (lambda q, c, br_: lambda e: e.matmul(out=pbanks[br_][:, c * 128:(c + 1) * 128], lhsT=wre[:, q * 128:(q + 1) * 128], rhs=TRIc,
                                                                       start=True, stop=True))(q, c, br_), R=[b_wre, b_TRIc], W=[pbuf[br_]])
                        P.op("pe", (lambda q, c, bi_: lambda e: e.matmul(out=pbanks[bi_][:, c * 128:(c + 1) * 128], lhsT=wim[:, q * 128:(q + 1) * 128], rhs=TRIc,
                                                                       start=True, stop=True))(q, c, bi_), R=[b_wim, b_TRIc], W=[pbuf[bi_]])
                    qs = slice(q4 * 4, q4 * 4 + 4)
                    P.op("dve", (lambda br_, qs, zp: lambda e: e.tensor_tensor(out=zz[:, zp, 0, :, :], in0=pbanks[br_].rearrange("p (a b) -> p a b", b=128),
                                                                          in1=cre[:, 0, qs].unsqueeze(2).to_broadcast([128, 4, 128]), op=ALU.add))(br_, qs, zp), R=[pbuf[br_], b_cre], W=[b_zz[zp]])
                    P.op("dve", (lambda bi_, qs, zp: lambda e: e.tensor_tensor(out=zz[:, zp, 1, :, :], in0=pbanks[bi_].rearrange("p (a b) -> p a b", b=128),
                                                                          in1=cre[:, 1, qs].unsqueeze(2).to_broadcast([128, 4, 128]), op=ALU.add))(bi_, qs, zp), R=[pbuf[bi_], b_cre], W=[b_zz[zp]])
                    tv = lambda k_: ttb[:, k_, :].rearrange("p (a b) -> p a b", b=128)
                    t0v, t1v, t2v, t3v = tv(0), tv(1), tv(2), tv(3)
                    zrv, ziv = zz[:, zp, 0, :, :], zz[:, zp, 1, :, :]
                    P.op("pool", (lambda qs, t0v, zrv: lambda e: e.tensor_tensor(out=t0v, in0=zrv, in1=Pre[:, qs, :], op=ALU.mult))(qs, t0v, zrv), R=[b_zz[zp], b_Pre], W=[b_ttb[0]])
                    P.op("pool", (lambda qs, t1v, ziv: lambda e: e.tensor_tensor(out=t1v, in0=ziv, in1=Pim[:, qs, :], op=ALU.mult))(qs, t1v, ziv), R=[b_zz[zp], b_Pim], W=[b_ttb[1]])
                    P.op("pool", (lambda qs, t2v, ziv: lambda e: e.tensor_tensor(out=t2v, in0=ziv, in1=Pre[:, qs, :], op=ALU.mult))(qs, t2v, ziv), R=[b_zz[zp], b_Pre], W=[b_ttb[2]])
                    P.op("dve", (lambda qs, t3v, zrv: lambda e: e.tensor_tensor(out=t3v, in0=zrv, in1=Pim[:, qs, :], op=ALU.mult))(qs, t3v, zrv), R=[b_zz[zp], b_Pim], W=[b_ttb[3]])
                    P.op("dve", (lambda qs, t0v, t1v: lambda e: e.tensor_tensor(out=sre[:, qs, :], in0=t0v, in1=t1v, op=ALU.subtract))(qs, t0v, t1v), R=[b_ttb[0], b_ttb[1]], W=[b_sre])
                    P.op("dve", (lambda qs, t0v, t1v: lambda e: e.tensor_tensor(out=send[:, 0, qs], in0=t0v[:, :, 127], in1=t1v[:, :, 127], op=ALU.subtract))(qs, t0v, t1v),
                         R=[b_ttb[0], b_ttb[1]], W=[b_send])
                    P.op("dve", (lambda qs, t2v, t3v: lambda e: e.tensor_tensor(out=sim_[:, qs, :], in0=t2v, in1=t3v, op=ALU.add))(qs, t2v, t3v), R=[b_ttb[2], b_ttb[3]], W=[b_sim])
                    P.op("dve", (lambda qs, t2v, t3v: lambda e: e.tensor_tensor(out=send[:, 1, qs], in0=t2v[:, :, 127], in1=t3v[:, :, 127], op=ALU.add))(qs, t2v, t3v),
                         R=[b_ttb[2], b_ttb[3]], W=[b_send])
                P.op("dve", lambda e: e.tensor_tensor(out=ctmp[:, 0, :], in0=send[:, 0, :], in1=a1[:, 0, :], op=ALU.mult), R=[b_send, b_a1], W=[b_ctmp])
                P.op("dve", lambda e: e.tensor_tensor(out=ctmp[:, 1, :], in0=send[:, 1, :], in1=a1[:, 1, :], op=ALU.mult), R=[b_send, b_a1], W=[b_ctmp])
                P.op("dve", lambda e: e.tensor_tensor(out=ctmp[:, 2, :], in0=send[:, 0, :], in1=a1[:, 1, :], op=ALU.mult), R=[b_send, b_a1], W=[b_ctmp])
                P.op("dve", lambda e: e.tensor_tensor(out=ctmp[:, 3, :], in0=send[:, 1, :], in1=a1[:, 0, :], op=ALU.mult), R=[b_send, b_a1], W=[b_ctmp])
                P.op("dve", lambda e: e.tensor_tensor(out=cre[:, 0, :], in0=ctmp[:, 0, :], in1=ctmp[:, 1, :], op=ALU.subtract), R=[b_ctmp], W=[b_cre])
                P.op("dve", lambda e: e.tensor_tensor(out=cre[:, 1, :], in0=ctmp[:, 2, :], in1=ctmp[:, 3, :], op=ALU.add), R=[b_ctmp], W=[b_cre])
                for q in range(32):
                    bk_ = 4 + q // 16
                    col = (q % 16) * 32
                    P.op("pe", (lambda q, bk_, col: lambda e: e.matmul(out=pbanks[bk_][:, col:col + 32], lhsT=sre[:, q, :], rhs=CmR[:, q * 32:(q + 1) * 32], start=True, stop=False))(q, bk_, col),
                         R=[b_sre, b_CmR], W=[pbuf[bk_]])
                    P.op("pe", (lambda q, bk_, col: lambda e: e.matmul(out=pbanks[bk_][:, col:col + 32], lhsT=sim_[:, q, :], rhs=CmI[:, q * 32:(q + 1) * 32], start=False, stop=True))(q, bk_, col),
                         R=[b_sim, b_CmI], W=[pbuf[bk_]])
                for hh in range(2):
                    P.op("dve", (lambda hh: lambda e: e.tensor_tensor(out=yy[:, hh * 512:(hh + 1) * 512], in0=pbanks[4 + hh], in1=ud[:, hh * 512:(hh + 1) * 512], op=ALU.add))(hh),
                         R=[pbuf[4 + hh], b_ud], W=[b_yy])
                P.op("act", lambda e: e.activation(out=gb, in_=yy, func=AF.Gelu), R=[b_yy], W=[b_gb])
                for kc in range(8):
                    P.op("pe", (lambda kc: lambda e: e.transpose(out=pbf(6)[:, kc * 128:(kc + 1) * 128], in_=gb[:, kc * 128:(kc + 1) * 128], identity=identb))(kc),
                         R=[b_gb, b_identb], W=[pbuf[6]])
                copy_rr(gTt, pbf(6).rearrange("p (a b) -> p a b", b=128), [pbuf[6]], [b_gTt])
                P.dma(gT_dram[s_, :, :, qt * 128:(qt + 1) * 128], gTt, R=[b_gTt], W=[gT_b[s_]])

            NGT = SEQ_PER_CORE * NT

            def hload(gi):
                P.dma(ht[:, gi % 2, :], hres[gi * 128:(gi + 1) * 128, :], R=[hres_b[gi]], W=[b_ht[gi % 2]])
            hload(0)
            hload(1)
            front(0)
            for gi in range(NGT):
                if gi + 2 < NGT:
                    hload(gi + 2)
                if gi + 1 < NGT:
                    P.fork(2)
                    P.stream(0)
                    front(gi + 1)
                    P.stream(1)
                    back(gi)
                    n0, n1 = len(P.streams[0]), len(P.streams[1])
                    P.join([max(1, n0 // n1), max(1, n1 // n0)])
                else:
                    back(gi)
            A.release(m_scan)
            A.release(m_s5)
            m_g = A.mark()
            LG1b, b_LG1b = A.alloc([128, 1024], F32, name="LG1b")
            LB1b, b_LB1b = A.alloc([128, 1024], F32, name="LB1b")
            P.dma(LG1b, ln_mix_g[1].partition_broadcast(128), W=[b_LG1b])
            P.dma(LB1b, ln_mix_b[1].partition_broadcast(128), W=[b_LB1b])
            W1, b_W1 = A.alloc([128, 8, 1024], BF16, name="W1")
            W2, b_W2 = A.alloc([128, 8, 1024], BF16, name="W2")
            Wo1, b_Wo1 = A.alloc([128, 8, 1024], BF16, name="Wo1")
            wdma(W1, b_W1, s5_glu_w1[0])
            wdma(W2, b_W2, s5_glu_w2[0])
            wdma(Wo1, b_Wo1, s5_w_out[0])
            gTs2, b_gTs2 = A.alloc([128, 2, 8, 512], BF16, nbufs=2, name="gTs")
            zT2, b_zT2 = A.alloc([128, 2, 8, 512], BF16, nbufs=2, name="zT")
            sgm, b_sgm = A.alloc([128, 2, 512], F32, nbufs=2, name="sgm")
            ght, b_ght = A.alloc([128, 2, 1024], F32, nbufs=2, name="ght")
            rr3, b_rr3 = A.alloc([128, 1024], F32, name="rr3")
            h3, b_h3 = A.alloc([128, 2, 1024], F32, nbufs=2, name="h3")
            if True:
                def gfront(bi_):
                    s_, nb_ = bi_ // 4, bi_ % 4
                    gTs, b_gTs, zT, b_zT = gTs2[:, bi_ % 2], b_gTs2[bi_ % 2], zT2[:, bi_ % 2], b_zT2[bi_ % 2]
                    for fc in range(8):
                        b1_, b2_ = (fc % 2) * 2, 1 + (fc % 2) * 2
                        for kc in range(8):
                            P.op("pe", (lambda fc, kc, b1_: lambda e: e.matmul(out=pbanks[b1_], lhsT=W1[:, kc, fc * 128:(fc + 1) * 128], rhs=gTs[:, kc, :], start=(kc == 0), stop=(kc == 7)))(fc, kc, b1_),
                                 R=[b_W1, b_gTs], W=[pbuf[b1_]])
                        for kc in range(8):
                            P.op("pe", (lambda fc, kc, b2_: lambda e: e.matmul(out=pbanks[b2_], lhsT=W2[:, kc, fc * 128:(fc + 1) * 128], rhs=gTs[:, kc, :], start=(kc == 0), stop=(kc == 7)))(fc, kc, b2_),
                                 R=[b_W2, b_gTs], W=[pbuf[b2_]])
                        sp_ = fc % 2
                        P.op("act", (lambda b2_, sp_: lambda e: e.activation(out=sgm[:, sp_, :], in_=pbanks[b2_], func=AF.Sigmoid))(b2_, sp_), R=[pbuf[b2_]], W=[b_sgm[sp_]])
                        P.op("dve", (lambda fc, b1_, sp_: lambda e: e.tensor_tensor(out=zT[:, fc, :], in0=pbanks[b1_], in1=sgm[:, sp_, :], op=ALU.mult))(fc, b1_, sp_),
                             R=[pbuf[b1_], b_sgm[sp_]], W=[b_zT])
                def gback(bi_):
                    s_, nb_ = bi_ // 4, bi_ % 4
                    gTs, b_gTs, zT, b_zT = gTs2[:, bi_ % 2], b_gTs2[bi_ % 2], zT2[:, bi_ % 2], b_zT2[bi_ % 2]
                    for t4 in range(4):
                        gi = s_ * NT + nb_ * 4 + t4
                        par = gi % 2
                        for hh in range(2):
                            for fc in range(8):
                                P.op("pe", (lambda t4, hh, fc: lambda e: e.matmul(out=pbanks[4 + hh], lhsT=zT[:, fc, t4 * 128:(t4 + 1) * 128], rhs=Wo1[:, fc, hh * 512:(hh + 1) * 512],
                                                                                start=(fc == 0), stop=(fc == 7)))(t4, hh, fc), R=[b_zT, b_Wo1], W=[pbuf[4 + hh]])
                        P.dma(ght[:, par, :], hres[gi * 128:(gi + 1) * 128, :], R=[hres_b[gi]], W=[b_ght[par]])
                        for hh in range(2):
                            P.op("dve", (lambda hh, par: lambda e: e.scalar_tensor_tensor(out=rr3[:, hh * 512:(hh + 1) * 512], in0=ght[:, par, hh * 512:(hh + 1) * 512], scalar=ALPHA,
                                                                                       in1=pbanks[4 + hh], op0=ALU.mult, op1=ALU.add))(hh, par), R=[b_ght[par], pbuf[4 + hh]], W=[b_rr3])
                        m_ln = A.mark()
                        layer_norm_tile(rr3, b_rr3, LG1b, LB1b, b_LG1b, h3[:, par, :], b_h3[par], b_LB1b)
                        A.release(m_ln)
                        P.dma(hres[gi * 128:(gi + 1) * 128, :], h3[:, par, :], R=[b_h3[par]], W=[hres_b[gi]])
            NB_ = SEQ_PER_CORE * 4

            def gload(bi_):
                P.dma(gTs2[:, bi_ % 2], gT_dram[bi_ // 4, :, :, (bi_ % 4) * 512:(bi_ % 4 + 1) * 512], R=[gT_b[bi_ // 4]], W=[b_gTs2[bi_ % 2]])
            gload(0)
            gload(1)
            gfront(0)
            for bi_ in range(NB_):
                if bi_ + 2 < NB_:
                    gload(bi_ + 2)
                if bi_ + 1 < NB_:
                    P.fork(2)
                    P.stream(0)
                    gfront(bi_ + 1)
                    P.stream(1)
                    gback(bi_)
                    n0, n1 = len(P.streams[0]), len(P.streams[1])
                    P.join([max(1, n0 // n1), max(1, n1 // n0)])
                else:
                    gback(bi_)
            A.release(m_g)

        if os.environ.get("PRECAST", "1") == "1":
            while precast_state["i"] < 32:
                precast_next()
        if debug != "l0":
            moe_stage(0, final=(debug == "moe0"))
        if debug is None or debug in ("s5", "full", "s5dbg"):
            s5_stage()
            if debug == "s5dbg":
                pass
            elif debug == "s5":
                for gi in range(SEQ_PER_CORE * NT):
                    P.dma(y[gi // NT, (gi % NT) * 128:(gi % NT + 1) * 128, :], hres[gi * 128:(gi + 1) * 128, :], R=[hres_b[gi]], W=[y_b])
            else:
                moe_stage(1, final=True)
        if debug == "l0":
            for gi in range(SEQ_PER_CORE * NT):
                s, qt = gi // NT, gi % NT
                if s >= nseq or qt >= nqt or stop:
                    continue
                P.dma(y[s, qt * 128:(qt + 1) * 128, :], hres[gi * 128:(gi + 1) * 128, :], R=[hres_b[gi]], W=[y_b])
        P.emit()
        print("ops", len(P.ops), P.stats, flush=True)
    return nc


_NC_CACHE = {}


def kernel(**inputs):
    n = 8
    if "nc" not in _NC_CACHE:
        _NC_CACHE["nc"] = build()
    nc = _NC_CACHE["nc"]
    x = np.ascontiguousarray(inputs["x"], dtype=np.float32)
    in_maps = []
    for c in range(n):
        m = {k: np.ascontiguousarray(v) for k, v in inputs.items() if k != "x"}
        m["x"] = x[c * SEQ_PER_CORE:(c + 1) * SEQ_PER_CORE]
        in_maps.append(m)
    res = run_bass_kernel_spmd(nc, in_maps, core_ids=list(range(n)))
    return np.concatenate([r["y"] for r in res.results], axis=0)
```

```python
import math
import os
import contextlib
import numpy as np
import concourse.bass as bass
import concourse.mybir as mybir
from concourse.bass_utils import run_bass_kernel_spmd

F32 = mybir.dt.float32
BF16 = mybir.dt.bfloat16
I32 = mybir.dt.int32
ALU = mybir.AluOpType
AF = mybir.ActivationFunctionType
AX = mybir.AxisListType

ENGS = ("pe", "act", "dve", "pool", "sp")
N_DMA_SEMS = 56
NSW = int(os.environ.get('NSW', '3'))

D = 1024
L = 2048
NT = 16
SEQ_PER_CORE = 2
ALPHA = (2.0 * 2) ** 0.25
EPS = 1e-5
NEG = -1e30
NTILE = 31
NG_TOK = SEQ_PER_CORE * NT
T5_STARTS = [1, 2, 3, 4, 5, 6, 7, 8, 9, 10, 11, 12, 13, 14, 15, 16, 19, 21, 24, 27, 31, 35, 40,
             46, 52, 59, 67, 77, 87, 99, 113]


class Buf:
    __slots__ = ("name", "lw", "rd")

    def __init__(self, name=""):
        self.name = name
        self.lw = None
        self.rd = []


class Prog:
    def __init__(self, nc):
        self.nc = nc
        self.ops = []
        self.cur = self.ops
        self.nuid = 0
        self.dma_rr = 0
        self.dma_rr_sw = 0

    def op(self, eng, fn, R=(), W=(), dma=False):
        if getattr(self, "mute", False):
            return None
        deps = set()
        raw = set()
        for b in R:
            if b.lw is not None:
                deps.add(b.lw)
                raw.add(b.lw)
        for b in W:
            if b.lw is not None:
                deps.add(b.lw)
            deps.update(b.rd)
        oid = self.nuid
        self.nuid += 1
        self.cur.append(dict(uid=oid, eng=eng, fn=fn, deps=deps, raw=raw, dma=dma))
        for b in R:
            b.rd.append(oid)
        for b in W:
            b.lw = oid
            b.rd = []
        return oid

    def fork(self, n):
        self.streams = [[] for _ in range(n)]

    def stream(self, k):
        self.cur = self.streams[k] if k is not None else self.ops

    def join(self, ratio=None):
        st = self.streams
        ratio = ratio or [1] * len(st)
        idx = [0] * len(st)
        while any(idx[k] < len(st[k]) for k in range(len(st))):
            for k in range(len(st)):
                for _ in range(ratio[k]):
                    if idx[k] < len(st[k]):
                        self.ops.append(st[k][idx[k]])
                        idx[k] += 1
        self.cur = self.ops
        self.streams = None

    def dma(self, out, in_, R=(), W=(), q="sp", **kw):
        return self.op(q, lambda e: e.dma_start(out=out, in_=in_, **kw), R, W, dma=True)

    def emit(self):
        nc = self.nc
        ops = self.ops
        pos = {o["uid"]: i for i, o in enumerate(ops)}
        for i, o in enumerate(ops):
            o["deps"] = set(pos[d] for d in o["deps"])
            o["raw"] = set(pos[d] for d in o["raw"])
            assert all(d < i for d in o["deps"]), "stream merge broke dependency order"
        eng_idx = {e: 0 for e in ENGS}
        dma_cnt = [0] * N_DMA_SEMS
        dma_last = [None] * N_DMA_SEMS
        for i, o in enumerate(ops):
            if o["dma"]:
                if o["eng"] == "pool":
                    k = N_DMA_SEMS - NSW + self.dma_rr_sw % NSW
                    self.dma_rr_sw += 1
                else:
                    k = self.dma_rr % (N_DMA_SEMS - NSW)
                    self.dma_rr += 1
                if dma_last[k] is not None:
                    o["deps"].add(dma_last[k])
                dma_last[k] = i
                dma_cnt[k] += 1
                o["tok"] = ("d%d" % k, dma_cnt[k])
            else:
                eng_idx[o["eng"]] += 1
                o["tok"] = (o["eng"], eng_idx[o["eng"]])
        eclock = {e: {} for e in ENGS}
        for i, o in enumerate(ops):
            e = o["eng"]
            ck = eclock[e]
            wm = {}
            for d in sorted(o["deps"]):
                od = ops[d]
                s, v = od["tok"]
                if (not od["dma"]) and od["eng"] == e and (e == "pe" or d not in o["raw"]):
                    continue
                if ck.get(s, 0) >= v:
                    continue
                if wm.get(s, 0) < v:
                    wm[s] = v
                for s2, v2 in od["clock"].items():
                    if ck.get(s2, 0) < v2:
                        ck[s2] = v2
            o["waits"] = wm
            c2 = dict(ck)
            c2[o["tok"][0]] = o["tok"][1]
            o["clock"] = c2
        need = {e: set() for e in ENGS}
        for o in ops:
            for s, v in o["waits"].items():
                if s in need:
                    need[s].add(v)
        remap = {e: {v: k + 1 for k, v in enumerate(sorted(need[e]))} for e in ENGS}
        self.stats = {e: (eng_idx[e], len(need[e])) for e in ENGS}
        for o in ops:
            o.pop("clock", None)
        with contextlib.ExitStack() as st:
            sems = {}
            for e in ENGS:
                sems[e] = st.enter_context(nc.semaphore("s_" + e))
            for k in range(N_DMA_SEMS):
                if dma_cnt[k]:
                    sems["d%d" % k] = st.enter_context(nc.semaphore("s_d%d" % k))
            block = st.enter_context(nc.Block())
            per = {e: [o for o in ops if o["eng"] == e] for e in ENGS}
            final_dma = [("d%d" % k, dma_cnt[k] * 16) for k in range(N_DMA_SEMS) if dma_cnt[k]]

            def run(engname, eng):
                for o in per[engname]:
                    for s, v in o["waits"].items():
                        if s in remap:
                            eng.wait_ge(sems[s], remap[s][v])
                        else:
                            eng.wait_ge(sems[s], v * 16)
                    ins = o["fn"](eng)
                    s, v = o["tok"]
                    if o["dma"]:
                        ins.then_inc(sems[s], 16)
                    elif v in remap[s]:
                        ins.then_inc(sems[s], 1)
                if engname == "sp":
                    for s, v in final_dma:
                        eng.wait_ge(sems[s], v)

            @block.tensor
            def _(eng):
                run("pe", eng)

            @block.scalar
            def _(eng):
                run("act", eng)

            @block.vector
            def _(eng):
                run("dve", eng)

            @block.gpsimd
            def _(eng):
                run("pool", eng)

            @block.sync
            def _(eng):
                run("sp", eng)


class Arena:
    def __init__(self, ap, words):
        self.ap = ap
        self.words = words
        self.top = 0
        self.live = []
        self.dead = []

    def mark(self):
        return (self.top, len(self.live))

    def release(self, m):
        top, n = m
        self.dead.extend(self.live[n:])
        del self.live[n:]
        self.top = top

    def alloc(self, shape, dt=F32, nbufs=1, name=""):
        per = int(np.prod(shape[1:]))
        words = (per * (2 if dt == BF16 else 4) + 3) // 4
        words = (words + 7) // 8 * 8
        s, e = self.top, self.top + words
        assert e <= self.words, "arena overflow %s %d > %d" % (name, e, self.words)
        self.top = e
        bufs = [Buf(name) for _ in range(nbufs)]
        keep = []
        for (ds, de, db) in self.dead:
            if ds < e and s < de:
                for ob in db:
                    for nb in bufs:
                        nb.rd.extend(ob.rd)
                        if ob.lw is not None:
                            nb.rd.append(ob.lw)
            keep.append((ds, de, db))
        self.dead = keep
        self.live.append((s, e, bufs))
        v = self.ap[0:shape[0], s:e]
        if dt == BF16:
            v = v.bitcast(BF16)[:, 0:per]
        elif dt == I32:
            v = v.bitcast(I32)[:, 0:per]
        else:
            v = v[:, 0:per]
        if len(shape) == 3:
            v = v.rearrange("p (a b) -> p a b", b=shape[2])
        elif len(shape) == 4:
            v = v.rearrange("p (a b c) -> p a b c", b=shape[2], c=shape[3])
        return (v, bufs[0]) if nbufs == 1 else (v, bufs)


def build(debug=None, stop=None, nqt=NT, nseq=SEQ_PER_CORE, gstop=0):
    nc = bass.Bass("TRN2", target_bir_lowering=False)

    def din(name, shape, dt=F32):
        return nc.dram_tensor(name, list(shape), dt, kind="ExternalInput").ap()

    x = din("x", [SEQ_PER_CORE, L, D])
    rel_bias = din("rel_bias", [32, 8])
    ab_w_in = din("ab_w_in", [1, D, 2644])
    gla_gate_w2 = din("gla_gate_w2", [1, 16, 256])
    gla_gate_b = din("gla_gate_b", [1, 256])
    gla_norm_g = din("gla_norm_g", [1, 512])
    ab_w_out = din("ab_w_out", [1, D, D])
    ln_mix_g = din("ln_mix_g", [2, D])
    ln_mix_b = din("ln_mix_b", [2, D])
    s5_w_in = din("s5_w_in", [1, D, D])
    s5_lam_re = din("s5_lam_re", [1, 64, 64])
    s5_lam_im = din("s5_lam_im", [1, 64, 64])
    s5_log_dt = din("s5_log_dt", [1, 64])
    s5_b_re = din("s5_b_re", [1, 64, 64, 16])
    s5_b_im = din("s5_b_im", [1, 64, 64, 16])
    s5_c_re = din("s5_c_re", [1, 64, 16, 64])
    s5_c_im = din("s5_c_im", [1, 64, 16, 64])
    s5_d = din("s5_d", [1, D])
    s5_glu_w1 = din("s5_glu_w1", [1, D, D])
    s5_glu_w2 = din("s5_glu_w2", [1, D, D])
    s5_w_out = din("s5_w_out", [1, D, D])
    gT_dram = nc.dram_tensor("gT_dram", [SEQ_PER_CORE, 128, 8, L], BF16, kind="Internal").ap()
    ln_ffn_g = din("ln_ffn_g", [2, D])
    ln_ffn_b = din("ln_ffn_b", [2, D])
    moe_r_coarse = din("moe_r_coarse", [2, D, 4])
    moe_rb_coarse = din("moe_rb_coarse", [2, 4])
    moe_r_fine = din("moe_r_fine", [2, 4, D, 4])
    moe_rb_fine = din("moe_rb_fine", [2, 4, 4])
    moe_w_gate = din("moe_w_gate", [2, 4, 4, D, 512])
    moe_w_up = din("moe_w_up", [2, 4, 4, D, 512])
    moe_w_down = din("moe_w_down", [2, 4, 4, 512, D])
    wgb = nc.dram_tensor("wgb", [2, 4, 4, D, 512], BF16, kind="Internal").ap()
    wub = nc.dram_tensor("wub", [2, 4, 4, D, 512], BF16, kind="Internal").ap()
    wdb = nc.dram_tensor("wdb", [2, 4, 4, 512, D], BF16, kind="Internal").ap()
    NSLOT = NTILE * 512
    xs_dram = nc.dram_tensor("xs_dram", [NSLOT, D], BF16, kind="Internal").ap()
    ys_dram = nc.dram_tensor("ys_dram", [NSLOT, D], F32, kind="Internal").ap()
    y = nc.dram_tensor("y", [SEQ_PER_CORE, L, D], F32, kind="ExternalOutput").ap()
    hres = nc.dram_tensor("hres", [SEQ_PER_CORE * L, D], F32, kind="Internal").ap()

    st = contextlib.ExitStack()
    with st:
        AW = 53200
        arena_t = st.enter_context(nc.sbuf_tensor("arena", [128, AW], F32))
        A = Arena(arena_t[:, :], AW)
        pbanks = [st.enter_context(nc.psum_tensor("pb%d" % i, [128, 512], F32))[:, :] for i in range(8)]
        pbuf = [Buf("pb%d" % i) for i in range(8)]
        P = Prog(nc)
        hres_b = [Buf("hres%d" % i) for i in range(SEQ_PER_CORE * NT)]
        y_b = Buf("y")

        def pbf(i):
            return pbanks[i].bitcast(BF16)

        rr = {"cp": 0}

        def copy_rr(out, in_, R, W):
            rr["cp"] += 1
            if rr["cp"] % 2:
                P.op("act", lambda e: e.copy(out=out, in_=in_), R=R, W=W)
            else:
                P.op("dve", lambda e: e.tensor_copy(out=out, in_=in_), R=R, W=W)

        ident, b_ident = A.alloc([128, 128], F32, name="ident")
        identb, b_identb = A.alloc([128, 128], BF16, name="identb")
        P.op("pool", lambda e: e.memset(ident, 1.0), W=[b_ident])
        P.op("pool", lambda e: e.affine_select(out=ident, in_=ident, pattern=[[-1, 128]], compare_op=ALU.is_equal,
                                               fill=0.0, base=0, channel_multiplier=1), R=[b_ident], W=[b_ident])
        P.op("dve", lambda e: e.tensor_copy(out=identb, in_=ident), R=[b_ident], W=[b_identb])

        def wload(shape_cols, src, name):
            t, b = A.alloc([128, 8, shape_cols], BF16, name=name)
            return t, b

        def wdma(dst, b, src2d):
            P.dma(dst, src2d.rearrange("(kc p) n -> p kc n", p=128), W=[b], q="pool")

        def layer_norm_tile(r, b_r, g_bc, b_bc, b_gb, out, b_out, b_bb=None, tmps=None):
            if tmps is not None:
                stt, b_st, mv, b_mv, sd, b_sd = tmps
            else:
                stt, b_st = A.alloc([128, 2, 6], F32, name="lnst")
                mv, b_mv = A.alloc([128, 2], F32, name="lnmv")
            for hh in range(2):
                P.op("dve", (lambda hh: lambda e: e.bn_stats(out=stt[:, hh, :], in_=r[:, hh * 512:(hh + 1) * 512]))(hh),
                     R=[b_r], W=[b_st])
            P.op("dve", lambda e: e.bn_aggr(out=mv, in_=stt.rearrange("p a b -> p (a b)")), R=[b_st], W=[b_mv])
            if tmps is None:
                sd, b_sd = A.alloc([128, 2], F32, name="lnsd")
            P.op("act", lambda e: e.activation(out=sd[:, 0:1], in_=mv[:, 1:2], func=AF.Sqrt, bias=eps_t[:, 0:1], scale=1.0),
                 R=[b_mv, b_eps], W=[b_sd])
            P.op("dve", lambda e: e.reciprocal(out=sd[:, 1:2], in_=sd[:, 0:1]), R=[b_sd], W=[b_sd])
            P.op("dve", lambda e: e.tensor_scalar(out=r, in0=r, scalar1=mv[:, 0:1], scalar2=sd[:, 1:2],
                                                  op0=ALU.subtract, op1=ALU.mult), R=[b_r, b_mv, b_sd], W=[b_r])
            P.op("pool", lambda e: e.tensor_tensor(out=r, in0=r, in1=g_bc, op=ALU.mult), R=[b_r, b_gb], W=[b_r])
            P.op("pool", lambda e: e.tensor_tensor(out=out, in0=r, in1=b_bc, op=ALU.add), R=[b_r, b_gb] + ([b_bb] if b_bb is not None else []), W=[b_out])

        bar_t, b_bar = A.alloc([128, 8], F32, name="bar")

        def barrier():
            bufs = [b for (_, _, bs) in A.live + A.dead for b in bs] + pbuf + hres_b + [b_bar]
            seen = {}
            for b in bufs:
                seen[id(b)] = b
            bufs = list(seen.values())
            P.op("dve", lambda e: e.memset(bar_t, 0.0), R=bufs, W=bufs)

        eps_t, b_eps = A.alloc([128, 1], F32, name="eps")
        P.op("pool", lambda e: e.memset(eps_t, EPS), W=[b_eps])
        halfpi, b_hp = A.alloc([128, 1], F32, name="halfpi")
        P.op("pool", lambda e: e.memset(halfpi, math.pi / 2), W=[b_hp])

        m_l0 = A.mark()
        W0 = ab_w_in[0]
        Wq, b_Wq = wload(512, None, "Wq")
        Wiq, b_Wiq = wload(256, None, "Wiq")
        Wiw, b_Wiw = wload(4, None, "Wiw")
        Wk, b_Wk = wload(256, None, "Wk")
        Wik, b_Wik = wload(128, None, "Wik")
        Wv, b_Wv = wload(128, None, "Wv")
        Wbq, b_Wbq = wload(256, None, "Wbq")
        Wbk, b_Wbk = wload(256, None, "Wbk")
        Wbv, b_Wbv = wload(512, None, "Wbv")
        Wbg, b_Wbg = wload(16, None, "Wbg")
        Wbr, b_Wbr = wload(512, None, "Wbr")
        Wo, b_Wo = wload(1024, None, "Wo")
        wdma(Wq, b_Wq, W0[:, 0:512])
        for g in range(2):
            for r2 in range(2):
                c0 = (2 * g + r2) * 64
                P.dma(Wk[:, :, c0:c0 + 64], W0[:, 512 + g * 64:512 + (g + 1) * 64].rearrange("(kc p) n -> p kc n", p=128),
                      W=[b_Wk], q="pool")
        wdma(Wv, b_Wv, W0[:, 640:768])
        wdma(Wiq, b_Wiq, W0[:, 768:1024])
        for r2 in range(2):
            P.dma(Wik[:, :, r2 * 64:(r2 + 1) * 64], W0[:, 1024:1088].rearrange("(kc p) n -> p kc n", p=128),
                  W=[b_Wik], q="pool")
        wdma(Wiw, b_Wiw, W0[:, 1088:1092])
        wdma(Wbq, b_Wbq, W0[:, 1092:1348])
        wdma(Wbk, b_Wbk, W0[:, 1348:1604])
        wdma(Wbv, b_Wbv, W0[:, 1604:2116])
        wdma(Wbg, b_Wbg, W0[:, 2116:2132])
        wdma(Wbr, b_Wbr, W0[:, 2132:2644])
        wdma(Wo, b_Wo, ab_w_out[0])

        LG, b_LG = A.alloc([128, 1024], F32, name="LG")
        LB, b_LB = A.alloc([128, 1024], F32, name="LB")
        P.dma(LG, ln_mix_g[0].partition_broadcast(128), W=[b_LG])
        P.dma(LB, ln_mix_b[0].partition_broadcast(128), W=[b_LB])
        NG, b_NG = A.alloc([128, 512], F32, name="NG")
        P.dma(NG, gla_norm_g[0].partition_broadcast(128), W=[b_NG])
        G2, b_G2 = A.alloc([32, 256], F32, name="G2")
        P.op("pool", lambda e: e.memset(G2, 0.0), W=[b_G2])
        P.dma(G2[0:16, :], gla_gate_w2[0], W=[b_G2])
        P.dma(G2[16:17, :], gla_gate_b[0:1, :], W=[b_G2])

        TRI, b_TRI = A.alloc([128, 128], F32, name="TRI")
        TRI2, b_TRI2 = A.alloc([128, 128], F32, name="TRI2")
        MASKT, b_MASKT = A.alloc([128, 128], F32, name="MASKT")
        for (T_, b_, val, strict) in ((TRI, b_TRI, -1.0 / 16, False), (MASKT, b_MASKT, 1.0, False), (TRI2, b_TRI2, -1.0 / 16, True)):
            P.op("pool", (lambda T_, val: lambda e: e.memset(T_, val))(T_, val), W=[b_])
            if not strict:
                P.op("pool", (lambda T_: lambda e: e.affine_select(out=T_, in_=T_, pattern=[[1, 128]], compare_op=ALU.is_ge,
                                                                   fill=0.0, base=0, channel_multiplier=-1))(T_), R=[b_], W=[b_])
                P.op("pool", (lambda T_: lambda e: e.memset(T_[0:64, 64:128], 0.0))(T_), R=[b_], W=[b_])
            else:
                P.op("pool", (lambda T_: lambda e: e.affine_select(out=T_, in_=T_, pattern=[[-1, 128]], compare_op=ALU.is_ge,
                                                                   fill=0.0, base=-1, channel_multiplier=1))(T_), R=[b_], W=[b_])
                P.op("pool", (lambda T_: lambda e: e.memset(T_[64:128, 0:64], 0.0))(T_), R=[b_], W=[b_])

        Bp, b_Bp = A.alloc([128, 8, 256], F32, name="Bp")
        m_tmp = A.mark()
        RB, b_RB = A.alloc([128, 32, 8], F32, name="RB")
        DL, b_DL = A.alloc([128, 32, 8], F32, name="DL")
        P.dma(RB.rearrange("p a b -> p (a b)"), rel_bias.rearrange("a b -> (a b)").partition_broadcast(128), W=[b_RB])
        P.op("dve", lambda e: e.tensor_tensor(out=DL[:, 1:32, :], in0=RB[:, 1:32, :], in1=RB[:, 0:31, :], op=ALU.subtract),
             R=[b_RB], W=[b_DL])
        P.op("dve", lambda e: e.tensor_tensor(out=DL[:, 0:1, :], in0=RB[:, 0:1, :], in1=RB[:, 31:32, :], op=ALU.subtract),
             R=[b_RB], W=[b_DL])
        Dt, b_Dt = A.alloc([128, 256], F32, name="Dt")
        Ib, b_Ib = A.alloc([128, 31, 256], BF16, name="Ib")
        P.op("pool", lambda e: e.iota(out=Dt, pattern=[[-1, 256]], base=128, channel_multiplier=1,
                                      allow_small_or_imprecise_dtypes=True), W=[b_Dt])
        for bi, sv in enumerate(T5_STARTS):
            P.op("dve", (lambda bi, sv: lambda e: e.tensor_scalar(out=Ib[:, bi, :], in0=Dt, scalar1=float(sv) - 0.5, scalar2=None,
                                                                  op0=ALU.is_ge))(bi, sv), R=[b_Dt], W=[b_Ib])
        for h in range(8):
            P.op("dve", (lambda h: lambda e: e.tensor_scalar(out=Bp[:, h, :], in0=Ib[:, 0, :], scalar1=DL[:, 1, h:h + 1],
                                                             scalar2=DL[:, 0, h:h + 1], op0=ALU.mult, op1=ALU.add))(h),
                 R=[b_Ib, b_DL], W=[b_Bp])
            for bi in range(1, 31):
                P.op("dve", (lambda h, bi: lambda e: e.scalar_tensor_tensor(out=Bp[:, h, :], in0=Ib[:, bi, :],
                                                                           scalar=DL[:, bi + 1, h:h + 1], in1=Bp[:, h, :],
                                                                           op0=ALU.mult, op1=ALU.add))(h, bi),
                     R=[b_Ib, b_DL, b_Bp], W=[b_Bp])
        A.release(m_tmp)

        bg1, b_bg1 = A.alloc([32, 128], F32, name="bg1")
        P.op("pool", lambda e: e.memset(bg1, 1.0), W=[b_bg1])

        hT, b_hT = A.alloc([128, 8, L], BF16, nbufs=NT, name="hT")
        kTd, b_kTd = A.alloc([128, 2, L], BF16, name="kTd")
        ikT2, b_ikT2 = A.alloc([128, L], BF16, name="ikT2")
        vtok, b_vtok = A.alloc([128, NT, 128], BF16, name="vtok")
        xs, b_xs = A.alloc([128, 1, 1024], F32, nbufs=1, name="xs")
        b_xs = [b_xs, b_xs]
        sc, b_sc = A.alloc([128, L], F32, name="sc")
        rt, b_rt = A.alloc([128, 2, 512], F32, nbufs=2, name="rt")
        selm2, b_selm2 = A.alloc([128, 2, L], BF16, nbufs=2, name="selm1")
        lg2, b_lg2 = A.alloc([128, 2, L], F32, nbufs=2, name="lg")
        pp2, b_pp2 = A.alloc([128, 2, L], BF16, nbufs=2, name="pp")
        pT2, b_pT2 = A.alloc([128, 1, L], BF16, nbufs=1, name="pT")
        b_pT2 = [b_pT2, b_pT2]
        qTt2, b_qTt2 = A.alloc([128, 2, 4, 128], BF16, nbufs=2, name="qTt")
        iqTt, b_iqTt = A.alloc([128, 2, 128], BF16, name="iqTt")
        iwt, b_iwt = A.alloc([128, 4], F32, name="iwt")
        m8, b_m8 = A.alloc([128, 8], F32, name="m8")
        MB = 20
        TOPK_BISECT = os.environ.get("TOPK", "bisect") == "bisect"
        tkb, b_tkb = A.alloc([128, 16], F32, name="tkb")
        Wbis, b_Wbis = A.alloc([128, 24], F32, name="Wbis")
        POW2, b_POW2 = A.alloc([128, 24], F32, name="POW2")
        for k in range(MB):
            P.op("pool", (lambda k: lambda e: e.memset(POW2[:, k:k + 1], 2.0 ** -(k + 1)))(k), W=[b_POW2])
        sm2, b_sm2 = A.alloc([128, 2, 4], F32, nbufs=2, name="sm")
        oa, b_oa = A.alloc([128, 1024], BF16, name="oa")
        oT, b_oT = A.alloc([128, 8, 128], BF16, name="oT")
        lap, b_lap = A.alloc([128, 256], F32, name="lap")
        EcT, b_EcT = A.alloc([128, 2, 128], F32, name="EcT")
        EnT, b_EnT = A.alloc([128, 2, 128], F32, name="EnT")
        Er, b_Er = A.alloc([128, 256], F32, name="Er")
        qdT, b_qdT = A.alloc([128, 2, 128], BF16, name="qdT")
        kiT, b_kiT = A.alloc([128, 2, 128], BF16, name="kiT")
        kend, b_kend = A.alloc([128, 256], BF16, name="kend")
        vt, b_vt = A.alloc([128, 512], BF16, name="vt")
        sbr, b_sbr = A.alloc([128, 512], F32, name="sbr")
        scT, b_scT = A.alloc([128, 4, 128], BF16, name="scT")
        Sf, b_Sf = A.alloc([128, 2, 128], F32, name="Sf")
        Sb, b_Sb = A.alloc([128, 2, 128], BF16, name="Sb")
        on, b_on = A.alloc([128, 512], F32, name="on")
        gst, b_gst = A.alloc([128, 4, 6], F32, name="gst")
        gmv, b_gmv = A.alloc([128, 4, 2], F32, name="gmv")
        gsd, b_gsd = A.alloc([128, 4, 2], F32, name="gsd")
        rr_, b_rr = A.alloc([128, 1024], F32, name="r")

        wcast_b = [[Buf("wc%d_%d" % (l, k)) for k in range(48)] for l in range(2)]
        precast_state = {"i": 0}

        def precast_next():
            i = precast_state["i"]
            if i >= 32:
                return
            precast_state["i"] = i + 1
            l_, g_, e_ = i // 16, (i % 16) // 4, i % 4
            k_ = (i % 16) * 3
            P.dma(wgb[l_, g_, e_], moe_w_gate[l_, g_, e_], W=[wcast_b[l_][k_]], q="pool")
            P.dma(wub[l_, g_, e_], moe_w_up[l_, g_, e_], W=[wcast_b[l_][k_ + 1]], q="pool")
            P.dma(wdb[l_, g_, e_], moe_w_down[l_, g_, e_], W=[wcast_b[l_][k_ + 2]], q="pool")

        for s in range(nseq):
            if stop == "consts":
                break
            for tt in range(NT):
                par = 0
                P.dma(xs[:, par, :], x[s, tt * 128:(tt + 1) * 128, :], W=[b_xs[par]])
                for g in range(2):
                    bk_ = (2 * tt + g) % 2
                    for c in range(4):
                        kc = 4 * g + c
                        P.op("pe", (lambda par, kc, c, bk_: lambda e: e.transpose(out=pbanks[bk_][:, c * 128:(c + 1) * 128],
                                                                                in_=xs[:, par, kc * 128:(kc + 1) * 128], identity=ident))(par, kc, c, bk_),
                             R=[b_xs[par], b_ident], W=[pbuf[bk_]])
                    copy_rr(hT[:, 4 * g:4 * g + 4, tt * 128:(tt + 1) * 128],
                            pbanks[bk_].rearrange("p (a b) -> p a b", b=128), [pbuf[bk_]], [b_hT[tt]])
            if stop == "s1":
                break
            for n in range(4):
                tsl = slice(n * 512, (n + 1) * 512)
                hb = b_hT[4 * n:4 * n + 4]
                for g in range(3):
                    bk_ = 2 + (n * 3 + g) % 2
                    Wt, bW, c0 = (Wk, b_Wk, g * 128) if g < 2 else (Wik, b_Wik, 0)
                    for kc in range(8):
                        P.op("pe", (lambda Wt, c0, kc, bk_, tsl: lambda e: e.matmul(out=pbanks[bk_], lhsT=Wt[:, kc, c0:c0 + 128], rhs=hT[:, kc, tsl],
                                                                                 start=(kc == 0), stop=(kc == 7)))(Wt, c0, kc, bk_, tsl),
                             R=[bW] + hb, W=[pbuf[bk_]])
                    dst = kTd[:, g, tsl] if g < 2 else ikT2[:, tsl]
                    copy_rr(dst, pbanks[bk_], [pbuf[bk_]], [b_kTd if g < 2 else b_ikT2])
            for g4 in range(4):
                bk_ = 2 + g4 % 2
                for c in range(4):
                    tt = 4 * g4 + c
                    for kc in range(8):
                        P.op("pe", (lambda tt, c, kc, bk_: lambda e: e.matmul(out=pbanks[bk_][:, c * 128:(c + 1) * 128], lhsT=hT[:, kc, tt * 128:(tt + 1) * 128],
                                                                           rhs=Wv[:, kc, :], start=(kc == 0), stop=(kc == 7)))(tt, c, kc, bk_),
                             R=[b_Wv, b_hT[tt]], W=[pbuf[bk_]])
                copy_rr(vtok[:, 4 * g4:4 * g4 + 4, :], pbanks[bk_].rearrange("p (a b) -> p a b", b=128), [pbuf[bk_]], [b_vtok])
            P.op("pool", lambda e: e.memset(Sf, 0.0), W=[b_Sf])
            P.op("pool", lambda e: e.memset(Sb, 0.0), W=[b_Sb])

            if stop == "s2":
                break
            def pre(qt):
                t0 = qt * 128
                S = (qt + 1) * 128
                tq = slice(t0, t0 + 128)
                bh = b_hT[qt]
                nch = (S + 511) // 512
                qTt, b_qTt = qTt2[:, qt % 2], b_qTt2[qt % 2]
                selm1, b_selm1 = selm2[:, qt % 2, :], b_selm2[qt % 2]
                for c in range(4):
                    for kc in range(8):
                        P.op("pe", (lambda c, kc, tq: lambda e: e.matmul(out=pbanks[0][:, c * 128:(c + 1) * 128], lhsT=Wq[:, kc, c * 128:(c + 1) * 128],
                                                                      rhs=hT[:, kc, tq], start=(kc == 0), stop=(kc == 7)))(c, kc, tq),
                             R=[b_Wq, bh], W=[pbuf[0]])
                P.op("act", lambda e: e.activation(out=qTt, in_=pbanks[0].rearrange("p (a b) -> p a b", b=128), func=AF.Copy, scale=0.125),
                     R=[pbuf[0]], W=[b_qTt])
                for c in range(2):
                    for kc in range(8):
                        P.op("pe", (lambda c, kc, tq: lambda e: e.matmul(out=pbanks[1][:, c * 128:(c + 1) * 128], lhsT=Wiq[:, kc, c * 128:(c + 1) * 128],
                                                                      rhs=hT[:, kc, tq], start=(kc == 0), stop=(kc == 7)))(c, kc, tq),
                             R=[b_Wiq, bh], W=[pbuf[1]])
                for kc in range(8):
                    P.op("pe", (lambda kc, tq: lambda e: e.matmul(out=pbanks[1][:, 256:260], lhsT=hT[:, kc, tq], rhs=Wiw[:, kc, :],
                                                               start=(kc == 0), stop=(kc == 7)))(kc, tq), R=[b_Wiw, bh], W=[pbuf[1]])
                P.op("act", lambda e: e.activation(out=iqTt, in_=pbanks[1][:, 0:256].rearrange("p (a b) -> p a b", b=128), func=AF.Copy, scale=0.125),
                     R=[pbuf[1]], W=[b_iqTt])
                P.op("act", lambda e: e.activation(out=iwt, in_=pbanks[1][:, 256:260], func=AF.Copy, scale=0.5), R=[pbuf[1]], W=[b_iwt])
                nch = (S + 511) // 512
                k_ = 0
                for j in range(nch):
                    w_ = min(512, S - j * 512)
                    cs = slice(j * 512, j * 512 + w_)
                    for hi in range(4):
                        c, base = hi // 2, (hi % 2) * 64
                        bk_ = 2 + hi % 2
                        par = k_ % 2
                        k_ += 1
                        P.op("pe", (lambda c, base, bk_, w_, cs: lambda e: e.matmul(out=pbanks[bk_][:, 0:w_], lhsT=iqTt[base:base + 64, c, :],
                                                                                 rhs=ikT2[base:base + 64, cs], start=True, stop=True))(c, base, bk_, w_, cs),
                             R=[b_iqTt, b_ikT2], W=[pbuf[bk_]])
                        P.op("act", (lambda par, bk_, w_: lambda e: e.activation(out=rt[:, par, 0:w_], in_=pbanks[bk_][:, 0:w_], func=AF.Relu))(par, bk_, w_),
                             R=[pbuf[bk_]], W=[b_rt[par]])
                        if hi == 0:
                            P.op("dve", (lambda par, w_, cs: lambda e: e.tensor_scalar(out=sc[:, cs], in0=rt[:, par, 0:w_], scalar1=iwt[:, 0:1], scalar2=None,
                                                                                    op0=ALU.mult))(par, w_, cs), R=[b_rt[par], b_iwt], W=[b_sc])
                        else:
                            P.op("dve", (lambda par, w_, cs, hi: lambda e: e.scalar_tensor_tensor(out=sc[:, cs], in0=rt[:, par, 0:w_], scalar=iwt[:, hi:hi + 1],
                                                                                               in1=sc[:, cs], op0=ALU.mult, op1=ALU.add))(par, w_, cs, hi),
                                 R=[b_rt[par], b_iwt, b_sc], W=[b_sc])
                P.op("pool", (lambda S: lambda e: e.affine_select(out=sc[:, S - 128:S], in_=sc[:, S - 128:S], pattern=[[-1, 128]], compare_op=ALU.is_ge,
                                                                  fill=NEG, base=0, channel_multiplier=1))(S), R=[b_sc], W=[b_sc])
            def topk(qt):
                t0 = qt * 128
                S = (qt + 1) * 128
                tq = slice(t0, t0 + 128)
                bh = b_hT[qt]
                nch = (S + 511) // 512
                qTt, b_qTt = qTt2[:, qt % 2], b_qTt2[qt % 2]
                selm1, b_selm1 = selm2[:, qt % 2, :], b_selm2[qt % 2]
                if qt >= 2 and TOPK_BISECT:
                    rtb = rt.rearrange("p a b -> p (a b)").bitcast(BF16)
                    Wrt = [b_rt[0], b_rt[1]]
                    T = lambda k: tkb[:, k:k + 1]
                    P.op("dve", (lambda S: lambda e: e.tensor_reduce(out=T(0), in_=sc[:, 0:S], axis=AX.X, op=ALU.max))(S), R=[b_sc], W=[b_tkb])
                    P.op("dve", (lambda S: lambda e: e.tensor_reduce(out=T(1), in_=sc[:, 0:S - 128], axis=AX.X, op=ALU.min))(S), R=[b_sc], W=[b_tkb])
                    P.op("dve", lambda e: e.tensor_scalar(out=T(2), in0=T(1), scalar1=-1.0, scalar2=None, op0=ALU.add), R=[b_tkb], W=[b_tkb])
                    P.op("dve", lambda e: e.tensor_tensor(out=T(3), in0=T(0), in1=T(2), op=ALU.subtract), R=[b_tkb], W=[b_tkb])
                    P.op("dve", lambda e: e.tensor_scalar(out=Wbis, in0=POW2, scalar1=T(3), scalar2=None, op0=ALU.mult), R=[b_tkb, b_POW2], W=[b_Wbis])
                    for k in range(MB):
                        P.op("dve", (lambda k: lambda e: e.tensor_tensor(out=T(4), in0=T(2), in1=Wbis[:, k:k + 1], op=ALU.add))(k), R=[b_tkb, b_Wbis], W=[b_tkb])
                        P.op("dve", (lambda S: lambda e: e.tensor_scalar(out=rtb[:, 0:S], in0=sc[:, 0:S], scalar1=T(4), scalar2=None, op0=ALU.is_gt, op1=ALU.add,
                                                                         accum_out=T(5)))(S), R=[b_sc, b_tkb], W=Wrt + [b_tkb])
                        P.op("dve", lambda e: e.tensor_scalar(out=T(6), in0=T(5), scalar1=255.5, scalar2=None, op0=ALU.is_ge), R=[b_tkb], W=[b_tkb])
                        P.op("dve", (lambda k: lambda e: e.scalar_tensor_tensor(out=T(2), in0=T(6), scalar=Wbis[:, k:k + 1], in1=T(2), op0=ALU.mult, op1=ALU.add))(k),
                             R=[b_tkb, b_Wbis], W=[b_tkb])
                    P.op("dve", lambda e: e.tensor_tensor(out=T(7), in0=T(2), in1=Wbis[:, MB - 1:MB], op=ALU.add), R=[b_tkb, b_Wbis], W=[b_tkb])
                    P.op("dve", (lambda S: lambda e: e.tensor_scalar(out=selm1[:, 0:S], in0=sc[:, 0:S], scalar1=T(7), scalar2=None, op0=ALU.is_gt, op1=ALU.add,
                                                                     accum_out=T(8)))(S), R=[b_sc, b_tkb], W=[b_selm1, b_tkb])
                    P.op("dve", (lambda S: lambda e: e.tensor_scalar(out=rtb[:, 0:S], in0=sc[:, 0:S], scalar1=T(2), scalar2=None, op0=ALU.is_gt))(S),
                         R=[b_sc, b_tkb], W=Wrt)
                    P.op("dve", (lambda S: lambda e: e.tensor_tensor(out=rtb[:, 0:S], in0=rtb[:, 0:S], in1=selm1[:, 0:S], op=ALU.subtract))(S), R=Wrt + [b_selm1], W=Wrt)
                    P.op("dve", lambda e: e.tensor_scalar(out=T(9), in0=T(8), scalar1=-1.0, scalar2=256.0, op0=ALU.mult, op1=ALU.add), R=[b_tkb], W=[b_tkb])
                    P.op("dve", (lambda S: lambda e: e.tensor_tensor_scan(out=sc[:, 0:S], data0=rtb[:, 0:S], data1=rtb[:, 0:S], initial=0.0, op0=ALU.add, op1=ALU.bypass))(S),
                         R=Wrt, W=[b_sc])
                    P.op("dve", (lambda S: lambda e: e.scalar_tensor_tensor(out=rtb[:, 0:S], in0=sc[:, 0:S], scalar=T(9), in1=rtb[:, 0:S], op0=ALU.is_le, op1=ALU.mult))(S),
                         R=[b_sc, b_tkb] + Wrt, W=Wrt)
                    P.op("dve", (lambda S: lambda e: e.scalar_tensor_tensor(out=selm1[:, 0:S], in0=rtb[:, 0:S], scalar=-1.0, in1=selm1[:, 0:S], op0=ALU.add, op1=ALU.add))(S),
                         R=Wrt + [b_selm1], W=[b_selm1])
                elif qt >= 2:
                    for it in range(32):
                        P.op("dve", (lambda S: lambda e: e.max(out=m8, in_=sc[:, 0:S]))(S), R=[b_sc], W=[b_m8])
                        P.op("dve", (lambda S: lambda e: e.match_replace(out=sc[:, 0:S], in_to_replace=m8, in_values=sc[:, 0:S], imm_value=-3e38))(S),
                             R=[b_sc, b_m8], W=[b_sc])
                    P.op("dve", (lambda S: lambda e: e.tensor_scalar(out=selm1[:, 0:S], in0=sc[:, 0:S], scalar1=-1e37, scalar2=1.0,
                                                                     op0=ALU.is_le, op1=ALU.subtract))(S), R=[b_sc], W=[b_selm1])
                else:
                    P.op("dve", (lambda S: lambda e: e.tensor_scalar(out=selm1[:, 0:S], in0=sc[:, 0:S], scalar1=-1e29, scalar2=1.0,
                                                                     op0=ALU.is_ge, op1=ALU.subtract))(S), R=[b_sc], W=[b_selm1])
            def rest(qt):
                t0 = qt * 128
                S = (qt + 1) * 128
                tq = slice(t0, t0 + 128)
                bh = b_hT[qt]
                nch = (S + 511) // 512
                qTt, b_qTt = qTt2[:, qt % 2], b_qTt2[qt % 2]
                selm1, b_selm1 = selm2[:, qt % 2, :], b_selm2[qt % 2]
                if os.environ.get("PRECAST", "1") == "1":
                    precast_next()
                P.dma(xs[:, 0, :], x[s, t0:t0 + 128, :], W=[b_xs[0]])
                def hbufs(h):
                    hp = h % 2
                    return (lg2[:, hp, :], b_lg2[hp], pp2[:, hp, :], b_pp2[hp], pT2[:, 0, :], b_pT2[hp], sm2[:, hp, :], b_sm2[hp])

                def stageA(h):
                    c, base, g = h // 2, (h % 2) * 64, h // 4
                    hp = h % 2
                    lg, b_lg, pp, b_pp, pT, b_pT, sm, b_sm = hbufs(h)
                    for j in range(nch):
                        w_ = min(512, S - j * 512)
                        cs = slice(j * 512, j * 512 + w_)
                        bk_ = (2 if j % 2 == 0 else 0) + (h % 2)
                        P.op("pe", (lambda c, base, g, bk_, w_, cs: lambda e: e.matmul(out=pbanks[bk_][:, 0:w_], lhsT=qTt[base:base + 64, c, :],
                                                                                    rhs=kTd[base:base + 64, g, cs], start=True, stop=True))(c, base, g, bk_, w_, cs),
                             R=[b_qTt, b_kTd], W=[pbuf[bk_]])
                        P.op("dve", (lambda bk_, w_, cs, lg=lg: lambda e: e.scalar_tensor_tensor(out=lg[:, cs], in0=selm1[:, cs], scalar=1e30, in1=pbanks[bk_][:, 0:w_],
                                                                                       op0=ALU.mult, op1=ALU.add))(bk_, w_, cs),
                             R=[b_selm1, pbuf[bk_]], W=[b_lg])
                    if qt == 0:
                        P.op("pool", (lambda h, lg=lg: lambda e: e.tensor_tensor(out=lg[:, 0:128], in0=lg[:, 0:128], in1=Bp[:, h, 128:256], op=ALU.add))(h),
                             R=[b_lg, b_Bp], W=[b_lg])
                    else:
                        P.op("pool", (lambda h, S, lg=lg: lambda e: e.tensor_tensor(out=lg[:, S - 256:S], in0=lg[:, S - 256:S], in1=Bp[:, h, :], op=ALU.add))(h, S),
                             R=[b_lg, b_Bp], W=[b_lg])
                    P.op("dve", (lambda S, lg=lg, sm=sm: lambda e: e.tensor_reduce(out=sm[:, 0:1], in_=lg[:, 0:S], axis=AX.X, op=ALU.max, negate=True))(S),
                         R=[b_lg], W=[b_sm])
                    P.op("act", (lambda S, lg=lg, sm=sm, pp=pp: lambda e: e.activation(out=pp[:, 0:S], in_=lg[:, 0:S], func=AF.Exp, bias=sm[:, 0:1], scale=1.0,
                                                                  accum_out=sm[:, 1:2]))(S), R=[b_lg, b_sm], W=[b_pp, b_sm])
                    P.op("dve", (lambda sm=sm: lambda e: e.reciprocal(out=sm[:, 2:3], in_=sm[:, 1:2]))(), R=[b_sm], W=[b_sm])
                def stageB(h):
                    c, base, g = h // 2, (h % 2) * 64, h // 4
                    hp = h % 2
                    lg, b_lg, pp, b_pp, pT, b_pT, sm, b_sm = hbufs(h)
                    nb = qt + 1
                    for g8 in range((nb + 7) // 8):
                        bk_ = 4 + hp
                        n8 = min(8, nb - g8 * 8)
                        for u in range(n8):
                            kb = g8 * 8 + u
                            P.op("pe", (lambda bk_, u, kb, pp=pp: lambda e: e.transpose(out=pbf(bk_)[:, u * 128:(u + 1) * 128], in_=pp[:, kb * 128:(kb + 1) * 128],
                                                                              identity=identb))(bk_, u, kb), R=[b_pp, b_identb], W=[pbuf[bk_]])
                        copy_rr(pT[:, g8 * 1024:g8 * 1024 + n8 * 128], pbf(bk_)[:, 0:n8 * 128], [pbuf[bk_]], [b_pT])
                    for kb in range(nb):
                        P.op("pe", (lambda h, g, kb, nb, pT=pT: lambda e: e.matmul(out=pbanks[6][:, h * 64:(h + 1) * 64], lhsT=pT[:, kb * 128:(kb + 1) * 128],
                                                                         rhs=vtok[:, kb, g * 64:(g + 1) * 64], start=(h == 0 and kb == 0), stop=(kb == nb - 1),
                                                                         skip_group_check=True))(h, g, kb, nb),
                             R=[b_pT, b_vtok], W=[pbuf[6]])
                    P.op("act", (lambda h, sm=sm: lambda e: e.activation(out=oa[:, h * 64:(h + 1) * 64], in_=pbanks[6][:, h * 64:(h + 1) * 64], func=AF.Copy,
                                                                  scale=sm[:, 2:3]))(h), R=[pbuf[6], b_sm], W=[b_oa])

                stageA(0)
                for h in range(8):
                    if h + 1 < 8:
                        stageA(h + 1)
                    stageB(h)

                if stop == "dsa":
                    return
                def ck(k):
                    if gstop == k:
                        P.mute = True
                for c in range(2):
                    for kc in range(8):
                        P.op("pe", (lambda c, kc, tq: lambda e: e.matmul(out=pbanks[0][:, c * 128:(c + 1) * 128], lhsT=Wbq[:, kc, c * 128:(c + 1) * 128],
                                                                      rhs=hT[:, kc, tq], start=(kc == 0), stop=(kc == 7)))(c, kc, tq), R=[b_Wbq, bh], W=[pbuf[0]])
                for c in range(2):
                    for kc in range(8):
                        P.op("pe", (lambda c, kc, tq: lambda e: e.matmul(out=pbanks[0][:, 256 + c * 128:256 + (c + 1) * 128], lhsT=Wbk[:, kc, c * 128:(c + 1) * 128],
                                                                      rhs=hT[:, kc, tq], start=(kc == 0), stop=(kc == 7)))(c, kc, tq), R=[b_Wbk, bh], W=[pbuf[0]])
                for kc in range(8):
                    P.op("pe", (lambda kc, tq: lambda e: e.matmul(out=pbanks[1][:, 0:256], lhsT=hT[:, kc, tq], rhs=Wbk[:, kc, :],
                                                               start=(kc == 0), stop=(kc == 7)))(kc, tq), R=[b_Wbk, bh], W=[pbuf[1]])
                for kc in range(8):
                    P.op("pe", (lambda kc, tq: lambda e: e.matmul(out=pbanks[1][0:16, 256:384], lhsT=Wbg[:, kc, :], rhs=hT[:, kc, tq],
                                                               start=(kc == 0), stop=(kc == 7)))(kc, tq), R=[b_Wbg, bh], W=[pbuf[1]])
                for kc in range(8):
                    P.op("pe", (lambda kc, tq: lambda e: e.matmul(out=pbanks[2], lhsT=hT[:, kc, tq], rhs=Wbv[:, kc, :],
                                                               start=(kc == 0), stop=(kc == 7)))(kc, tq), R=[b_Wbv, bh], W=[pbuf[2]])
                for kc in range(8):
                    P.op("pe", (lambda kc, tq: lambda e: e.matmul(out=pbanks[3], lhsT=hT[:, kc, tq], rhs=Wbr[:, kc, :],
                                                               start=(kc == 0), stop=(kc == 7)))(kc, tq), R=[b_Wbr, bh], W=[pbuf[3]])
                ck(1)
                P.op("act", lambda e: e.copy(out=bg1[0:16, :], in_=pbanks[1][0:16, 256:384]), R=[pbuf[1]], W=[b_bg1])
                P.op("act", lambda e: e.copy(out=vt, in_=pbanks[2]), R=[pbuf[2]], W=[b_vt])
                P.op("act", lambda e: e.activation(out=sbr, in_=pbanks[3], func=AF.Silu), R=[pbuf[3]], W=[b_sbr])
                ck(2)
                P.op("pe", lambda e: e.matmul(out=pbanks[4][:, 0:256], lhsT=bg1[0:17, :], rhs=G2[0:17, :], start=True, stop=True),
                     R=[b_bg1, b_G2], W=[pbuf[4]])
                P.op("act", lambda e: e.activation(out=lap, in_=pbanks[4][:, 0:256], func=AF.Exp, scale=-1.0), R=[pbuf[4]], W=[b_lap])
                P.op("act", lambda e: e.activation(out=lap, in_=lap, func=AF.Ln, bias=1.0, scale=1.0), R=[b_lap], W=[b_lap])
                ck(3)
                for c in range(2):
                    P.op("pe", (lambda c: lambda e: e.matmul(out=pbanks[5][:, c * 128:(c + 1) * 128], lhsT=lap[:, c * 128:(c + 1) * 128], rhs=TRI,
                                                             start=True, stop=True))(c), R=[b_lap, b_TRI], W=[pbuf[5]])
                P.op("pe", lambda e: e.matmul(out=pbanks[5][:, 256:512], lhsT=TRI2, rhs=lap, start=True, stop=True), R=[b_lap, b_TRI2], W=[pbuf[5]])
                ck(4)
                P.op("act", lambda e: e.activation(out=EcT, in_=pbanks[5][:, 0:256].rearrange("p (a b) -> p a b", b=128), func=AF.Exp), R=[pbuf[5]], W=[b_EcT])
                P.op("act", lambda e: e.activation(out=EnT, in_=pbanks[5][:, 0:256].rearrange("p (a b) -> p a b", b=128), func=AF.Exp, scale=-1.0),
                     R=[pbuf[5]], W=[b_EnT])
                P.op("act", lambda e: e.activation(out=Er, in_=pbanks[5][:, 256:512], func=AF.Exp), R=[pbuf[5]], W=[b_Er])
                P.op("dve", lambda e: e.scalar_tensor_tensor(out=qdT, in0=pbanks[0][:, 0:256].rearrange("p (a b) -> p a b", b=128), scalar=0.125, in1=EcT,
                                                             op0=ALU.mult, op1=ALU.mult), R=[pbuf[0], b_EcT], W=[b_qdT])
                P.op("dve", lambda e: e.tensor_tensor(out=kiT, in0=pbanks[0][:, 256:512].rearrange("p (a b) -> p a b", b=128), in1=EnT, op=ALU.mult),
                     R=[pbuf[0], b_EnT], W=[b_kiT])
                P.op("dve", lambda e: e.tensor_tensor(out=kend, in0=pbanks[1][:, 0:256], in1=Er, op=ALU.mult), R=[pbuf[1], b_Er], W=[b_kend])
                ck(5)
                for h in range(4):
                    c, base = h // 2, (h % 2) * 64
                    sb_ = 4 if h % 2 == 0 else 3
                    P.op("pe", (lambda h, c, base, sb_: lambda e: e.matmul(out=pbanks[sb_][:, c * 128:(c + 1) * 128], lhsT=kiT[base:base + 64, c, :],
                                                                          rhs=qdT[base:base + 64, c, :], start=True, stop=True))(h, c, base, sb_),
                         R=[b_kiT, b_qdT], W=[pbuf[sb_]])
                ck(55)
                for h in range(4):
                    sb_ = 4 if h % 2 == 0 else 3
                    P.op("dve", (lambda h, sb_: lambda e: e.tensor_tensor(out=scT[:, h, :], in0=pbanks[sb_][:, (h // 2) * 128:(h // 2 + 1) * 128],
                                                                         in1=MASKT, op=ALU.mult))(h, sb_), R=[pbuf[sb_], b_MASKT], W=[b_scT])
                ck(6)
                for h in range(4):
                    ob_ = 7 if h % 2 == 0 else 6
                    P.op("pe", (lambda h, ob_: lambda e: e.matmul(out=pbanks[ob_][:, (h // 2) * 128:(h // 2 + 1) * 128], lhsT=scT[:, h, :], rhs=vt[:, h * 128:(h + 1) * 128],
                                                                  start=(h < 2), stop=False, skip_group_check=True))(h, ob_), R=[b_scT, b_vt], W=[pbuf[ob_]])
                ck(7)
                for u in range(2):
                    us = slice(u * 64, (u + 1) * 64)
                    for h in range(4):
                        c, base = h // 2, (h % 2) * 64
                        ob_ = 7 if h % 2 == 0 else 6
                        P.op("pe", (lambda h, c, base, us, ob_: lambda e: e.matmul(out=pbanks[ob_][us, c * 128:(c + 1) * 128], lhsT=qdT[base:base + 64, c, us],
                                                                                  rhs=Sb[base:base + 64, c, :], start=False, stop=True,
                                                                                  skip_group_check=True))(h, c, base, us, ob_), R=[b_qdT, b_Sb], W=[pbuf[ob_]])
                    for h in range(4):
                        c, base = h // 2, (h % 2) * 64
                        P.op("pe", (lambda h, c, base, us: lambda e: e.matmul(out=pbanks[5][base:base + 64, c * 128:(c + 1) * 128], lhsT=kend[us, h * 64:(h + 1) * 64],
                                                                             rhs=vt[us, h * 128:(h + 1) * 128], start=True, stop=True))(h, c, base, us),
                             R=[b_kend, b_vt], W=[pbuf[5]])
                    for c in range(2):
                        P.op("dve", (lambda c, u: lambda e: e.scalar_tensor_tensor(out=Sf[:, c, :], in0=Sf[:, c, :], scalar=EcT[:, c, u * 64 + 63:u * 64 + 64],
                                                                                   in1=pbanks[5][:, c * 128:(c + 1) * 128], op0=ALU.mult, op1=ALU.add))(c, u),
                             R=[b_Sf, b_EcT, pbuf[5]], W=[b_Sf])
                    P.op("act", lambda e: e.copy(out=Sb, in_=Sf), R=[b_Sf], W=[b_Sb])
                ck(8)
                for h in range(4):
                    ob_ = 7 if h % 2 == 0 else 6
                    P.op("dve", (lambda h, ob_: lambda e: e.bn_stats(out=gst[:, h, :], in_=pbanks[ob_][:, (h // 2) * 128:(h // 2 + 1) * 128]))(h, ob_), R=[pbuf[ob_]], W=[b_gst])
                for h in range(4):
                    P.op("dve", (lambda h: lambda e: e.bn_aggr(out=gmv[:, h, :], in_=gst[:, h, :]))(h), R=[b_gst], W=[b_gmv])
                P.op("act", lambda e: e.activation(out=gsd[:, :, 0], in_=gmv[:, :, 1], func=AF.Sqrt, bias=eps_t[:, 0:1], scale=1.0),
                     R=[b_gmv, b_eps], W=[b_gsd])
                P.op("dve", lambda e: e.reciprocal(out=gsd[:, :, 1], in_=gsd[:, :, 0]), R=[b_gsd], W=[b_gsd])
                for h in range(4):
                    ob_ = 7 if h % 2 == 0 else 6
                    P.op("dve", (lambda h, ob_: lambda e: e.tensor_scalar(out=on[:, h * 128:(h + 1) * 128], in0=pbanks[ob_][:, (h // 2) * 128:(h // 2 + 1) * 128],
                                                                         scalar1=gmv[:, h, 0:1], scalar2=gsd[:, h, 1:2], op0=ALU.subtract, op1=ALU.mult))(h, ob_),
                         R=[pbuf[ob_], b_gmv, b_gsd], W=[b_on])
                P.op("pool", lambda e: e.tensor_tensor(out=on, in0=on, in1=NG, op=ALU.mult), R=[b_on, b_NG], W=[b_on])
                P.op("pool", lambda e: e.tensor_tensor(out=oa[:, 512:1024], in0=on, in1=sbr, op=ALU.mult), R=[b_on, b_sbr], W=[b_oa])

                P.mute = False
                if stop == "gla":
                    return
                for kc in range(8):
                    P.op("pe", (lambda kc: lambda e: e.transpose(out=pbf(4)[:, kc * 128:(kc + 1) * 128], in_=oa[:, kc * 128:(kc + 1) * 128], identity=identb))(kc),
                         R=[b_oa, b_identb], W=[pbuf[4]])
                copy_rr(oT, pbf(4).rearrange("p (a b) -> p a b", b=128), [pbuf[4]], [b_oT])
                for hh in range(2):
                    for kc in range(8):
                        P.op("pe", (lambda hh, kc: lambda e: e.matmul(out=pbanks[2 + hh], lhsT=oT[:, kc, :], rhs=Wo[:, kc, hh * 512:(hh + 1) * 512],
                                                                    start=(kc == 0), stop=(kc == 7)))(hh, kc), R=[b_oT, b_Wo], W=[pbuf[2 + hh]])
                par = 0
                for hh in range(2):
                    P.op("dve", (lambda hh, par: lambda e: e.scalar_tensor_tensor(out=rr_[:, hh * 512:(hh + 1) * 512], in0=xs[:, par, hh * 512:(hh + 1) * 512],
                                                                               scalar=ALPHA, in1=pbanks[2 + hh], op0=ALU.mult, op1=ALU.add))(hh, par),
                         R=[b_xs[par], pbuf[2 + hh]], W=[b_rr])
                m_ln = A.mark()
                layer_norm_tile(rr_, b_rr, LG, LB, b_LG, rr_, b_rr, b_LB)
                A.release(m_ln)
                gi = s * NT + qt
                P.dma(hres[gi * 128:(gi + 1) * 128, :], rr_, R=[b_rr], W=[hres_b[gi]])

            pre(0)
            topk(0)
            for qt in range(nqt):
                if qt + 1 < nqt:
                    pre(qt + 1)
                    P.fork(2)
                    P.stream(0)
                    topk(qt + 1)
                    P.stream(1)
                    rest(qt)
                    n0, n1 = len(P.streams[0]), len(P.streams[1])
                    P.join([1, max(1, n1 // max(1, n0))])
                else:
                    rest(qt)

        A.release(m_l0)


        xs_b = Buf("xs_dram")
        ys_b = Buf("ys_dram")

        _oob_regs = {}

        def oobkw(e, mx):
            if os.environ.get("OOB", "1") != "1":
                return {}
            if mx not in _oob_regs:
                _oob_regs[mx] = e.to_reg(mx)
            return dict(bounds_check=_oob_regs[mx], oob_is_err=False)

        def moe_stage(layer, final):
            m_moe = A.mark()
            NG = NG_TOK
            LGf, b_LGf = A.alloc([128, 1024], F32, name="LGf")
            LBf, b_LBf = A.alloc([128, 1024], F32, name="LBf")
            P.dma(LGf, ln_ffn_g[layer].partition_broadcast(128), W=[b_LGf])
            P.dma(LBf, ln_ffn_b[layer].partition_broadcast(128), W=[b_LBf])
            Wr, b_Wr = A.alloc([128, 8, 20], F32, name="Wr")
            P.dma(Wr[:, :, 0:4], moe_r_coarse[layer].rearrange("(kc p) n -> p kc n", p=128), W=[b_Wr])
            for g in range(4):
                P.dma(Wr[:, :, 4 + 4 * g:8 + 4 * g], moe_r_fine[layer, g].rearrange("(kc p) n -> p kc n", p=128), W=[b_Wr])
            RBb, b_RBb = A.alloc([128, 20], F32, name="RBb")
            P.dma(RBb[:, 0:4], moe_rb_coarse[layer].partition_broadcast(128), W=[b_RBb])
            P.dma(RBb[:, 4:20], moe_rb_fine[layer].rearrange("a b -> (a b)").partition_broadcast(128), W=[b_RBb])
            OH, b_OH = A.alloc([128, NG, 32], BF16, name="OH")
            GW, b_GW = A.alloc([128, NG, 2], F32, name="GW")
            POSi, b_POSi = A.alloc([128, NG * 2], I32, name="POSi")
            IDXG, b_IDXG = A.alloc([128, NTILE * 2], I32, name="IDXG")
            IDXD, b_IDXD = A.alloc([128, NTILE * 4], I32, name="IDXD")
            m_r = A.mark()
            ht, b_ht = A.alloc([128, 2, 1024], F32, nbufs=2, name="ht")
            h1T, b_h1T = A.alloc([128, 8, 128], F32, name="h1T")
            LGT, b_LGT = A.alloc([128, NG, 20], F32, name="LGT")
            rmx, b_rmx = A.alloc([128, NG], F32, name="rmx")
            dg, b_dg = A.alloc([128, NG, 4], F32, name="dg")
            eg, b_eg = A.alloc([128, NG, 4], F32, name="eg")
            gwc, b_gwc = A.alloc([128, NG], F32, name="gwc")
            ohgB, b_ohgB = A.alloc([128, NG, 4], F32, name="ohgB")
            tmpB, b_tmpB = A.alloc([128, NG, 4, 4], F32, name="tmpB")
            flsB, b_flsB = A.alloc([128, NG, 4], F32, name="flsB")
            fl2B, b_fl2B = A.alloc([128, NG, 4], F32, name="fl2B")
            m1B, b_m1B = A.alloc([128, NG], F32, name="m1B")
            m2B, b_m2B = A.alloc([128, NG], F32, name="m2B")
            ohB, b_ohB = A.alloc([128, 2, NG, 4], F32, name="ohB")
            lgt, b_lgt = A.alloc([128, 20], F32, name="lgt")
            sm_, b_sm_ = A.alloc([128, 16], F32, name="rsm")
            ohg, b_ohg = A.alloc([128, 4], F32, name="ohg")
            tmp16, b_tmp16 = A.alloc([128, 4, 4], F32, name="tmp16")
            fls, b_fls = A.alloc([128, 4], F32, name="fls")
            fl2, b_fl2 = A.alloc([128, 4], F32, name="fl2")
            oh12, b_oh12 = A.alloc([128, 2, 4], F32, name="oh12")
            zt, b_zt = A.alloc([128, 4, 1024], BF16, name="zt")
            P.op("pool", lambda e: e.memset(zt, 0.0), W=[b_zt])
            for gi in range(NG):
                par = gi % 2
                if gi < NTILE:
                    P.dma(xs_dram[gi * 512:(gi + 1) * 512, :].rearrange("(p a) d -> p a d", a=4), zt, R=[b_zt], W=[xs_b])
                P.dma(ht[:, par, :], hres[gi * 128:(gi + 1) * 128, :], R=[hres_b[gi]], W=[b_ht[par]])
                for g in range(2):
                    bk_ = g
                    for c in range(4):
                        kc = 4 * g + c
                        P.op("pe", (lambda par, kc, c, bk_: lambda e: e.transpose(out=pbanks[bk_][:, c * 128:(c + 1) * 128],
                                                                                in_=ht[:, par, kc * 128:(kc + 1) * 128], identity=ident))(par, kc, c, bk_),
                             R=[b_ht[par], b_ident], W=[pbuf[bk_]])
                    copy_rr(h1T[:, 4 * g:4 * g + 4, :], pbanks[bk_].rearrange("p (a b) -> p a b", b=128), [pbuf[bk_]], [b_h1T])
                for kc in range(8):
                    P.op("pe", (lambda kc: lambda e: e.matmul(out=pbanks[2][:, 0:20], lhsT=h1T[:, kc, :], rhs=Wr[:, kc, :], start=(kc == 0), stop=(kc == 7)))(kc),
                         R=[b_h1T, b_Wr], W=[pbuf[2]])
                P.op("dve", (lambda gi: lambda e: e.tensor_tensor(out=LGT[:, gi, :], in0=pbanks[2][:, 0:20], in1=RBb, op=ALU.add))(gi), R=[pbuf[2], b_RBb], W=[b_LGT])
            B3 = lambda ap2, n: ap2.unsqueeze(2).to_broadcast([128, NG, n])
            gl = LGT[:, :, 0:4]
            P.op("dve", lambda e: e.tensor_reduce(out=rmx, in_=gl, axis=AX.X, op=ALU.max), R=[b_LGT], W=[b_rmx])
            P.op("dve", lambda e: e.tensor_tensor(out=dg, in0=gl, in1=B3(rmx, 4), op=ALU.subtract), R=[b_LGT, b_rmx], W=[b_dg])
            P.op("act", lambda e: e.activation(out=eg, in_=dg, func=AF.Exp), R=[b_dg], W=[b_eg])
            P.op("dve", lambda e: e.tensor_reduce(out=gwc, in_=eg, axis=AX.X, op=ALU.add), R=[b_eg], W=[b_gwc])
            P.op("dve", lambda e: e.reciprocal(out=gwc, in_=gwc), R=[b_gwc], W=[b_gwc])
            P.op("dve", lambda e: e.tensor_scalar(out=ohgB, in0=dg, scalar1=0.0, scalar2=None, op0=ALU.is_ge), R=[b_dg], W=[b_ohgB])
            P.op("dve", lambda e: e.tensor_tensor(out=tmpB, in0=LGT[:, :, 4:20].rearrange("p t (g e) -> p t g e", e=4),
                                                  in1=ohgB.unsqueeze(3).to_broadcast([128, NG, 4, 4]), op=ALU.mult), R=[b_LGT, b_ohgB], W=[b_tmpB])
            P.op("dve", lambda e: e.tensor_reduce(out=flsB, in_=tmpB.rearrange("p t g e -> p t e g"), axis=AX.X, op=ALU.add), R=[b_tmpB], W=[b_flsB])
            P.op("dve", lambda e: e.tensor_reduce(out=m1B, in_=flsB, axis=AX.X, op=ALU.max), R=[b_flsB], W=[b_m1B])
            P.op("dve", lambda e: e.tensor_tensor(out=ohB[:, 0], in0=flsB, in1=B3(m1B, 4), op=ALU.is_ge), R=[b_flsB, b_m1B], W=[b_ohB])
            P.op("dve", lambda e: e.scalar_tensor_tensor(out=fl2B, in0=ohB[:, 0], scalar=NEG, in1=flsB, op0=ALU.mult, op1=ALU.add), R=[b_ohB, b_flsB], W=[b_fl2B])
            P.op("dve", lambda e: e.tensor_reduce(out=m2B, in_=fl2B, axis=AX.X, op=ALU.max), R=[b_fl2B], W=[b_m2B])
            P.op("dve", lambda e: e.tensor_tensor(out=ohB[:, 1], in0=fl2B, in1=B3(m2B, 4), op=ALU.is_ge), R=[b_fl2B, b_m2B], W=[b_ohB])
            P.op("dve", lambda e: e.tensor_tensor(out=m2B, in0=m2B, in1=m1B, op=ALU.subtract), R=[b_m2B, b_m1B], W=[b_m2B])
            P.op("act", lambda e: e.activation(out=m2B, in_=m2B, func=AF.Exp), R=[b_m2B], W=[b_m2B])
            P.op("dve", lambda e: e.tensor_scalar(out=m1B, in0=m2B, scalar1=1.0, scalar2=None, op0=ALU.add), R=[b_m2B], W=[b_m1B])
            P.op("dve", lambda e: e.reciprocal(out=m1B, in_=m1B), R=[b_m1B], W=[b_m1B])
            P.op("dve", lambda e: e.tensor_tensor(out=GW[:, :, 0], in0=m1B, in1=gwc, op=ALU.mult), R=[b_m1B, b_gwc], W=[b_GW])
            P.op("dve", lambda e: e.tensor_tensor(out=m2B, in0=m2B, in1=m1B, op=ALU.mult), R=[b_m2B, b_m1B], W=[b_m2B])
            P.op("dve", lambda e: e.tensor_tensor(out=GW[:, :, 1], in0=m2B, in1=gwc, op=ALU.mult), R=[b_m2B, b_gwc], W=[b_GW])
            for k in range(2):
                P.op("dve", (lambda k: lambda e: e.tensor_tensor(out=OH[:, :, k * 16:(k + 1) * 16].rearrange("p t (g e) -> p t g e", e=4),
                                                                in0=ohgB.unsqueeze(3).to_broadcast([128, NG, 4, 4]),
                                                                in1=ohB[:, k].unsqueeze(2).to_broadcast([128, NG, 4, 4]), op=ALU.mult))(k),
                     R=[b_ohgB, b_ohB], W=[b_OH])
            STRI, b_STRI = A.alloc([128, 128], BF16, name="STRI")
            ONESM, b_ONESM = A.alloc([128, 128], BF16, name="ONESM")
            P.op("pool", lambda e: e.memset(ONESM, 1.0), W=[b_ONESM])
            P.op("pool", lambda e: e.memset(STRI, 1.0), W=[b_STRI])
            P.op("pool", lambda e: e.affine_select(out=STRI, in_=STRI, pattern=[[1, 128]], compare_op=ALU.is_ge, fill=0.0, base=-1,
                                                   channel_multiplier=-1), R=[b_STRI], W=[b_STRI])
            PC, b_PC = A.alloc([128, NG, 64], F32, name="PC")
            for gi in range(NG):
                bk_ = gi % 2
                P.op("pe", (lambda gi, bk_: lambda e: e.matmul(out=pbanks[bk_][:, 0:32], lhsT=STRI, rhs=OH[:, gi, :], start=True, stop=True))(gi, bk_),
                     R=[b_STRI, b_OH], W=[pbuf[bk_]])
                P.op("pe", (lambda gi, bk_: lambda e: e.matmul(out=pbanks[bk_][:, 32:64], lhsT=ONESM, rhs=OH[:, gi, :], start=True, stop=True))(gi, bk_),
                     R=[b_ONESM, b_OH], W=[pbuf[bk_]])
                copy_rr(PC[:, gi, :], pbanks[bk_][:, 0:64], [pbuf[bk_]], [b_PC])
            BASE, b_BASE = A.alloc([128, NG * 2, 16], F32, name="BASE")
            run, b_run = A.alloc([128, 16], F32, name="run")
            P.op("pool", lambda e: e.memset(run, 0.0), W=[b_run])
            for gi in range(NG):
                for k in range(2):
                    P.op("dve", (lambda gi, k: lambda e: e.tensor_copy(out=BASE[:, gi * 2 + k, :], in_=run))(gi, k), R=[b_run], W=[b_BASE])
                    P.op("dve", (lambda gi, k: lambda e: e.tensor_tensor(out=run, in0=run, in1=PC[:, gi, 32 + k * 16:48 + k * 16], op=ALU.add))(gi, k),
                         R=[b_run, b_PC], W=[b_run])
            npad, b_npad = A.alloc([128, 16], F32, name="npad")
            cum, b_cum = A.alloc([128, 16], F32, name="cum")
            ones16, b_ones16 = A.alloc([128, 16], F32, name="ones16")
            offs, b_offs = A.alloc([128, 16], F32, name="offs")
            P.op("pool", lambda e: e.memset(ones16, 1.0), W=[b_ones16])
            P.op("dve", lambda e: e.tensor_scalar(out=npad, in0=run, scalar1=0.0, scalar2=512.0, op0=ALU.is_gt, op1=ALU.mult), R=[b_run], W=[b_npad])
            for m_ in range(1, 8):
                P.op("dve", (lambda m_: lambda e: e.tensor_scalar(out=cum, in0=run, scalar1=512.0 * m_, scalar2=512.0, op0=ALU.is_gt, op1=ALU.mult))(m_),
                     R=[b_run], W=[b_cum])
                P.op("dve", lambda e: e.tensor_tensor(out=npad, in0=npad, in1=cum, op=ALU.add), R=[b_npad, b_cum], W=[b_npad])
            P.op("dve", lambda e: e.tensor_tensor_scan(out=cum, data0=ones16, data1=npad, initial=0.0, op0=ALU.mult, op1=ALU.add),
                 R=[b_ones16, b_npad], W=[b_cum])
            P.op("dve", lambda e: e.tensor_tensor(out=offs, in0=cum, in1=npad, op=ALU.subtract), R=[b_cum, b_npad], W=[b_offs])
            P.op("dve", lambda e: e.tensor_tensor(out=BASE, in0=BASE, in1=offs.unsqueeze(1).to_broadcast([128, NG * 2, 16]), op=ALU.add),
                 R=[b_BASE, b_offs], W=[b_BASE])
            T1, b_T1 = A.alloc([128, NG, 32], F32, name="T1")
            P.op("dve", lambda e: e.tensor_tensor(out=T1, in0=PC[:, :, 0:32], in1=BASE.rearrange("p (a k) e -> p a (k e)", k=2), op=ALU.add),
                 R=[b_PC, b_BASE], W=[b_T1])
            P.op("dve", lambda e: e.tensor_tensor(out=T1, in0=T1, in1=OH, op=ALU.mult), R=[b_T1, b_OH], W=[b_T1])
            POSf, b_POSf = A.alloc([128, NG * 2], F32, name="POSf")
            P.op("dve", lambda e: e.tensor_reduce(out=POSf, in_=T1.rearrange("p a (k e) -> p (a k) e", k=2), axis=AX.X, op=ALU.add), R=[b_T1], W=[b_POSf])
            P.op("dve", lambda e: e.tensor_copy(out=POSi, in_=POSf), R=[b_POSf], W=[b_POSi])
            EJ, b_EJ = A.alloc([128, NTILE], F32, name="EJ")
            for j in range(NTILE):
                P.op("dve", (lambda j: lambda e: e.tensor_scalar(out=ones16, in0=cum, scalar1=float(j * 512) + 0.5, scalar2=None, op0=ALU.is_le,
                                                                 op1=ALU.add, accum_out=EJ[:, j:j + 1]))(j), R=[b_cum], W=[b_ones16, b_EJ])
            EJu, b_EJu = A.alloc([128, NTILE], F32, name="EJu")
            P.op("dve", lambda e: e.tensor_scalar(out=EJu, in0=EJ, scalar1=15.5, scalar2=1.0e7, op0=ALU.is_ge, op1=ALU.mult), R=[b_EJ], W=[b_EJu])
            P.op("dve", lambda e: e.tensor_scalar(out=EJ, in0=EJ, scalar1=15.0, scalar2=float(layer * 16), op0=ALU.min, op1=ALU.add), R=[b_EJ], W=[b_EJ])
            P2, b_P2 = A.alloc([128, 2], F32, name="P2")
            P4, b_P4 = A.alloc([128, 4], F32, name="P4")
            P.op("pool", lambda e: e.iota(out=P2, pattern=[[1, 2]], base=0, channel_multiplier=2, allow_small_or_imprecise_dtypes=True), W=[b_P2])
            P.op("pool", lambda e: e.iota(out=P4, pattern=[[128, 4]], base=0, channel_multiplier=1, allow_small_or_imprecise_dtypes=True), W=[b_P4])
            IGf, b_IGf = A.alloc([128, NTILE, 2], F32, name="IGf")
            IDf, b_IDf = A.alloc([128, NTILE, 4], F32, name="IDf")
            P.op("dve", lambda e: e.tensor_scalar(out=IGf, in0=EJ.unsqueeze(2).to_broadcast([128, NTILE, 2]), scalar1=256.0, scalar2=None, op0=ALU.mult),
                 R=[b_EJ], W=[b_IGf])
            P.op("dve", lambda e: e.tensor_tensor(out=IGf, in0=IGf, in1=P2.unsqueeze(1).to_broadcast([128, NTILE, 2]), op=ALU.add), R=[b_IGf, b_P2], W=[b_IGf])
            P.op("dve", lambda e: e.tensor_scalar(out=IDf, in0=EJ.unsqueeze(2).to_broadcast([128, NTILE, 4]), scalar1=512.0, scalar2=None, op0=ALU.mult),
                 R=[b_EJ], W=[b_IDf])
            P.op("dve", lambda e: e.tensor_tensor(out=IDf, in0=IDf, in1=P4.unsqueeze(1).to_broadcast([128, NTILE, 4]), op=ALU.add), R=[b_IDf, b_P4], W=[b_IDf])
            if os.environ.get("OOB", "1") == "1":
                P.op("dve", lambda e: e.tensor_tensor(out=IGf, in0=IGf, in1=EJu.unsqueeze(2).to_broadcast([128, NTILE, 2]), op=ALU.add), R=[b_IGf, b_EJu], W=[b_IGf])
                P.op("dve", lambda e: e.tensor_tensor(out=IDf, in0=IDf, in1=EJu.unsqueeze(2).to_broadcast([128, NTILE, 4]), op=ALU.add), R=[b_IDf, b_EJu], W=[b_IDf])
            P.op("dve", lambda e: e.tensor_copy(out=IDXG, in_=IGf.rearrange("p a b -> p (a b)")), R=[b_IGf], W=[b_IDXG])
            P.op("dve", lambda e: e.tensor_copy(out=IDXD, in_=IDf.rearrange("p a b -> p (a b)")), R=[b_IDf], W=[b_IDXD])
            if debug == "moe_route":
                P.dma(y[0, 0:128, 0:64], POSf, R=[b_POSf], W=[y_b])
                P.dma(y[0, 0:128, 64:128], GW.rearrange("p a b -> p (a b)"), R=[b_GW], W=[y_b])
                P.dma(y[0, 0:128, 128:128 + NTILE], EJ, R=[b_EJ], W=[y_b])
                P.dma(y[0, 0:128, 256:272], cum, R=[b_cum], W=[y_b])
            hb, b_hb = A.alloc([128, 2, 1024], BF16, nbufs=2, name="hb")
            for gi in range(NG):
                par = gi % 2
                P.dma(ht[:, par, :], hres[gi * 128:(gi + 1) * 128, :], R=[hres_b[gi]], W=[b_ht[par]])
                P.op("act", (lambda par: lambda e: e.copy(out=hb[:, par, :], in_=ht[:, par, :]))(par), R=[b_ht[par]], W=[b_hb[par]])
                for k in range(2):
                    P.op("pool", (lambda gi, k, par: lambda e: e.indirect_dma_start(
                        out=xs_dram[:, :], out_offset=bass.IndirectOffsetOnAxis(ap=POSi[:, gi * 2 + k:gi * 2 + k + 1], axis=0),
                        in_=hb[:, par, :], in_offset=None))(gi, k, par), R=[b_hb[par], b_POSi], W=[xs_b], dma=True)
            A.release(m_r)
            wg, b_wg = A.alloc([128, 2, 8 * 512], BF16, nbufs=2, name="wg")
            wu, b_wu = A.alloc([128, 2, 8 * 512], BF16, nbufs=2, name="wu")
            wd, b_wd = A.alloc([128, 2, 4 * 1024], BF16, nbufs=2, name="wd")
            xsb, b_xsb = A.alloc([128, 2, 4 * 1024], BF16, nbufs=2, name="xsb")
            xT2, b_xT2 = A.alloc([128, 2, 8, 512], BF16, nbufs=2, name="xT")
            hid, b_hid = A.alloc([128, 4, 512], BF16, name="hid")
            sg, b_sg = A.alloc([128, 2, 512], F32, nbufs=2, name="sg")
            ysb, b_ysb = A.alloc([128, 4, 1024], F32, name="ysb")
            PC_ = os.environ.get("PRECAST", "1") == "1"
            WGv = (wgb if PC_ else moe_w_gate).rearrange("l g e (p two r) n -> (l g e p two) (r n)", p=128, two=2)
            WUv = (wub if PC_ else moe_w_up).rearrange("l g e (p two r) n -> (l g e p two) (r n)", p=128, two=2)
            WDv = (wdb if PC_ else moe_w_down).rearrange("l g e f n -> (l g e f) n")
            wc_dep = list(wcast_b[layer]) if PC_ else []
            def mxload(j):
                par = j % 2
                P.dma(xsb[:, par, :].rearrange("p (a d) -> p a d", a=4), xs_dram[j * 512:(j + 1) * 512, :].rearrange("(a p) d -> p a d", p=128),
                      R=[xs_b], W=[b_xsb[par]])

            def mfront(j):
                par = j % 2
                xT, b_xT = xT2[:, par], b_xT2[par]
                for hf in range(2):
                    P.op("pool", (lambda j, hf, par: lambda e: e.indirect_dma_start(
                        out=wg[:, par, hf * 2048:(hf + 1) * 2048], out_offset=None, in_=WGv,
                        in_offset=bass.IndirectOffsetOnAxis(ap=IDXG[:, j * 2 + hf:j * 2 + hf + 1], axis=0), **oobkw(e, 2 * 16 * 256 - 1)))(j, hf, par),
                        R=[b_IDXG] + wc_dep, W=[b_wg[par]], dma=True)
                    P.op("pool", (lambda j, hf, par: lambda e: e.indirect_dma_start(
                        out=wu[:, par, hf * 2048:(hf + 1) * 2048], out_offset=None, in_=WUv,
                        in_offset=bass.IndirectOffsetOnAxis(ap=IDXG[:, j * 2 + hf:j * 2 + hf + 1], axis=0), **oobkw(e, 2 * 16 * 256 - 1)))(j, hf, par),
                        R=[b_IDXG] + wc_dep, W=[b_wu[par]], dma=True)
                for fc in range(4):
                    P.op("pool", (lambda j, fc, par: lambda e: e.indirect_dma_start(
                        out=wd[:, par, fc * 1024:(fc + 1) * 1024], out_offset=None, in_=WDv,
                        in_offset=bass.IndirectOffsetOnAxis(ap=IDXD[:, j * 4 + fc:j * 4 + fc + 1], axis=0), **oobkw(e, 2 * 16 * 512 - 1)))(j, fc, par),
                        R=[b_IDXD] + wc_dep, W=[b_wd[par]], dma=True)
                for st_ in range(4):
                    bk_ = st_ % 2
                    src = xsb[:, par, st_ * 1024:(st_ + 1) * 1024].rearrange("s (p k) -> s k p", k=8)
                    for kc in range(8):
                        P.op("pe", (lambda src, kc, bk_: lambda e: e.transpose(out=pbf(bk_)[:, kc * 128:(kc + 1) * 128], in_=src[:, kc, :], identity=identb))(src, kc, bk_),
                             R=[b_xsb[par], b_identb], W=[pbuf[bk_]])
                    copy_rr(xT[:, :, st_ * 128:(st_ + 1) * 128], pbf(bk_).rearrange("p (a b) -> p a b", b=128), [pbuf[bk_]], [b_xT])
            def mback(j):
                par = j % 2
                xT, b_xT = xT2[:, par], b_xT2[par]
                wgv = wg[:, par, :].rearrange("p (k n) -> p k n", n=512)
                wuv = wu[:, par, :].rearrange("p (k n) -> p k n", n=512)
                wdv = wd[:, par, :].rearrange("p (k n) -> p k n", n=1024)
                for fc in range(4):
                    bg_, bu_ = 2 + (fc % 2) * 2, 3 + (fc % 2) * 2
                    for kc in range(8):
                        P.op("pe", (lambda fc, kc, bg_, wgv: lambda e: e.matmul(out=pbanks[bg_], lhsT=wgv[:, kc, fc * 128:(fc + 1) * 128], rhs=xT[:, kc, :],
                                                                              start=(kc == 0), stop=(kc == 7)))(fc, kc, bg_, wgv), R=[b_wg[par], b_xT], W=[pbuf[bg_]])
                    for kc in range(8):
                        P.op("pe", (lambda fc, kc, bu_, wuv: lambda e: e.matmul(out=pbanks[bu_], lhsT=wuv[:, kc, fc * 128:(fc + 1) * 128], rhs=xT[:, kc, :],
                                                                              start=(kc == 0), stop=(kc == 7)))(fc, kc, bu_, wuv), R=[b_wu[par], b_xT], W=[pbuf[bu_]])
                    sp_ = fc % 2
                    P.op("act", (lambda bg_, sp_: lambda e: e.activation(out=sg[:, sp_, :], in_=pbanks[bg_], func=AF.Silu))(bg_, sp_), R=[pbuf[bg_]], W=[b_sg[sp_]])
                    P.op("dve", (lambda fc, bu_, sp_: lambda e: e.tensor_tensor(out=hid[:, fc, :], in0=pbanks[bu_], in1=sg[:, sp_, :], op=ALU.mult))(fc, bu_, sp_),
                         R=[pbuf[bu_], b_sg[sp_]], W=[b_hid])
                for st_ in range(4):
                    for hh in range(2):
                        bk_ = 6 + hh
                        for fc in range(4):
                            P.op("pe", (lambda st_, hh, fc, bk_, wdv: lambda e: e.matmul(out=pbanks[bk_], lhsT=hid[:, fc, st_ * 128:(st_ + 1) * 128],
                                                                                        rhs=wdv[:, fc, hh * 512:(hh + 1) * 512], start=(fc == 0), stop=(fc == 3)))(st_, hh, fc, bk_, wdv),
                                 R=[b_hid, b_wd[par]], W=[pbuf[bk_]])
                        copy_rr(ysb[:, st_, hh * 512:(hh + 1) * 512], pbanks[bk_], [pbuf[bk_]], [b_ysb])
                P.dma(ys_dram[j * 512:(j + 1) * 512, :].rearrange("(a p) d -> p a d", p=128), ysb, R=[b_ysb], W=[ys_b])

            mxload(0)
            mxload(1)
            mfront(0)
            for j in range(NTILE):
                if j + 2 < NTILE:
                    mxload(j + 2)
                if j + 1 < NTILE:
                    P.fork(2)
                    P.stream(0)
                    mfront(j + 1)
                    P.stream(1)
                    mback(j)
                    n0, n1 = len(P.streams[0]), len(P.streams[1])
                    P.join([max(1, n0 // n1), max(1, n1 // n0)])
                else:
                    mback(j)
            A.release(m_r)
            ht5, b_ht5 = A.alloc([128, 2, 1024], F32, nbufs=2, name="ht2")
            Y1, b_Y1 = A.alloc([128, 2, 1024], F32, nbufs=2, name="Y1")
            Y2, b_Y2 = A.alloc([128, 2, 1024], F32, nbufs=2, name="Y2")
            rr2_, b_rr2_ = A.alloc([128, 2, 1024], F32, nbufs=2, name="rr2")
            lnt = []
            for _ in range(2):
                a_, ba_ = A.alloc([128, 2, 6], F32, name="lnst")
                c_, bc_ = A.alloc([128, 2], F32, name="lnmv")
                d_, bd_ = A.alloc([128, 2], F32, name="lnsd")
                lnt.append((a_, ba_, c_, bc_, d_, bd_))
            y_bs = [Buf("y%d" % i) for i in range(NG)]
            h2, b_h2 = A.alloc([128, 2, 1024], F32, nbufs=2, name="h2")
            def r5(gi):
                par = gi % 2
                rr2, b_rr2 = rr2_[:, par, :], b_rr2_[par]
                P.dma(ht5[:, par, :], hres[gi * 128:(gi + 1) * 128, :], R=[hres_b[gi]], W=[b_ht5[par]])
                for k, (Y_, bY) in enumerate(((Y1, b_Y1), (Y2, b_Y2))):
                    P.op("pool", (lambda gi, k, par, Y_: lambda e: e.indirect_dma_start(
                        out=Y_[:, par, :], out_offset=None, in_=ys_dram[:, :],
                        in_offset=bass.IndirectOffsetOnAxis(ap=POSi[:, gi * 2 + k:gi * 2 + k + 1], axis=0)))(gi, k, par, Y_),
                        R=[ys_b, b_POSi], W=[bY[par]], dma=True)
                P.op("dve", (lambda gi, par: lambda e: e.tensor_scalar(out=rr2, in0=Y1[:, par, :], scalar1=GW[:, gi, 0:1], scalar2=None, op0=ALU.mult))(gi, par),
                     R=[b_Y1[par], b_GW], W=[b_rr2])
                P.op("dve", (lambda gi, par: lambda e: e.scalar_tensor_tensor(out=rr2, in0=Y2[:, par, :], scalar=GW[:, gi, 1:2], in1=rr2, op0=ALU.mult, op1=ALU.add))(gi, par),
                     R=[b_Y2[par], b_GW, b_rr2], W=[b_rr2])
                P.op("dve", (lambda par: lambda e: e.scalar_tensor_tensor(out=rr2, in0=ht5[:, par, :], scalar=ALPHA, in1=rr2, op0=ALU.mult, op1=ALU.add))(par),
                     R=[b_ht5[par], b_rr2], W=[b_rr2])
                layer_norm_tile(rr2, b_rr2, LGf, LBf, b_LGf, h2[:, par, :], b_h2[par], b_LBf, tmps=lnt[par])
                if final:
                    s_, qt_ = gi // NT, gi % NT
                    P.dma(y[s_, qt_ * 128:(qt_ + 1) * 128, :], h2[:, par, :], R=[b_h2[par]], W=[y_bs[gi]])
                else:
                    P.dma(hres[gi * 128:(gi + 1) * 128, :], h2[:, par, :], R=[b_h2[par]], W=[hres_b[gi]])
            P.fork(2)
            for gi in range(NG):
                P.stream(gi % 2)
                r5(gi)
            P.join([1, 1])
            A.release(m_moe)


        def s5_stage():
            m_s5 = A.mark()
            gT_b = [Buf("gTd%d" % i) for i in range(SEQ_PER_CORE)]
            Win, b_Win = A.alloc([128, 8, 1024], BF16, name="Win")
            wdma(Win, b_Win, s5_w_in[0])
            DV, b_DV = A.alloc([128, 1024], F32, name="DV")
            P.dma(DV, s5_d[0].partition_broadcast(128), W=[b_DV])
            TA, b_TA = A.alloc([128, 4096], F32, name="TA")
            TB, b_TB = A.alloc([128, 4096], F32, name="TB")
            Pre, b_Pre = A.alloc([128, 32, 128], F32, name="Pre")
            Pim, b_Pim = A.alloc([128, 32, 128], F32, name="Pim")
            BDr, b_BDr = A.alloc([128, 8, 512], BF16, name="BDr")
            BDi, b_BDi = A.alloc([128, 8, 512], BF16, name="BDi")
            CmR, b_CmR = A.alloc([128, 1024], BF16, name="CmR")
            CmI, b_CmI = A.alloc([128, 1024], BF16, name="CmI")
            TRIc, b_TRIc = A.alloc([128, 128], BF16, name="TRIc")
            a1, b_a1 = A.alloc([128, 2, 32], F32, name="a1")
            P.op("pool", lambda e: e.memset(TRIc, 1.0), W=[b_TRIc])
            P.op("pool", lambda e: e.affine_select(out=TRIc, in_=TRIc, pattern=[[1, 128]], compare_op=ALU.is_ge, fill=0.0, base=0,
                                                   channel_multiplier=-1), R=[b_TRIc], W=[b_TRIc])
            m_p = A.mark()
            P.fork(2)
            P.stream(0)
            lr, b_lr = A.alloc([128, 32], F32, name="lr")
            li, b_li = A.alloc([128, 32], F32, name="li")
            ldt, b_ldt = A.alloc([128, 32], F32, name="ldt")
            for two in range(2):
                ps_ = slice(two * 64, (two + 1) * 64)
                P.dma(lr[ps_, :], s5_lam_re[0].rearrange("(q two) p -> two p q", two=2)[two], W=[b_lr], allow_slow_non_contiguous=True)
                P.dma(li[ps_, :], s5_lam_im[0].rearrange("(q two) p -> two p q", two=2)[two], W=[b_li], allow_slow_non_contiguous=True)
                P.dma(ldt[ps_, :], s5_log_dt[0].rearrange("(q two) -> two q", two=2)[two].partition_broadcast(64), W=[b_ldt],
                      allow_slow_non_contiguous=True)
            sc_, b_sc_ = A.alloc([128, 16, 32], F32, name="s5sc")
            V = lambda k: sc_[:, k, :]
            def dv(fn, R=(), W=()):
                P.op("dve", fn, R=[b_sc_, b_lr, b_li, b_ldt] + list(R), W=[b_sc_] + list(W))
            def ac(fn, R=(), W=()):
                P.op("act", fn, R=[b_sc_, b_lr, b_li, b_ldt] + list(R), W=[b_sc_] + list(W))
            dv(lambda e: e.tensor_scalar(out=lr, in0=lr, scalar1=-1e-4, scalar2=None, op0=ALU.min), W=[b_lr])
            ac(lambda e: e.activation(out=V(0), in_=ldt, func=AF.Exp))
            dv(lambda e: e.tensor_tensor(out=V(1), in0=lr, in1=V(0), op=ALU.mult))
            dv(lambda e: e.tensor_tensor(out=V(2), in0=li, in1=V(0), op=ALU.mult))
            dv(lambda e: e.tensor_scalar(out=V(3), in0=V(2), scalar1=1.0 / 64, scalar2=1.5, op0=ALU.mult, op1=ALU.min))
            dv(lambda e: e.tensor_scalar(out=V(3), in0=V(3), scalar1=-1.5, scalar2=None, op0=ALU.max))
            ac(lambda e: e.activation(out=V(4), in_=V(3), func=AF.Sin))
            ac(lambda e: e.activation(out=V(5), in_=V(3), func=AF.Sin, bias=halfpi[:, 0:1], scale=1.0), R=[b_hp])
            ac(lambda e: e.activation(out=V(6), in_=V(1), func=AF.Exp, scale=1.0 / 64))
            dv(lambda e: e.tensor_tensor(out=V(7), in0=V(5), in1=V(6), op=ALU.mult))
            dv(lambda e: e.tensor_tensor(out=V(8), in0=V(4), in1=V(6), op=ALU.mult))
            for _ in range(6):
                dv(lambda e: e.tensor_tensor(out=V(9), in0=V(7), in1=V(7), op=ALU.mult))
                dv(lambda e: e.tensor_tensor(out=V(10), in0=V(8), in1=V(8), op=ALU.mult))
                dv(lambda e: e.tensor_tensor(out=V(11), in0=V(7), in1=V(8), op=ALU.mult))
                dv(lambda e: e.tensor_tensor(out=V(7), in0=V(9), in1=V(10), op=ALU.subtract))
                dv(lambda e: e.tensor_scalar(out=V(8), in0=V(11), scalar1=2.0, scalar2=None, op0=ALU.mult))
            dv(lambda e: e.tensor_copy(out=a1[:, 0, :], in_=V(7)), W=[b_a1])
            dv(lambda e: e.tensor_copy(out=a1[:, 1, :], in_=V(8)), W=[b_a1])
            ac(lambda e: e.activation(out=V(9), in_=V(1), func=AF.Exp, scale=-2.0))
            dv(lambda e: e.tensor_tensor(out=V(10), in0=V(7), in1=V(9), op=ALU.mult))
            dv(lambda e: e.scalar_tensor_tensor(out=V(11), in0=V(8), scalar=-1.0, in1=V(9), op0=ALU.mult, op1=ALU.mult))
            dv(lambda e: e.tensor_tensor(out=V(12), in0=lr, in1=lr, op=ALU.mult))
            dv(lambda e: e.tensor_tensor(out=V(13), in0=li, in1=li, op=ALU.mult))
            dv(lambda e: e.tensor_tensor(out=V(12), in0=V(12), in1=V(13), op=ALU.add))
            dv(lambda e: e.reciprocal(out=V(12), in_=V(12)))
            dv(lambda e: e.tensor_scalar(out=V(13), in0=V(7), scalar1=-1.0, scalar2=None, op0=ALU.add))
            dv(lambda e: e.tensor_tensor(out=V(14), in0=V(13), in1=lr, op=ALU.mult))
            dv(lambda e: e.tensor_tensor(out=V(15), in0=V(8), in1=li, op=ALU.mult))
            dv(lambda e: e.tensor_tensor(out=V(14), in0=V(14), in1=V(15), op=ALU.add))
            dv(lambda e: e.tensor_tensor(out=V(14), in0=V(14), in1=V(12), op=ALU.mult))
            dv(lambda e: e.tensor_tensor(out=V(15), in0=V(8), in1=lr, op=ALU.mult))
            dv(lambda e: e.tensor_tensor(out=V(13), in0=V(13), in1=li, op=ALU.mult))
            dv(lambda e: e.tensor_tensor(out=V(15), in0=V(15), in1=V(13), op=ALU.subtract))
            dv(lambda e: e.tensor_tensor(out=V(15), in0=V(15), in1=V(12), op=ALU.mult))
            Qre, b_Qre = A.alloc([128, 32, 128], F32, name="Qre")
            Qim, b_Qim = A.alloc([128, 32, 128], F32, name="Qim")
            tq1, b_tq1 = A.alloc([128, 32, 64], F32, name="tq1")
            tq2, b_tq2 = A.alloc([128, 32, 64], F32, name="tq2")
            pw, b_pw = A.alloc([128, 2, 32], F32, name="pw")
            sq, b_sq = A.alloc([128, 3, 32], F32, name="sq")
            for (Tr, bTr, Ti, bTi, i0r, i0i, br_, bi_) in ((Pre, b_Pre, Pim, b_Pim, None, None, 7, 8), (Qre, b_Qre, Qim, b_Qim, 14, 15, 10, 11)):
                if i0r is None:
                    P.op("pool", (lambda Tr: lambda e: e.memset(Tr[:, :, 0:1], 1.0))(Tr), W=[bTr])
                    P.op("pool", (lambda Ti: lambda e: e.memset(Ti[:, :, 0:1], 0.0))(Ti), W=[bTi])
                else:
                    P.op("dve", (lambda Tr, i0r: lambda e: e.tensor_copy(out=Tr[:, :, 0], in_=V(i0r)))(Tr, i0r), R=[b_sc_], W=[bTr])
                    P.op("dve", (lambda Ti, i0i: lambda e: e.tensor_copy(out=Ti[:, :, 0], in_=V(i0i)))(Ti, i0i), R=[b_sc_], W=[bTi])
                P.op("dve", (lambda br_: lambda e: e.tensor_copy(out=pw[:, 0, :], in_=V(br_)))(br_), R=[b_sc_], W=[b_pw])
                P.op("dve", (lambda bi_: lambda e: e.tensor_copy(out=pw[:, 1, :], in_=V(bi_)))(bi_), R=[b_sc_], W=[b_pw])
                n_ = 1
                while n_ < 128:
                    pr = pw[:, 0, :].unsqueeze(2).to_broadcast([128, 32, n_])
                    pi_ = pw[:, 1, :].unsqueeze(2).to_broadcast([128, 32, n_])
                    Rb = [bTr, bTi, b_pw, b_tq1, b_tq2, b_sq]
                    P.op("dve", (lambda Tr, pr, n_: lambda e: e.tensor_tensor(out=tq1[:, :, 0:n_], in0=Tr[:, :, 0:n_], in1=pr, op=ALU.mult))(Tr, pr, n_), R=Rb, W=[b_tq1])
                    P.op("dve", (lambda Ti, pi_, n_: lambda e: e.tensor_tensor(out=tq2[:, :, 0:n_], in0=Ti[:, :, 0:n_], in1=pi_, op=ALU.mult))(Ti, pi_, n_), R=Rb, W=[b_tq2])
                    P.op("dve", (lambda Tr, n_: lambda e: e.tensor_tensor(out=Tr[:, :, n_:2 * n_], in0=tq1[:, :, 0:n_], in1=tq2[:, :, 0:n_], op=ALU.subtract))(Tr, n_), R=Rb, W=[bTr])
                    P.op("dve", (lambda Tr, pi_, n_: lambda e: e.tensor_tensor(out=tq1[:, :, 0:n_], in0=Tr[:, :, 0:n_], in1=pi_, op=ALU.mult))(Tr, pi_, n_), R=Rb, W=[b_tq1])
                    P.op("dve", (lambda Ti, pr, n_: lambda e: e.tensor_tensor(out=tq2[:, :, 0:n_], in0=Ti[:, :, 0:n_], in1=pr, op=ALU.mult))(Ti, pr, n_), R=Rb, W=[b_tq2])
                    P.op("dve", (lambda Ti, n_: lambda e: e.tensor_tensor(out=Ti[:, :, n_:2 * n_], in0=tq1[:, :, 0:n_], in1=tq2[:, :, 0:n_], op=ALU.add))(Ti, n_), R=Rb, W=[bTi])
                    P.op("dve", lambda e: e.tensor_tensor(out=sq[:, 0, :], in0=pw[:, 0, :], in1=pw[:, 0, :], op=ALU.mult), R=Rb, W=[b_sq])
                    P.op("dve", lambda e: e.tensor_tensor(out=sq[:, 1, :], in0=pw[:, 1, :], in1=pw[:, 1, :], op=ALU.mult), R=Rb, W=[b_sq])
                    P.op("dve", lambda e: e.tensor_tensor(out=sq[:, 2, :], in0=pw[:, 0, :], in1=pw[:, 1, :], op=ALU.mult), R=Rb, W=[b_sq])
                    P.op("dve", lambda e: e.tensor_tensor(out=pw[:, 0, :], in0=sq[:, 0, :],
                                                          in1=sq[:, 1, :], op=ALU.subtract), R=Rb, W=[b_pw])
                    P.op("dve", lambda e: e.tensor_scalar(out=pw[:, 1, :], in0=sq[:, 2, :], scalar1=2.0, scalar2=None, op0=ALU.mult),
                         R=Rb, W=[b_pw])
                    n_ *= 2
            for (Q_, bQ, T_, bT) in ((Qre, b_Qre, TA, b_TA), (Qim, b_Qim, TB, b_TB)):
                for q4 in range(8):
                    bk_ = q4 % 2
                    for c in range(4):
                        q = q4 * 4 + c
                        P.op("pe", (lambda Q_, q, c, bk_: lambda e: e.transpose(out=pbanks[bk_][:, c * 128:(c + 1) * 128], in_=Q_[:, q, :], identity=ident))(Q_, q, c, bk_),
                             R=[bQ, b_ident], W=[pbuf[bk_]])
                    copy_rr(T_[:, q4 * 512:(q4 + 1) * 512], pbanks[bk_], [pbuf[bk_]], [bT])
            P.stream(1)
            m_bc = A.mark()
            Xb, b_Xb = A.alloc([128, 4, 128], F32, nbufs=4, name="Xb")
            Xbb, b_Xbb = A.alloc([128, 4, 128], BF16, nbufs=4, name="Xbb")
            cnt_ = 0
            for (src, BD_, bBD) in ((s5_b_re, BDr, b_BDr), (s5_b_im, BDi, b_BDi)):
                for kc in range(8):
                    for pair in range(4):
                        sl = cnt_ % 4
                        cnt_ += 1
                        P.op("pool", (lambda sl: lambda e: e.memset(Xb[:, sl, :], 0.0))(sl), W=[b_Xb[sl]])
                        for two in range(2):
                            g = 8 * kc + 2 * pair + two
                            c0 = (2 * pair + two) * 16
                            P.dma(Xb[two * 64:(two + 1) * 64, sl, c0:c0 + 16], src[0, g], W=[b_Xb[sl]])
                        P.op("act", (lambda sl: lambda e: e.copy(out=Xbb[:, sl, :], in_=Xb[:, sl, :]))(sl), R=[b_Xb[sl]], W=[b_Xbb[sl]])
                        bk_ = 2 + cnt_ % 2
                        P.op("pe", (lambda sl, bk_: lambda e: e.transpose(out=pbf(bk_)[:, 0:128], in_=Xbb[:, sl, :], identity=identb))(sl, bk_),
                             R=[b_Xbb[sl], b_identb], W=[pbuf[bk_]])
                        copy_rr(BD_[:, kc, pair * 128:(pair + 1) * 128], pbf(bk_)[:, 0:128], [pbuf[bk_]], [bBD])
            for (src, Cm_, bCm, sgn) in ((s5_c_re, CmR, b_CmR, 1.0), (s5_c_im, CmI, b_CmI, -1.0)):
                for kc in range(8):
                    sl = cnt_ % 4
                    cnt_ += 1
                    P.op("pool", (lambda sl: lambda e: e.memset(Xb[:, sl, :], 0.0))(sl), W=[b_Xb[sl]])
                    for gl in range(8):
                        g = 8 * kc + gl
                        two = g % 2
                        P.dma(Xb[gl * 16:(gl + 1) * 16, sl, two * 64:(two + 1) * 64], src[0, g], W=[b_Xb[sl]])
                    P.op("act", (lambda sl: lambda e: e.copy(out=Xbb[:, sl, :], in_=Xb[:, sl, :]))(sl), R=[b_Xb[sl]], W=[b_Xbb[sl]])
                    bk_ = 2 + cnt_ % 2
                    P.op("pe", (lambda sl, bk_: lambda e: e.transpose(out=pbf(bk_)[:, 0:128], in_=Xbb[:, sl, :], identity=identb))(sl, bk_),
                         R=[b_Xbb[sl], b_identb], W=[pbuf[bk_]])
                    P.op("act", (lambda Cm_, kc, bk_, sgn: lambda e: e.activation(out=Cm_[:, kc * 128:(kc + 1) * 128], in_=pbf(bk_)[:, 0:128], func=AF.Copy, scale=sgn))(Cm_, kc, bk_, sgn),
                         R=[pbuf[bk_]], W=[bCm])
            n0, n1 = len(P.streams[0]), len(P.streams[1])
            P.join([max(1, n0 // n1), max(1, n1 // n0)])
            A.release(m_p)
            m_scan = A.mark()
            ht, b_ht = A.alloc([128, 2, 1024], F32, nbufs=2, name="s5ht")
            hTt, b_hTt = A.alloc([128, 8, 128], BF16, name="hTt")
            uTt, b_uTt = A.alloc([128, 8, 128], BF16, name="uTt")
            ud2, b_ud2 = A.alloc([128, 2, 1024], F32, nbufs=2, name="ud")
            wre2, b_wre2 = A.alloc([128, 2, 4096], BF16, nbufs=2, name="wre")
            wim2, b_wim2 = A.alloc([128, 2, 4096], BF16, nbufs=2, name="wim")
            sre, b_sre = A.alloc([128, 32, 128], BF16, name="sre")
            sim_, b_sim = A.alloc([128, 32, 128], BF16, name="sim")
            ttf, b_ttf = A.alloc([128, 4, 512], F32, nbufs=4, name="ttf")
            ttb, b_ttb = A.alloc([128, 4, 512], F32, nbufs=4, name="ttb")
            zz, b_zz = A.alloc([128, 2, 2, 4 * 128], F32, nbufs=2, name="zz")
            zz = zz.rearrange("p z r (c i) -> p z r c i", i=128)
            cre, b_cre = A.alloc([128, 2, 32], F32, name="cre")
            send, b_send = A.alloc([128, 2, 32], F32, name="send")
            ctmp, b_ctmp = A.alloc([128, 4, 32], F32, name="ctmp")
            yy, b_yy = A.alloc([128, 1024], F32, name="yy")
            gb, b_gb = A.alloc([128, 1024], BF16, name="gb")
            gTt, b_gTt = A.alloc([128, 8, 128], BF16, name="gTt")

            def front(gi):
                par = gi % 2
                ud, b_ud, wre, b_wre, wim, b_wim = ud2[:, par, :], b_ud2[par], wre2[:, par, :], b_wre2[par], wim2[:, par, :], b_wim2[par]
                for g2 in range(2):
                    for c in range(4):
                        kc = 4 * g2 + c
                        P.op("pe", (lambda par, kc, c, g2: lambda e: e.transpose(out=pbanks[g2][:, c * 128:(c + 1) * 128], in_=ht[:, par, kc * 128:(kc + 1) * 128],
                                                                               identity=ident))(par, kc, c, g2), R=[b_ht[par], b_ident], W=[pbuf[g2]])
                    copy_rr(hTt[:, 4 * g2:4 * g2 + 4, :], pbanks[g2].rearrange("p (a b) -> p a b", b=128), [pbuf[g2]], [b_hTt])
                for g2 in range(2):
                    for c in range(4):
                        fc = 4 * g2 + c
                        for kc in range(8):
                            P.op("pe", (lambda fc, c, kc, g2: lambda e: e.matmul(out=pbanks[g2][:, c * 128:(c + 1) * 128], lhsT=Win[:, kc, fc * 128:(fc + 1) * 128], rhs=hTt[:, kc, :],
                                                                                 start=(kc == 0), stop=(kc == 7)))(fc, c, kc, g2), R=[b_Win, b_hTt], W=[pbuf[g2]])
                    copy_rr(uTt[:, 4 * g2:4 * g2 + 4, :], pbanks[g2].rearrange("p (a b) -> p a b", b=128), [pbuf[g2]], [b_uTt])
                for hh in range(2):
                    for kc in range(8):
                        P.op("pe", (lambda hh, kc: lambda e: e.matmul(out=pbanks[2 + hh], lhsT=hTt[:, kc, :], rhs=Win[:, kc, hh * 512:(hh + 1) * 512],
                                                                    start=(kc == 0), stop=(kc == 7)))(hh, kc), R=[b_Win, b_hTt], W=[pbuf[2 + hh]])
                    P.op("dve", (lambda hh: lambda e: e.tensor_tensor(out=ud[:, hh * 512:(hh + 1) * 512], in0=pbanks[2 + hh], in1=DV[:, hh * 512:(hh + 1) * 512], op=ALU.mult))(hh),
                         R=[pbuf[2 + hh], b_DV], W=[b_ud])
                for kc in range(8):
                    br_, bi_ = (kc % 2) * 2, 1 + (kc % 2) * 2
                    cs = slice(kc * 512, (kc + 1) * 512)
                    P.op("pe", (lambda kc, br_: lambda e: e.matmul(out=pbanks[br_], lhsT=uTt[:, kc, :], rhs=BDr[:, kc, :], start=True, stop=True))(kc, br_),
                         R=[b_uTt, b_BDr], W=[pbuf[br_]])
                    P.op("pe", (lambda kc, bi_: lambda e: e.matmul(out=pbanks[bi_], lhsT=uTt[:, kc, :], rhs=BDi[:, kc, :], start=True, stop=True))(kc, bi_),
                         R=[b_uTt, b_BDi], W=[pbuf[bi_]])
                    for (k_, bnk, T_, bT_) in ((0, br_, TA, b_TA), (1, bi_, TB, b_TB), (2, bi_, TA, b_TA), (3, br_, TB, b_TB)):
                        P.op("dve", (lambda k_, bnk, T_, cs: lambda e: e.tensor_tensor(out=ttf[:, k_, :], in0=pbanks[bnk], in1=T_[:, cs], op=ALU.mult))(k_, bnk, T_, cs),
                             R=[pbuf[bnk], bT_], W=[b_ttf[k_]])
                    P.op("pool", (lambda cs: lambda e: e.tensor_tensor(out=wre[:, cs], in0=ttf[:, 0, :], in1=ttf[:, 1, :], op=ALU.subtract))(cs),
                         R=[b_ttf[0], b_ttf[1]], W=[b_wre])
                    P.op("pool", (lambda cs: lambda e: e.tensor_tensor(out=wim[:, cs], in0=ttf[:, 2, :], in1=ttf[:, 3, :], op=ALU.add))(cs),
                         R=[b_ttf[2], b_ttf[3]], W=[b_wim])

            def back(gi):
                par = gi % 2
                s_, qt = gi // NT, gi % NT
                ud, b_ud, wre, b_wre, wim, b_wim = ud2[:, par, :], b_ud2[par], wre2[:, par, :], b_wre2[par], wim2[:, par, :], b_wim2[par]
                if qt == 0:
                    P.op("pool", lambda e: e.memset(cre, 0.0), W=[b_cre])
                for q4 in range(8):
                    br_, bi_ = 4 + (q4 % 2) * 2, 5 + (q4 % 2) * 2
                    zp = q4 % 2
                    for c in range(4):
                        q = q4 * 4 + c
                        P.op("pe", (lambda q, c, br_: lambda e: e.matmul(out=pbanks[br_][:, c * 128:(c + 1) * 128], lhsT=wre[:, q * 128:(q + 1) * 128], rhs=TRIc,
                                                                       start=True, stop=True))(q, c, br_), R=[b_wre, b_TRIc], W=[pbuf[br_]])
                        P.op("pe", (lambda q, c, bi_: lambda e: e.matmul(out=pbanks[bi_][:, c * 128:(c + 1) * 128], lhsT=wim[:, q * 128:(q + 1) * 128], rhs=TRIc,
                                                                       start=True, stop=True))(q, c, bi_), R=[b_wim, b_TRIc], W=[pbuf[bi_]])
                    qs = slice(q4 * 4, q4 * 4 + 4)
                    P.op("dve", (lambda br_, qs, zp: lambda e: e.tensor_tensor(out=zz[:, zp, 0, :, :], in0=pbanks[br_].rearrange("p (a b) -> p a b", b=128),
                                                                          in1=cre[:, 0, qs].unsqueeze(2).to_broadcast([128, 4, 128]), op=ALU.add))(br_, qs, zp), R=[pbuf[br_], b_cre], W=[b_zz[zp]])
                    P.op("dve", (lambda bi_, qs, zp: lambda e: e.tensor_tensor(out=zz[:, zp, 1, :, :], in0=pbanks[bi_].rearrange("p (a b) -> p a b", b=128),
                                                                          in1=cre[:, 1, qs].unsqueeze(2).to_broadcast([128, 4, 128]), op=ALU.add))(bi_, qs, zp), R=[pbuf[bi_], b_cre], W=[b_zz[zp]])
                    tv = lambda k_: ttb[:, k_, :].rearrange("p (a b) -> p a b", b=128)
                    t0v, t1v, t2v, t3v = tv(0), tv(1), tv(2), tv(3)
                    zrv, ziv = zz[:, zp, 0, :, :], zz[:, zp, 1, :, :]
                    P.op("pool", (lambda qs, t0v, zrv: lambda e: e.tensor_tensor(out=t0v, in0=zrv, in1=Pre[:, qs, :], op=ALU.mult))(qs, t0v, zrv), R=[b_zz[zp], b_Pre], W=[b_ttb[0]])
                    P.op("pool", (lambda qs, t1v, ziv: lambda e: e.tensor_tensor(out=t1v, in0=ziv, in1=Pim[:, qs, :], op=ALU.mult))(qs, t1v, ziv), R=[b_zz[zp], b_Pim], W=[b_ttb[1]])
                    P.op("pool", (lambda qs, t2v, ziv: lambda e: e.tensor_tensor(out=t2v, in0=ziv, in1=Pre[:, qs, :], op=ALU.mult))(qs, t2v, ziv), R=[b_zz[zp], b_Pre], W=[b_ttb[2]])
                    P.op("dve", (lambda qs, t3v, zrv: lambda e: e.tensor_tensor(out=t3v, in0=zrv, in1=Pim[:, qs, :], op=ALU.mult))(qs, t3v, zrv), R=[b_zz[zp], b_Pim], W=[b_ttb[3]])
                    P.op("dve", (lambda qs, t0v, t1v: lambda e: e.tensor_tensor(out=sre[:, qs, :], in0=t0v, in1=t1v, op=ALU.subtract))(qs, t0v, t1v), R=[b_ttb[0], b_ttb[1]], W=[b_sre])
                    P.op("dve", (lambda qs, t0v, t1v: lambda e: e.tensor_tensor(out=send[:, 0, qs], in0=t0v[:, :, 127], in1=t1v[:, :, 127], op=ALU.subtract))(qs, t0v, t1v),
                         R=[b_ttb[0], b_ttb[1]], W=[b_send])
                    P.op("dve", (lambda qs, t2v, t3v: lambda e: e.tensor_tensor(out=sim_[:, qs, :], in0=t2v, in1=t3v, op=ALU.add))(qs, t2v, t3v), R=[b_ttb[2], b_ttb[3]], W=[b_sim])
                    P.op("dve", (lambda qs, t2v, t3v: lambda e: e.tensor_tensor(out=send[:, 1, qs], in0=t2v[:, :, 127], in1=t3v[:, :, 127], op=ALU.add))(qs, t2v, t3v),
                         R=[b_ttb[2], b_ttb[3]], W=[b_send])
                P.op("dve", lambda e: e.tensor_tensor(out=ctmp[:, 0, :], in0=send[:, 0, :], in1=a1[:, 0, :], op=ALU.mult), R=[b_send, b_a1], W=[b_ctmp])
                P.op("dve", lambda e: e.tensor_tensor(out=ctmp[:, 1, :], in0=send[:, 1, :], in1=a1[:, 1, :], op=ALU.mult), R=[b_send, b_a1], W=[b_ctmp])
                P.op("dve", lambda e: e.tensor_tensor(out=ctmp[:, 2, :], in0=send[:, 0, :], in1=a1[:, 1, :], op=ALU.mult), R=[b_send, b_a1], W=[b_ctmp])
                P.op("dve", lambda e: e.tensor_tensor(out=ctmp[:, 3, :], in0=send[:, 1, :], in1=a1[:, 0, :], op=ALU.mult), R=[b_send, b_a1], W=[b_ctmp])
                P.op("dve", lambda e: e.tensor_tensor(out=cre[:, 0, :], in0=ctmp[:, 0, :], in1=ctmp[:, 1, :], op=ALU.subtract), R=[b_ctmp], W=[b_cre])
                P.op("dve", lambda e: e.tensor_tensor(out=cre[:, 1, :], in0=ctmp[:, 2, :], in1=ctmp[:, 3, :], op=ALU.add), R=[b_ctmp], W=[b_cre])
                for q in range(32):
                    bk_ = 4 + q // 16
                    col = (q % 16) * 32
                    P.op("pe", (lambda q, bk_, col: lambda e: e.matmul(out=pbanks[bk_][:, col:col + 32], lhsT=sre[:, q, :], rhs=CmR[:, q * 32:(q + 1) * 32], start=True, stop=False))(q, bk_, col),
                         R=[b_sre, b_CmR], W=[pbuf[bk_]])
                    P.op("pe", (lambda q, bk_, col: lambda e: e.matmul(out=pbanks[bk_][:, col:col + 32], lhsT=sim_[:, q, :], rhs=CmI[:, q * 32:(q + 1) * 32], start=False, stop=True))(q, bk_, col),
                         R=[b_sim, b_CmI], W=[pbuf[bk_]])
                for hh in range(2):
                    P.op("dve", (lambda hh: lambda e: e.tensor_tensor(out=yy[:, hh * 512:(hh + 1) * 512], in0=pbanks[4 + hh], in1=ud[:, hh * 512:(hh + 1) * 512], op=ALU.add))(hh),
                         R=[pbuf[4 + hh], b_ud], W=[b_yy])
                P.op("act", lambda e: e.activation(out=gb, in_=yy, func=AF.Gelu), R=[b_yy], W=[b_gb])
                for kc in range(8):
                    P.op("pe", (lambda kc: lambda e: e.transpose(out=pbf(6)[:, kc * 128:(kc + 1) * 128], in_=gb[:, kc * 128:(kc + 1) * 128], identity=identb))(kc),
                         R=[b_gb, b_identb], W=[pbuf[6]])
                copy_rr(gTt, pbf(6).rearrange("p (a b) -> p a b", b=128), [pbuf[6]], [b_gTt])
                P.dma(gT_dram[s_, :, :, qt * 128:(qt + 1) * 128], gTt, R=[b_gTt], W=[gT_b[s_]])

            NGT = SEQ_PER_CORE * NT

            def hload(gi):
                P.dma(ht[:, gi % 2, :], hres[gi * 128:(gi + 1) * 128, :], R=[hres_b[gi]], W=[b_ht[gi % 2]])
            hload(0)
            hload(1)
            front(0)
            for gi in range(NGT):
                if gi + 2 < NGT:
                    hload(gi + 2)
                if gi + 1 < NGT:
                    P.fork(2)
                    P.stream(0)
                    front(gi + 1)
                    P.stream(1)
                    back(gi)
                    n0, n1 = len(P.streams[0]), len(P.streams[1])
                    P.join([max(1, n0 // n1), max(1, n1 // n0)])
                else:
                    back(gi)
            A.release(m_scan)
            A.release(m_s5)
            m_g = A.mark()
            LG1b, b_LG1b = A.alloc([128, 1024], F32, name="LG1b")
            LB1b, b_LB1b = A.alloc([128, 1024], F32, name="LB1b")
            P.dma(LG1b, ln_mix_g[1].partition_broadcast(128), W=[b_LG1b])
            P.dma(LB1b, ln_mix_b[1].partition_broadcast(128), W=[b_LB1b])
            W1, b_W1 = A.alloc([128, 8, 1024], BF16, name="W1")
            W2, b_W2 = A.alloc([128, 8, 1024], BF16, name="W2")
            Wo1, b_Wo1 = A.alloc([128, 8, 1024], BF16, name="Wo1")
            wdma(W1, b_W1, s5_glu_w1[0])
            wdma(W2, b_W2, s5_glu_w2[0])
            wdma(Wo1, b_Wo1, s5_w_out[0])
            gTs2, b_gTs2 = A.alloc([128, 2, 8, 512], BF16, nbufs=2, name="gTs")
            zT2, b_zT2 = A.alloc([128, 2, 8, 512], BF16, nbufs=2, name="zT")
            sgm, b_sgm = A.alloc([128, 2, 512], F32, nbufs=2, name="sgm")
            ght, b_ght = A.alloc([128, 2, 1024], F32, nbufs=2, name="ght")
            rr3, b_rr3 = A.alloc([128, 1024], F32, name="rr3")
            h3, b_h3 = A.alloc([128, 2, 1024], F32, nbufs=2, name="h3")
            if True:
                def gfront(bi_):
                    s_, nb_ = bi_ // 4, bi_ % 4
                    gTs, b_gTs, zT, b_zT = gTs2[:, bi_ % 2], b_gTs2[bi_ % 2], zT2[:, bi_ % 2], b_zT2[bi_ % 2]
                    for fc in range(8):
                        b1_, b2_ = (fc % 2) * 2, 1 + (fc % 2) * 2
                        for kc in range(8):
                            P.op("pe", (lambda fc, kc, b1_: lambda e: e.matmul(out=pbanks[b1_], lhsT=W1[:, kc, fc * 128:(fc + 1) * 128], rhs=gTs[:, kc, :], start=(kc == 0), stop=(kc == 7)))(fc, kc, b1_),
                                 R=[b_W1, b_gTs], W=[pbuf[b1_]])
                        for kc in range(8):
                            P.op("pe", (lambda fc, kc, b2_: lambda e: e.matmul(out=pbanks[b2_], lhsT=W2[:, kc, fc * 128:(fc + 1) * 128], rhs=gTs[:, kc, :], start=(kc == 0), stop=(kc == 7)))(fc, kc, b2_),
                                 R=[b_W2, b_gTs], W=[pbuf[b2_]])
                        sp_ = fc % 2
                        P.op("act", (lambda b2_, sp_: lambda e: e.activation(out=sgm[:, sp_, :], in_=pbanks[b2_], func=AF.Sigmoid))(b2_, sp_), R=[pbuf[b2_]], W=[b_sgm[sp_]])
                        P.op("dve", (lambda fc, b1_, sp_: lambda e: e.tensor_tensor(out=zT[:, fc, :], in0=pbanks[b1_], in1=sgm[:, sp_, :], op=ALU.mult))(fc, b1_, sp_),
                             R=[pbuf[b1_], b_sgm[sp_]], W=[b_zT])
                def gback(bi_):
                    s_, nb_ = bi_ // 4, bi_ % 4
                    gTs, b_gTs, zT, b_zT = gTs2[:, bi_ % 2], b_gTs2[bi_ % 2], zT2[:, bi_ % 2], b_zT2[bi_ % 2]
                    for t4 in range(4):
                        gi = s_ * NT + nb_ * 4 + t4
                        par = gi % 2
                        for hh in range(2):
                            for fc in range(8):
                                P.op("pe", (lambda t4, hh, fc: lambda e: e.matmul(out=pbanks[4 + hh], lhsT=zT[:, fc, t4 * 128:(t4 + 1) * 128], rhs=Wo1[:, fc, hh * 512:(hh + 1) * 512],
                                                                                start=(fc == 0), stop=(fc == 7)))(t4, hh, fc), R=[b_zT, b_Wo1], W=[pbuf[4 + hh]])
                        P.dma(ght[:, par, :], hres[gi * 128:(gi + 1) * 128, :], R=[hres_b[gi]], W=[b_ght[par]])
                        for hh in range(2):
                            P.op("dve", (lambda hh, par: lambda e: e.scalar_tensor_tensor(out=rr3[:, hh * 512:(hh + 1) * 512], in0=ght[:, par, hh * 512:(hh + 1) * 512], scalar=ALPHA,
                                                                                       in1=pbanks[4 + hh], op0=ALU.mult, op1=ALU.add))(hh, par), R=[b_ght[par], pbuf[4 + hh]], W=[b_rr3])
                        m_ln = A.mark()
                        layer_norm_tile(rr3, b_rr3, LG1b, LB1b, b_LG1b, h3[:, par, :], b_h3[par], b_LB1b)
                        A.release(m_ln)
                        P.dma(hres[gi * 128:(gi + 1) * 128, :], h3[:, par, :], R=[b_h3[par]], W=[hres_b[gi]])
            NB_ = SEQ_PER_CORE * 4

            def gload(bi_):
                P.dma(gTs2[:, bi_ % 2], gT_dram[bi_ // 4, :, :, (bi_ % 4) * 512:(bi_ % 4 + 1) * 512], R=[gT_b[bi_ // 4]], W=[b_gTs2[bi_ % 2]])
            gload(0)
            gload(1)
            gfront(0)
            for bi_ in range(NB_):
                if bi_ + 2 < NB_:
                    gload(bi_ + 2)
                if bi_ + 1 < NB_:
                    P.fork(2)
                    P.stream(0)
                    gfront(bi_ + 1)
                    P.stream(1)
                    gback(bi_)
                    n0, n1 = len(P.streams[0]), len(P.streams[1])
                    P.join([max(1, n0 // n1), max(1, n1 // n0)])
                else:
                    gback(bi_)
            A.release(m_g)

        if os.environ.get("PRECAST", "1") == "1":
            while precast_state["i"] < 32:
                precast_next()
        if debug != "l0":
            moe_stage(0, final=(debug == "moe0"))
        if debug is None or debug in ("s5", "full", "s5dbg"):
            s5_stage()
            if debug == "s5dbg":
                pass
            elif debug == "s5":
                for gi in range(SEQ_PER_CORE * NT):
                    P.dma(y[gi // NT, (gi % NT) * 128:(gi % NT + 1) * 128, :], hres[gi * 128:(gi + 1) * 128, :], R=[hres_b[gi]], W=[y_b])
            else:
                moe_stage(1, final=True)
        if debug == "l0":
            for gi in range(SEQ_PER_CORE * NT):
                s, qt = gi // NT, gi % NT
                if s >= nseq or qt >= nqt or stop:
                    continue
                P.dma(y[s, qt * 128:(qt + 1) * 128, :], hres[gi * 128:(gi + 1) * 128, :], R=[hres_b[gi]], W=[y_b])
        P.emit()
        print("ops", len(P.ops), P.stats, flush=True)
    return nc


_NC_CACHE = {}


def kernel(**inputs):
    n = 8
    if "nc" not in _NC_CACHE:
        _NC_CACHE["nc"] = build()
    nc = _NC_CACHE["nc"]
    x = np.ascontiguousarray(inputs["x"], dtype=np.float32)
    in_maps = []
    for c in range(n):
        m = {k: np.ascontiguousarray(v) for k, v in inputs.items() if k != "x"}
        m["x"] = x[c * SEQ_PER_CORE:(c + 1) * SEQ_PER_CORE]
        in_maps.append(m)
    res = run_bass_kernel_spmd(nc, in_maps, core_ids=list(range(n)))
    return np.concatenate([r["y"] for r in res.results], axis=0)
```
